# Optimizing a Trainium2 kernel written in Bass

```python
import jax, jax.numpy as jnp
from jax import lax
import numpy as np

D_MODEL = 1024
BATCH = 8
SEQ = 4096
DEPTH = 1

MEM_LEN = 256
GRID_W = 64
ROPE_THETA = 10000.0
Q_BLOCK = 128
RMS_EPS = 1e-6
LN_EPS = 1e-5

GQA_HEADS = 8
GQA_KV_HEADS = 2
GQA_HEAD_DIM = D_MODEL // 16
MLA_HEADS = 8
MLA_NOPE_DIM = D_MODEL // 16
MLA_ROPE_DIM = D_MODEL // 32
MLA_V_DIM = D_MODEL // 16
MLA_Q_LORA = 3 * D_MODEL // 8
MLA_KV_LORA = D_MODEL // 4
MEM_HEADS = 4
MEM_HEAD_DIM = D_MODEL // 8
N_BRANCH = 3

GQA_Q_W = GQA_HEADS * GQA_HEAD_DIM
GQA_KV_W = GQA_KV_HEADS * GQA_HEAD_DIM
MLA_OUT_W = MLA_HEADS * MLA_V_DIM
MEM_Q_W = MEM_HEADS * MEM_HEAD_DIM
GATE_W = N_BRANCH * D_MODEL
SPLIT_SIZES = (GQA_Q_W, GQA_KV_W, GQA_KV_W, MLA_Q_LORA, MLA_KV_LORA, MLA_ROPE_DIM, MEM_Q_W, GATE_W)
SPLIT_OFFSETS = tuple(int(v) for v in np.cumsum(SPLIT_SIZES)[:-1])
IN_PROJ_W = sum(SPLIT_SIZES)

N_EXPERTS = 32
TOP_K = 4
D_EXPERT = D_MODEL
SWIGLU_LIMIT = 7.0
SWIGLU_ALPHA = 1.702
EXPERT_BLOCK = 128

DEEPNORM_ALPHA = (2 * DEPTH) ** 0.25
DEEPNORM_BETA = (8 * DEPTH) ** -0.25

kernel_name = "hybrid_gqa_mla_mem_moe_deepnorm_encoder"


def layer_norm(x, g, b):
    xf = x.astype(jnp.float32)
    mu = jnp.mean(xf, axis=-1, keepdims=True)
    xc = xf - mu
    var = jnp.mean(xc * xc, axis=-1, keepdims=True)
    return (xc * lax.rsqrt(var + LN_EPS) * g + b).astype(x.dtype)


def rms_norm(x, g):
    xf = x.astype(jnp.float32)
    return (xf * lax.rsqrt(jnp.mean(xf * xf, axis=-1, keepdims=True) + RMS_EPS) * g).astype(x.dtype)


def rope_1d(x, pos):
    half = x.shape[-1] // 2
    inv = ROPE_THETA ** (-jnp.arange(half, dtype=jnp.float32) / half)
    ang = pos.astype(jnp.float32)[:, None] * inv[None, :]
    c = jnp.cos(ang)[None, :, None, :]
    s = jnp.sin(ang)[None, :, None, :]
    x1 = x[..., :half].astype(jnp.float32)
    x2 = x[..., half:].astype(jnp.float32)
    return jnp.concatenate([x1 * c - x2 * s, x2 * c + x1 * s], axis=-1).astype(x.dtype)


def axial_rope(x, row, col):
    d = x.shape[-1] // 2
    return jnp.concatenate([rope_1d(x[..., :d], row), rope_1d(x[..., d:], col)], axis=-1)


def blocked_attention(q, k, v):
    B, S, Hk, G, Dq = q.shape
    nqb = S // Q_BLOCK
    qb = (q * (Dq ** -0.5)).reshape(B, nqb, Q_BLOCK, Hk, G, Dq).transpose(1, 0, 2, 3, 4, 5)

    def attend(qblk):
        s = jnp.einsum('bqhgd,bthd->bhgqt', qblk, k, preferred_element_type=jnp.float32)
        p = jax.nn.softmax(s, axis=-1).astype(v.dtype)
        return jnp.einsum('bhgqt,bthe->bqhge', p, v)

    o = lax.map(attend, qb)
    return o.transpose(1, 0, 2, 3, 4, 5).reshape(B, S, Hk, G, v.shape[-1])


def hybrid_mixer(h, mem, row, col, w_in_proj, b_gate, gqa_q_norm, gqa_k_norm, mla_q_norm,
                 mla_kv_norm, w_mla_qb, w_mla_kvb, w_mem_kv, w_br_gqa, w_br_mla, w_br_mem, w_out):
    B, S, D = h.shape
    proj = h @ w_in_proj
    q_a, k_a, v_a, q_lat, kv_lat, k_rope, q_m, gate_pre = jnp.split(proj, SPLIT_OFFSETS, axis=-1)

    qa = axial_rope(rms_norm(q_a.reshape(B, S, GQA_HEADS, GQA_HEAD_DIM), gqa_q_norm), row, col)
    ka = axial_rope(rms_norm(k_a.reshape(B, S, GQA_KV_HEADS, GQA_HEAD_DIM), gqa_k_norm), row, col)
    va = v_a.reshape(B, S, GQA_KV_HEADS, GQA_HEAD_DIM)
    group = GQA_HEADS // GQA_KV_HEADS
    oa = blocked_attention(qa.reshape(B, S, GQA_KV_HEADS, group, GQA_HEAD_DIM), ka, va).reshape(B, S, GQA_Q_W)

    qm = (rms_norm(q_lat, mla_q_norm) @ w_mla_qb).reshape(B, S, MLA_HEADS, MLA_NOPE_DIM + MLA_ROPE_DIM)
    q_nope, q_pe = qm[..., :MLA_NOPE_DIM], axial_rope(qm[..., MLA_NOPE_DIM:], row, col)
    kv = (rms_norm(kv_lat, mla_kv_norm) @ w_mla_kvb).reshape(B, S, MLA_HEADS, MLA_NOPE_DIM + MLA_V_DIM)
    k_nope, v_b = kv[..., :MLA_NOPE_DIM], kv[..., MLA_NOPE_DIM:]
    k_pe = axial_rope(k_rope[:, :, None, :], row, col)
    k_b = jnp.concatenate([k_nope, jnp.broadcast_to(k_pe, (B, S, MLA_HEADS, MLA_ROPE_DIM))], axis=-1)
    q_b = jnp.concatenate([q_nope, q_pe], axis=-1)[:, :, :, None, :]
    ob = blocked_attention(q_b, k_b, v_b).reshape(B, S, MLA_OUT_W)

    mkv = mem @ w_mem_kv
    M = mem.shape[1]
    mk = mkv[..., :MEM_Q_W].reshape(B, M, MEM_HEADS, MEM_HEAD_DIM)
    mv = mkv[..., MEM_Q_W:].reshape(B, M, MEM_HEADS, MEM_HEAD_DIM)
    oc = blocked_attention(q_m.reshape(B, S, MEM_HEADS, 1, MEM_HEAD_DIM), mk, mv).reshape(B, S, MEM_Q_W)

    gates = jax.nn.sigmoid((gate_pre + b_gate).astype(jnp.float32)).astype(h.dtype).reshape(B, S, N_BRANCH, D)
    merged = (gates[:, :, 0] * (oa @ w_br_gqa)
              + gates[:, :, 1] * (ob @ w_br_mla)
              + gates[:, :, 2] * (oc @ w_br_mem))
    return merged @ w_out


def moe_ffn(h, w_router, b_router, w_exp_in, b_exp_in, w_exp_out, b_exp_out):
    B, S, D = h.shape
    N = B * S
    A = N * TOP_K
    xt = h.reshape(N, D)
    logits = jnp.dot(xt, w_router, preferred_element_type=jnp.float32) + b_router.astype(jnp.float32)
    top_val, top_idx = lax.top_k(logits, TOP_K)
    gate = jax.nn.softmax(top_val, axis=-1)

    flat_e = top_idx.reshape(A).astype(jnp.int32)
    flat_tok = jnp.repeat(jnp.arange(N, dtype=jnp.int32), TOP_K)
    flat_w = gate.reshape(A)
    order = jnp.argsort(flat_e)
    e_s, tok_s, w_s = flat_e[order], flat_tok[order], flat_w[order]
    counts = jnp.bincount(flat_e, length=N_EXPERTS).astype(jnp.int32)
    padded = (counts + EXPERT_BLOCK - 1) // EXPERT_BLOCK * EXPERT_BLOCK
    pad_end = jnp.cumsum(padded)
    pad_start = pad_end - padded
    grp_start = jnp.cumsum(counts) - counts
    dest = pad_start[e_s] + jnp.arange(A, dtype=jnp.int32) - grp_start[e_s]
    n_blocks = -(-A // EXPERT_BLOCK) + N_EXPERTS
    n_slots = n_blocks * EXPERT_BLOCK
    slot_tok = jnp.full((n_slots,), N, jnp.int32).at[dest].set(tok_s)
    slot_w = jnp.zeros((n_slots,), jnp.float32).at[dest].set(w_s)
    blk_start = jnp.arange(n_blocks, dtype=jnp.int32) * EXPERT_BLOCK
    blk_e = jnp.minimum(jnp.searchsorted(pad_end, blk_start, side='right'), N_EXPERTS - 1).astype(jnp.int32)
    x_pad = jnp.concatenate([xt, jnp.zeros((1, D), xt.dtype)], axis=0)
    xs = x_pad[slot_tok].reshape(n_blocks, EXPERT_BLOCK, D)

    def expert_block(args):
        xb, e = args
        hb = xb @ w_exp_in[e] + b_exp_in[e]
        x_glu = jnp.minimum(hb[:, :D_EXPERT], SWIGLU_LIMIT)
        x_lin = jnp.clip(hb[:, D_EXPERT:], -SWIGLU_LIMIT, SWIGLU_LIMIT)
        act = x_glu * jax.nn.sigmoid(SWIGLU_ALPHA * x_glu) * (x_lin + 1.0)
        return act @ w_exp_out[e] + b_exp_out[e]

    ys = lax.map(expert_block, (xs, blk_e)).reshape(n_slots, D)
    ys = ys * slot_w[:, None].astype(ys.dtype)
    out = jnp.zeros((N + 1, D), ys.dtype).at[slot_tok].add(ys)[:N]
    return out.reshape(B, S, D)


def setup_inputs(seed: int = 0) -> dict:
    key = jax.random.key(seed)
    ks = iter(jax.random.split(key, 32))

    def w(shape, fan_in, scale=1.0):
        return jax.random.normal(next(ks), shape, jnp.float32) * (fan_in ** -0.5) * scale

    def gain(shape):
        return 1.0 + 0.05 * jax.random.normal(next(ks), shape, jnp.float32)

    def bias(shape, scale=0.02):
        return scale * jax.random.normal(next(ks), shape, jnp.float32)

    L, D = DEPTH, D_MODEL
    return {
        "x": jax.random.normal(next(ks), (BATCH, SEQ, D), jnp.float32),
        "mem": jax.random.normal(next(ks), (BATCH, MEM_LEN, D), jnp.float32),
        "ln_in_g": gain((D,)),
        "ln_in_b": bias((D,)),
        "w_in_proj": w((L, D, IN_PROJ_W), D),
        "b_gate": bias((L, GATE_W)),
        "gqa_q_norm": gain((L, GQA_HEAD_DIM)),
        "gqa_k_norm": gain((L, GQA_HEAD_DIM)),
        "mla_q_norm": gain((L, MLA_Q_LORA)),
        "mla_kv_norm": gain((L, MLA_KV_LORA)),
        "w_mla_qb": w((L, MLA_Q_LORA, MLA_HEADS * (MLA_NOPE_DIM + MLA_ROPE_DIM)), MLA_Q_LORA),
        "w_mla_kvb": w((L, MLA_KV_LORA, MLA_HEADS * (MLA_NOPE_DIM + MLA_V_DIM)), MLA_KV_LORA),
        "w_mem_kv": w((L, D, 2 * MEM_Q_W), D),
        "w_br_gqa": w((L, GQA_Q_W, D), GQA_Q_W),
        "w_br_mla": w((L, MLA_OUT_W, D), MLA_OUT_W),
        "w_br_mem": w((L, MEM_Q_W, D), MEM_Q_W),
        "w_out": w((L, D, D), D, DEEPNORM_BETA),
        "ln1_g": gain((L, D)),
        "ln1_b": bias((L, D)),
        "w_router": w((L, D, N_EXPERTS), D),
        "b_router": bias((L, N_EXPERTS), 0.01),
        "w_exp_in": w((L, N_EXPERTS, D, 2 * D_EXPERT), D),
        "b_exp_in": bias((L, N_EXPERTS, 2 * D_EXPERT)),
        "w_exp_out": w((L, N_EXPERTS, D_EXPERT, D), D_EXPERT, DEEPNORM_BETA),
        "b_exp_out": bias((L, N_EXPERTS, D)),
        "ln2_g": gain((L, D)),
        "ln2_b": bias((L, D)),
    }


def reference(x, mem, ln_in_g, ln_in_b, w_in_proj, b_gate, gqa_q_norm, gqa_k_norm, mla_q_norm,
              mla_kv_norm, w_mla_qb, w_mla_kvb, w_mem_kv, w_br_gqa, w_br_mla, w_br_mem, w_out,
              ln1_g, ln1_b, w_router, b_router, w_exp_in, b_exp_in, w_exp_out, b_exp_out,
              ln2_g, ln2_b):
    S = x.shape[1]
    rows = S // GRID_W
    row = jnp.repeat(jnp.arange(rows, dtype=jnp.int32), GRID_W)
    col = jnp.tile(jnp.arange(GRID_W, dtype=jnp.int32), rows)
    h = layer_norm(x, ln_in_g, ln_in_b)
    for l in range(DEPTH):
        mix = hybrid_mixer(h, mem, row, col, w_in_proj[l], b_gate[l], gqa_q_norm[l], gqa_k_norm[l],
                           mla_q_norm[l], mla_kv_norm[l], w_mla_qb[l], w_mla_kvb[l], w_mem_kv[l],
                           w_br_gqa[l], w_br_mla[l], w_br_mem[l], w_out[l])
        h = layer_norm(DEEPNORM_ALPHA * h + mix, ln1_g[l], ln1_b[l])
        ffn = moe_ffn(h, w_router[l], b_router[l], w_exp_in[l], b_exp_in[l], w_exp_out[l], b_exp_out[l])
        h = layer_norm(DEEPNORM_ALPHA * h + ffn, ln2_g[l], ln2_b[l])
    return h
```

```python
import numpy as np
import ml_dtypes
from contextlib import ExitStack, contextmanager

import concourse.bass as bass
import concourse.mybir as mybir
from concourse.bass_utils import run_bass_kernel_spmd

F32 = mybir.dt.float32
BF16 = mybir.dt.bfloat16
I32 = mybir.dt.int32
U32 = mybir.dt.uint32
ALU = mybir.AluOpType
AF = mybir.ActivationFunctionType
AX = mybir.AxisListType

S = 4096
D = 1024
NT = S // 128
NB = S // 512
MEM = 256
NE = 32
TOPK = 4
CAPT = 6
CAP = CAPT * 128
NSLOT = NE * CAP
ALPHA = 2.0 ** 0.25
RMS_EPS = 1e-6
LN_EPS = 1e-5
OFF_QA, OFF_KA, OFF_VA, OFF_QL, OFF_KVL, OFF_KR, OFF_QM, OFF_G = 0, 512, 640, 768, 1152, 1408, 1440, 1952
INW = 5024

ENGS = ("pe", "act", "dve", "pool", "sp")


class Sched:
    EPOCH = 30000

    def __init__(self, nc, es, ndma=None):
        self.nc, self.es = nc, es
        self.prog = {e: [] for e in ENGS}
        self.esem = {e: es.enter_context(nc.semaphore(f"e_{e}_0")) for e in ENGS}
        self.eold = {e: set() for e in ENGS}
        self.nep = {e: 0 for e in ENGS}
        self.ecnt = {e: 0 for e in ENGS}
        self.seen = {e: {} for e in ENGS}
        self.lastw, self.readers = {}, {}
        ndma = ndma or {"sp": 28, "pool": 24, "act": 6}
        self.dsem = {q: [es.enter_context(nc.semaphore(f"d_{q}_{i}")) for i in range(n)] for q, n in ndma.items()}
        self.duse = {q: [0] * n for q, n in ndma.items()}
        self.dnext = {q: 0 for q in ndma}
        self.ninstr = 0

    def _deps(self, e, R, W):
        deps = {}

        def add(tok):
            if tok is None:
                return
            s, v = tok
            if deps.get(id(s), (None, 0))[1] < v:
                deps[id(s)] = (s, v)
        for r in R:
            add(self.lastw.get(r))
        for w in W:
            add(self.lastw.get(w))
            for tok in self.readers.get(w, {}).values():
                add(tok)
        waits = []
        for k, (s, v) in deps.items():
            if id(s) in self.eold[e]:
                continue
            if s is self.esem[e] and e == "pe":
                continue
            if self.seen[e].get(k, 0) >= v:
                continue
            self.seen[e][k] = v
            waits.append((s, v))
        return waits

    def _record(self, tok, R, W):
        for w in W:
            self.lastw[w] = tok
            self.readers[w] = {}
        for r in R:
            d = self.readers.setdefault(r, {})
            if d.get(id(tok[0]), (None, 0))[1] < tok[1]:
                d[id(tok[0])] = tok

    def op(self, e, fn, R=(), W=()):
        waits = self._deps(e, R, W)
        if self.ecnt[e] >= self.EPOCH:
            self.eold[e].add(id(self.esem[e]))
            self.nep[e] += 1
            self.esem[e] = self.es.enter_context(self.nc.semaphore(f"e_{e}_{self.nep[e]}"))
            self.ecnt[e] = 0
        self.ecnt[e] += 1
        sem = self.esem[e]
        tok = (sem, self.ecnt[e])

        def emit(eng, waits=waits, fn=fn, sem=sem):
            for s, v in waits:
                eng.wait_ge(s, v)
            fn(eng).then_inc(sem, 1)
        self.prog[e].append(emit)
        self._record(tok, R, W)
        self.ninstr += 1 + len(waits)
        return tok

    def dma(self, q, fn, R=(), W=()):
        waits = self._deps(q, R, W)
        i = self.dnext[q]
        self.dnext[q] = (i + 1) % len(self.dsem[q])
        sem = self.dsem[q][i]
        prev = self.duse[q][i]
        if prev and self.seen[q].get(id(sem), 0) < 16 * prev:
            self.seen[q][id(sem)] = 16 * prev
            waits.append((sem, 16 * prev))
        self.duse[q][i] = prev + 1
        tok = (sem, 16 * (prev + 1))

        def emit(eng, waits=waits, fn=fn, sem=sem):
            for s, v in waits:
                eng.wait_ge(s, v)
            fn(eng).then_inc(sem, 16)
        self.prog[q].append(emit)
        self._record(tok, R, W)
        self.ninstr += 1 + len(waits)
        return tok

    def barrier(self):
        toks = [(self.esem[e], self.ecnt[e]) for e in ENGS if self.ecnt[e] > 0]
        for q in self.dsem:
            for i, s in enumerate(self.dsem[q]):
                if self.duse[q][i]:
                    toks.append((s, 16 * self.duse[q][i]))
        for e in ENGS:
            waits = [(s, v) for (s, v) in toks if self.seen[e].get(id(s), 0) < v]
            for s, v in waits:
                self.seen[e][id(s)] = v

            def emit(eng, waits=waits):
                for s, v in waits:
                    eng.wait_ge(s, v)
            self.prog[e].append(emit)
            self.ninstr += len(waits)

    def fence(self, e, toks):
        best = {}
        for s, v in toks:
            if best.get(id(s), (None, 0))[1] < v:
                best[id(s)] = (s, v)

        def emit(eng, best=best):
            for s, v in best.values():
                eng.wait_ge(s, v)
        self.prog[e].append(emit)

    def run(self):
        nc = self.nc
        with nc.Block() as block:
            @block.tensor
            def _(eng):
                for f in self.prog["pe"]:
                    f(eng)

            @block.scalar
            def _(eng):
                for f in self.prog["act"]:
                    f(eng)

            @block.vector
            def _(eng):
                for f in self.prog["dve"]:
                    f(eng)

            @block.gpsimd
            def _(eng):
                self.bc_reg = eng.to_reg(NSLOT - 1)
                for f in self.prog["pool"]:
                    f(eng)

            @block.sync
            def _(eng):
                for f in self.prog["sp"]:
                    f(eng)


@contextmanager
def scope(k):
    with ExitStack() as es:
        yield es
        k.sc.barrier()


def run_pipelined(gens, depth, skew=1):
    gens = iter(gens)
    active, rnd, done = [], 0, False
    while True:
        if not done and len(active) < depth and rnd % skew == 0:
            g = next(gens, None)
            if g is None:
                done = True
            else:
                active.append(g)
        if done and not active:
            break
        for g in list(active):
            try:
                next(g)
            except StopIteration:
                active.remove(g)
        rnd += 1


def bcast_rows(ap, n=128):
    return ap.partition_broadcast(n)


class K:
    pass


def build(upto=99, dbg=()):
    nc = bass.Bass("TRN2", target_bir_lowering=False)
    k = K()
    k.nc = nc
    k.inputs = []
    k.dbg = dbg

    def dt_in(name, shape, dt=F32):
        k.inputs.append(name)
        return nc.dram_tensor(name, list(shape), dt, kind="ExternalInput")
    k.x = dt_in("x", [S, D])
    k.ln_in_g = dt_in("ln_in_g", [1, D])
    k.ln_in_b = dt_in("ln_in_b", [1, D])
    k.c_ident = dt_in("c_ident", [128, 128], BF16)
    k.c_identf = dt_in("c_identf", [128, 128])
    if upto >= 2:
        k.mem = dt_in("mem", [MEM, D])
        k.w_in = dt_in("w_in_proj", [D, INW])
        k.w_kr_sw = dt_in("w_kr_sw", [D, 32])
        k.gqa_qn = dt_in("gqa_q_norm", [1, 512])
        k.gqa_kn = dt_in("gqa_k_norm", [1, 128])
        k.mla_qn = dt_in("mla_q_norm", [1, 384])
        k.mla_kvn = dt_in("mla_kv_norm", [1, 256])
        k.w_qb = dt_in("w_mla_qb", [384, 768])
        k.w_qb_sw = dt_in("w_mla_qb_sw", [384, 768])
        k.w_kvb = dt_in("w_mla_kvb", [256, 1024])
        k.w_mkv = dt_in("w_mem_kv", [D, D])
        k.c_rope64 = dt_in("c_rope64", [128, 2, NT, 64])
        k.c_ropeT = dt_in("c_ropeT", [128, 2, S])
        k.c_sel = dt_in("c_sel", [128, 128])
    if upto >= 5:
        k.b_gate = dt_in("b_gate", [1, 3 * D])
        k.w_br_a = dt_in("w_br_gqa", [512, D])
        k.w_br_b = dt_in("w_br_mla", [512, D])
        k.w_br_c = dt_in("w_br_mem", [512, D])
        k.w_out = dt_in("w_out", [D, D])
        k.ln1_g = dt_in("ln1_g", [1, D])
        k.ln1_b = dt_in("ln1_b", [1, D])
        k.w_router = dt_in("w_router", [D, NE])
        k.b_router = dt_in("b_router", [1, NE])
        k.c_tri = dt_in("c_tri", [128, 128], BF16)
        k.c_ecap = dt_in("c_ecap", [128, NE])
    if upto >= 6:
        k.w_ein = dt_in("w_exp_in", [NE, D, 2 * D])
        k.b_ein = dt_in("b_exp_in", [128, NE * 16])
        k.w_eout = dt_in("w_exp_out", [NE, D, D])
        k.b_eout = dt_in("b_exp_out", [NE, D])
        k.ln2_g = dt_in("ln2_g", [1, D])
        k.ln2_b = dt_in("ln2_b", [1, D])
    k.y = nc.dram_tensor("y", [S, D], F32, kind="ExternalOutput")
    sk = lambda name: ("ExternalOutput" if name in dbg else "Internal")
    k.h0_d = nc.dram_tensor("h0_d", [S, D], F32, kind=sk("h0_d"))
    k.h0T_d = nc.dram_tensor("h0T_d", [NT, 128, D], BF16, kind=sk("h0T_d"))
    k.oA_d = nc.dram_tensor("oA_d", [8, 64, S], BF16, kind=sk("oA_d"))
    k.oB_d = nc.dram_tensor("oB_d", [8, 64, S], BF16, kind=sk("oB_d"))
    k.oC_d = nc.dram_tensor("oC_d", [4, 128, S], BF16, kind=sk("oC_d"))
    k.h1_d = nc.dram_tensor("h1_d", [S, D], F32, kind=sk("h1_d"))
    k.xs_d = nc.dram_tensor("xs_d", [NSLOT, D], BF16, kind=sk("xs_d"))
    k.ys_d = nc.dram_tensor("ys_d", [NSLOT, D], F32, kind=sk("ys_d"))
    k.rt_d = nc.dram_tensor("rt_d", [S, 8], F32, kind=sk("rt_d"))

    with ExitStack() as es:
        sc = Sched(nc, es)
        k.sc = sc
        k.pp = [es.enter_context(nc.psum_tensor(f"pp{i}", [128, 1024], F32)) for i in range(4)]
        k.ppb = [p.bitcast(BF16) for p in k.pp]
        sb = lambda name, shape, dt=F32: es.enter_context(nc.sbuf_tensor(name, list(shape), dt))
        k.ident = sb("ident", [128, 128], BF16)
        k.identf = sb("identf", [128, 128], F32)
        k.neghalf = sb("neghalf", [128, 8], F32)
        k.ones_bf = sb("ones_bf", [128, 128], BF16)
        k.ones_f = sb("ones_f", [128, 128], F32)
        k.slots = sb("slots_all", [128, NT, 4], I32)
        k.wts = sb("wts_all", [128, NT, 4], F32)
        sc.dma("sp", lambda e: e.dma_start(out=k.ident[:, :], in_=k.c_ident[:, :]), W=["ident"])
        sc.dma("sp", lambda e: e.dma_start(out=k.identf[:, :], in_=k.c_identf[:, :]), W=["identf"])
        if upto >= 2:
            k.sel = sb("sel", [128, 128], F32)
            sc.dma("sp", lambda e: e.dma_start(out=k.sel[:, :], in_=k.c_sel[:, :]), W=["sel"])
        sc.op("pool", lambda e: e.memset(k.neghalf[:, :], -0.5), W=["neghalf"])
        sc.op("pool", lambda e: e.memset(k.ones_bf[:, :], 1.0), W=["ones_bf"])
        sc.op("pool", lambda e: e.memset(k.ones_f[:, :], 1.0), W=["ones_f"])

        phase1(k)
        if upto >= 2:
            phase2(k)
        if upto >= 3:
            phase3(k)
        if upto >= 4:
            phase4(k)
        if upto >= 5:
            phase5(k)
        if upto >= 6:
            phase6(k)
            phase7(k)
        toks = [t for r, t in sc.lastw.items() if isinstance(r, tuple) and r[0] in ("y", "dram")]
        sc.fence("sp", toks)
        sc.run()
    k.ninstr = sc.ninstr
    return nc, k


def psb(k, i):
    return k.pp[i // 2][:, (i % 2) * 512:(i % 2 + 1) * 512]


def psbf(k, i):
    return k.ppb[i // 2][:, (i % 2) * 1024:(i % 2 + 1) * 1024]


def layer_norm_tile(k, tag, z, g_rep, b_rep, out, scr, stages=False):
    g = _ln_gen(k, tag, z, g_rep, b_rep, out, scr)
    if stages:
        return g
    for _ in g:
        pass


def _ln_gen(k, tag, z, g_rep, b_rep, out, scr):
    sc = k.sc
    st, mv, rs = scr["st"], scr["mv"], scr["rs"]
    for h in range(2):
        sc.op("dve", lambda e, h=h: e.bn_stats(st[:, h * 6:(h + 1) * 6], z[:, h * 512:(h + 1) * 512]),
              R=[tag + "z"], W=[tag + f"st{h}"])
    sc.op("dve", lambda e: e.bn_aggr(mv[:, 0:2], st[:, 0:12]), R=[tag + "st0", tag + "st1"], W=[tag + "mv"])
    sc.op("dve", lambda e: e.tensor_scalar_add(rs[:, 0:1], mv[:, 1:2], LN_EPS), R=[tag + "mv"], W=[tag + "rs0"])
    yield
    sc.op("pool", lambda e: e.tensor_tensor(rs[:, 1:2], rs[:, 0:1], k.neghalf[:, 0:1], ALU.pow),
          R=[tag + "rs0", "neghalf"], W=[tag + "rs"])
    yield
    sc.op("dve", lambda e: e.tensor_scalar(out[:, :], z[:, :], mv[:, 0:1], rs[:, 1:2], ALU.subtract, ALU.mult),
          R=[tag + "z", tag + "mv", tag + "rs"], W=[tag + "o"])
    sc.op("dve", lambda e: e.tensor_tensor(out[:, :], out[:, :], g_rep[:, :], ALU.mult),
          R=[tag + "o", "lnconst"], W=[tag + "o"])
    sc.op("dve", lambda e: e.tensor_tensor(out[:, :], out[:, :], b_rep[:, :], ALU.add),
          R=[tag + "o", "lnconst"], W=[tag + "o"])
    yield


def phase1(k):
    nc, sc = k.nc, k.sc
    with scope(k) as es:
        sb = lambda name, shape, dt=F32: es.enter_context(nc.sbuf_tensor(name, list(shape), dt))
        g_rep = sb("p1_g", [128, D])
        b_rep = sb("p1_b", [128, D])
        sc.dma("sp", lambda e: e.dma_start(out=g_rep[:, :], in_=bcast_rows(k.ln_in_g[0, :])), W=["lnconst"])
        sc.dma("sp", lambda e: e.dma_start(out=b_rep[:, :], in_=bcast_rows(k.ln_in_b[0, :])), W=["lnconst"])
        NBUF = 3
        xt = [sb(f"p1_x{i}", [128, D]) for i in range(NBUF)]
        ht = [sb(f"p1_h{i}", [128, D]) for i in range(NBUF)]
        hb = [sb(f"p1_hb{i}", [128, D], BF16) for i in range(NBUF)]
        hT = [sb(f"p1_hT{i}", [128, D], BF16) for i in range(NBUF)]
        scr = [dict(st=sb(f"p1_st{i}", [128, 12]), mv=sb(f"p1_mv{i}", [128, 2]), rs=sb(f"p1_rs{i}", [128, 2]))
               for i in range(NBUF)]
        def tile(t):
            i = t % NBUF
            tag = f"p1_{i}_"
            sc.dma("sp", lambda e: e.dma_start(out=xt[i][:, :], in_=k.x[t * 128:(t + 1) * 128, :]), W=[tag + "z"])
            yield
            for _ in layer_norm_tile(k, tag, xt[i], g_rep, b_rep, ht[i], scr[i], stages=True):
                yield
            sc.dma("sp", lambda e: e.dma_start(out=k.h0_d[t * 128:(t + 1) * 128, :], in_=ht[i][:, :]),
                   R=[tag + "o"], W=[("dram", "h0", t)])
            sc.op("act", lambda e: e.copy(hb[i][:, :], ht[i][:, :]), R=[tag + "o"], W=[tag + "hb"])
            yield
            pst = psbf(k, t % 2)
            for c in range(8):
                sc.op("pe", lambda e, c=c: e.transpose(pst[:, c * 128:(c + 1) * 128], hb[i][:, c * 128:(c + 1) * 128], k.ident[:, :]),
                      R=[tag + "hb", "ident"], W=[f"ps{t % 2}"])
            yield
            sc.op("dve", lambda e: e.tensor_copy(hT[i][:, :], pst[:, :]), R=[f"ps{t % 2}"], W=[tag + "hT"])
            yield
            sc.dma("sp", lambda e: e.dma_start(out=k.h0T_d[t, :, :], in_=hT[i][:, :]), R=[tag + "hT"], W=[("dram", "h0T", t)])
        run_pipelined((tile(t) for t in range(NT)), depth=3, skew=3)


def mmacc(k, out, pairs, R, W):
    n = len(pairs)
    for i, (l, r) in enumerate(pairs):
        k.sc.op("pe", lambda e, l=l, r=r, i=i: e.matmul(out, l, r, start=(i == 0), stop=(i == n - 1)), R=R, W=W)


def attention(k, tag, units, nkt, qk_fn, v_fn, dv, scale, out_fn, aug, Rres, nh, qk_dup=0):
    nc, sc = k.nc, k.sc
    W = 512 * nh
    with scope(k) as es:
        sb = lambda name, shape, dt=F32: es.enter_context(nc.sbuf_tensor(name, list(shape), dt))
        pt = [sb(f"{tag}_pt{i}", [128, 1024], BF16) for i in range(2)]
        osb = [sb(f"{tag}_osb{i}", [128, W]) for i in range(2)]
        rec = [sb(f"{tag}_rec{i}", [128, W]) for i in range(2)]
        onb = [sb(f"{tag}_onb{i}", [128, W], BF16) for i in range(2)]
        den = [sb(f"{tag}_den{i}", [1, 512]) for i in range(2)]
        G = nkt // 2 if nh == 1 else nkt
        gs = [(ui, g) for ui in range(len(units)) for g in range(G)]
        M = dv + 1 if aug else dv
        slot_of = {}
        pending = []

        def kts(g):
            return (2 * g, 2 * g + 1) if nh == 1 else (g, g)

        def qk(idx):
            ui, g = gs[idx]
            u = units[ui]
            sl = idx % 2
            slot_of[idx] = sl
            for rep in range(1 + qk_dup):
                for i in range(2):
                    if rep == 1 and nh == 1 and i == 1:
                        continue
                    l, r = qk_fn(u, kts(g)[i], i)
                    sc.op("pe", lambda e, l=l, r=r, b=2 * sl + i: e.matmul(psb(k, b), l, r, start=True, stop=True),
                          R=Rres, W=[f"ps{2 * sl + i}"])
            sc.op("act", lambda e, sl=sl: e.activation(out=pt[sl][:, :], in_=k.pp[sl][:, :], func=AF.Exp, scale=scale),
                  R=[f"ps{2 * sl}", f"ps{2 * sl + 1}"], W=[f"{tag}_pt{sl}"])

        def accb(ui, i):
            return 4 + ui % 2 if nh == 1 else 4 + 2 * (ui % 2) + i

        def epi2(ui, I):
            u = units[ui]
            s2 = ui % 2
            sl = (I + 1) % 2
            for i in range(nh):
                b = 2 * sl + i
                if aug:
                    sc.op("pe", lambda e, b=b, i=i: e.matmul(psb(k, b)[0:dv, :], k.sel[0:M, 0:dv], osb[s2][0:M, i * 512:(i + 1) * 512], start=True, stop=True),
                          R=[f"{tag}_osb{s2}", "sel"], W=[f"ps{b}"])
                else:
                    sc.op("pe", lambda e, b=b: e.matmul(psb(k, b)[0:dv, :], k.ones_f[0:1, 0:dv], den[s2][0:1, :], start=True, stop=True),
                          R=[f"{tag}_den{s2}", "ones_f"], W=[f"ps{b}"])
            sc.op("dve", lambda e: e.reciprocal(rec[s2][0:dv, :], k.pp[sl][0:dv, 0:W]),
                  R=[f"ps{2 * sl}", f"ps{2 * sl + 1}"][:nh], W=[f"{tag}_rec{s2}"])
            sc.op("dve", lambda e: e.tensor_tensor(onb[s2][0:dv, :], osb[s2][0:dv, :], rec[s2][0:dv, :], ALU.mult),
                  R=[f"{tag}_osb{s2}", f"{tag}_rec{s2}"], W=[f"{tag}_onb{s2}"])
            src = onb[s2][0:dv, :] if nh == 1 else onb[s2][0:dv, :].rearrange("p (h t) -> p h t", h=2)
            sc.dma("sp", lambda e, src=src: e.dma_start(out=out_fn(u), in_=src),
                   R=[f"{tag}_onb{s2}"], W=[("dram", tag, u[0], u[1])])

        def pv(idx):
            ui, g = gs[idx]
            u = units[ui]
            sl = slot_of.pop(idx)
            s2 = ui % 2
            for i in range(2):
                kt = kts(g)[i]
                ob = accb(ui, i)
                first = (kt == 0) if nh == 2 else (kt == 0)
                last = (kt == nkt - 1)
                vv = v_fn(u, kt, i)
                sc.op("pe", lambda e, ob=ob, vv=vv, i=i, first=first, last=last: e.matmul(
                    psb(k, ob)[0:vv.shape[1], :], vv, pt[sl][:, i * 512:(i + 1) * 512], start=first, stop=last),
                    R=[f"{tag}_pt{sl}"] + Rres, W=[f"ps{ob}"])
                if not aug:
                    sc.op("pe", lambda e, kt=kt, i=i, first=first, last=last: e.matmul(
                        psb(k, 6 + ui % 2)[0:1, :], k.ones_bf[:, 0:1], pt[sl][:, i * 512:(i + 1) * 512], start=first, stop=last),
                        R=[f"{tag}_pt{sl}"], W=[f"ps{6 + ui % 2}"])
            if g == G - 1:
                if nh == 1:
                    sc.op("dve", lambda e: e.tensor_copy(osb[s2][0:M, :], psb(k, accb(ui, 0))[0:M, :]),
                          R=[f"ps{accb(ui, 0)}"], W=[f"{tag}_osb{s2}"])
                else:
                    sc.op("dve", lambda e: e.tensor_copy(osb[s2][0:M, :], k.pp[2 + ui % 2][0:M, :]),
                          R=[f"ps{accb(ui, 0)}", f"ps{accb(ui, 1)}"], W=[f"{tag}_osb{s2}"])
                if not aug:
                    sc.op("dve", lambda e: e.tensor_copy(den[s2][0:1, :], psb(k, 6 + ui % 2)[0:1, :]),
                          R=[f"ps{6 + ui % 2}"], W=[f"{tag}_den{s2}"])
                pending.append((idx + 2, ui))

        n = len(gs)
        for idx in range(n + 1):
            if idx < n:
                qk(idx)
            while pending and pending[0][0] <= idx:
                epi2(pending.pop(0)[1], idx)
            if idx >= 1:
                pv(idx - 1)
        while pending:
            epi2(pending.pop(0)[1], n)


def rms_rope_tm(k, tag, src, srcres, nh, gain_rep, r64, t, scr, out_bf):
    sc = k.sc
    w = nh * 64
    sq, ss, r, qn, t1, t2 = scr["sq"], scr["ss"], scr["r"], scr["qn"], scr["t1"], scr["t2"]
    v3 = lambda ap: ap.rearrange("p (h d) -> p h d", h=nh)
    v5 = lambda ap: ap.rearrange("p (h b two j) -> p h b two j", h=nh, b=2, two=2, j=16)
    Ct = r64[:, 0, t, :].unsqueeze(1).broadcast_to([128, nh, 64])
    St = r64[:, 1, t, :].rearrange("p (b two j) -> p b two j", b=2, two=2, j=16)
    sc.op("act", lambda e: e.activation(out=sq[:, 0:w], in_=src, func=AF.Square), R=[srcres], W=[tag + "sq"])
    yield
    sc.op("dve", lambda e: e.reduce_sum(ss[:, 0:nh], v3(sq[:, 0:w]), AX.X), R=[tag + "sq"], W=[tag + "ss"])
    sc.op("dve", lambda e: e.tensor_scalar(ss[:, 0:nh], ss[:, 0:nh], 1.0 / 64, RMS_EPS, ALU.mult, ALU.add),
          R=[tag + "ss"], W=[tag + "ss"])
    yield
    sc.op("pool", lambda e: e.tensor_tensor(r[:, 0:nh], ss[:, 0:nh], k.neghalf[:, 0:nh], ALU.pow),
          R=[tag + "ss", "neghalf"], W=[tag + "r"])
    yield
    sc.op("dve", lambda e: e.tensor_tensor(v3(qn[:, 0:w]), v3(src), r[:, 0:nh].unsqueeze(2).broadcast_to([128, nh, 64]), ALU.mult),
          R=[srcres, tag + "r"], W=[tag + "qn"])
    sc.op("dve", lambda e: e.tensor_tensor(qn[:, 0:w], qn[:, 0:w], gain_rep[:, 0:w], ALU.mult),
          R=[tag + "qn", "gains"], W=[tag + "qn"])
    sc.op("dve", lambda e: e.tensor_tensor(v3(t1[:, 0:w]), v3(qn[:, 0:w]), Ct, ALU.mult),
          R=[tag + "qn", "r64"], W=[tag + "t1"])
    for half in range(2):
        sc.op("dve", lambda e, half=half: e.tensor_tensor(
            v5(t2[:, 0:w])[:, :, :, half, :], v5(qn[:, 0:w])[:, :, :, 1 - half, :],
            St[:, :, half, :].unsqueeze(1).broadcast_to([128, nh, 2, 16]), ALU.mult),
            R=[tag + "qn", "r64"], W=[tag + f"t2{half}"])
    sc.op("dve", lambda e: e.tensor_tensor(out_bf, t1[:, 0:w], t2[:, 0:w], ALU.add),
          R=[tag + "t1", tag + "t20", tag + "t21"], W=[tag + "out"])
    yield


def phase2(k):
    nc, sc = k.nc, k.sc
    with scope(k) as es:
        sb = lambda name, shape, dt=F32: es.enter_context(nc.sbuf_tensor(name, list(shape), dt))
        qT = sb("a_qT", [128, 4, S], BF16)
        kd = sb("a_kd", [128, 2, S], BF16)
        va = sb("a_v", [128, NT, 2, 65], BF16)
        with scope(k) as es2:
            sb2 = lambda name, shape, dt=F32: es2.enter_context(nc.sbuf_tensor(name, list(shape), dt))
            wA = sb2("a_w", [128, 8, 768], BF16)
            sc.dma("pool", lambda e: e.dma_start(out=wA[:, :, :], in_=k.w_in[:, 0:768].rearrange("(c p) n -> p c n", p=128)),
                   W=["a_w"])
            gq = sb2("a_gq", [128, 512])
            gk = sb2("a_gk", [128, 128])
            r64 = sb2("a_r64", [128, 2, NT, 64])
            sc.dma("sp", lambda e: e.dma_start(out=gq[:, :], in_=bcast_rows(k.gqa_qn[0, :])), W=["gains"])
            sc.dma("sp", lambda e: e.dma_start(out=gk[:, :], in_=bcast_rows(k.gqa_kn[0, :])), W=["gains"])
            sc.dma("sp", lambda e: e.dma_start(out=r64[:, :, :, :], in_=k.c_rope64[:, :, :, :]), W=["r64"])
            sc.op("pool", lambda e: e.memset(va[:, :, :, 64:65], 1.0), W=["aV1"])
            hb = [sb2(f"a_hb{i}", [128, 4, D], BF16) for i in range(2)]
            mk = lambda i, w: dict(sq=sb2(f"a_sq{w}{i}", [128, w]), ss=sb2(f"a_ss{w}{i}", [128, 8]), r=sb2(f"a_r{w}{i}", [128, 8]),
                                   qn=sb2(f"a_qn{w}{i}", [128, w]), t1=sb2(f"a_t1{w}{i}", [128, w]), t2=sb2(f"a_t2{w}{i}", [128, w]))
            scq = [mk(i, 512) for i in range(2)]
            sck = [mk(i, 128) for i in range(2)]
            qr = [sb2(f"a_qr{i}", [128, 512], BF16) for i in range(2)]
            kr = [sb2(f"a_kr{i}", [128, 128], BF16) for i in range(2)]
            kdb = [sb2(f"a_kdb{i}", [128, 256], BF16) for i in range(2)]
            ptq = psbf(k, 4)
            ptk = psbf(k, 5)
            def load_hb(b):
                sc.dma("sp", lambda e: e.dma_start(out=hb[b % 2][:, :, :], in_=k.h0T_d[4 * b:4 * b + 4, :, :].rearrange("t p f -> p t f")),
                       R=[("dram", "h0T", 4 * b + j) for j in range(4)], W=[f"a_hb{b % 2}"])

            def tile(t):
                b, j = t // 4, t % 4
                hbb = hb[b % 2]
                if t == 0:
                    load_hb(0)
                if j == 0 and b + 1 < NB:
                    load_hb(b + 1)
                i2 = t % 2
                pa, pb = 2 * i2, 2 * i2 + 1
                mmacc(k, psb(k, pa), [(hbb[:, j, c * 128:(c + 1) * 128], wA[:, c, 0:512]) for c in range(8)],
                      R=[f"a_hb{b % 2}", "a_w"], W=[f"ps{pa}"])
                mmacc(k, psb(k, pb)[:, 0:256], [(hbb[:, j, c * 128:(c + 1) * 128], wA[:, c, 512:768]) for c in range(8)],
                      R=[f"a_hb{b % 2}", "a_w"], W=[f"ps{pb}"])
                yield
                tq, tk = f"a_q{i2}_", f"a_k{i2}_"
                gq_ = rms_rope_tm(k, tq, psb(k, pa), f"ps{pa}", 8, gq, r64, t, scq[i2], qr[i2][:, :])
                gk_ = rms_rope_tm(k, tk, psb(k, pb)[:, 0:128], f"ps{pb}", 2, gk, r64, t, sck[i2], kr[i2][:, :])
                sc.op("act", lambda e: e.copy(va[:, t, :, 0:64], psb(k, pb)[:, 128:256].rearrange("p (g d) -> p g d", g=2)),
                      R=[f"ps{pb}"], W=["aV"])
                for _ in zip(gq_, gk_):
                    yield
                for p in range(4):
                    sc.op("pe", lambda e, p=p: e.transpose(ptq[:, p * 128:(p + 1) * 128], qr[i2][:, p * 128:(p + 1) * 128], k.ident[:, :]),
                          R=[tq + "out", "ident"], W=["ps4"])
                kdv = kdb[i2][:, :].rearrange("p (g u d) -> p g u d", g=2, u=2, d=64)
                for u in range(2):
                    sc.op("act", lambda e, u=u: e.copy(kdv[:, :, u, :], kr[i2][:, :].rearrange("p (g d) -> p g d", g=2)),
                          R=[tk + "out"], W=[f"a_kdb{i2}_{u}"])
                yield
                sc.op("dve", lambda e: e.tensor_copy(qT[:, :, t * 128:(t + 1) * 128], ptq[:, 0:512].rearrange("p (c n) -> p c n", c=4)),
                      R=["ps4"], W=["aQ"])
                for g in range(2):
                    sc.op("pe", lambda e, g=g: e.transpose(ptk[:, g * 128:(g + 1) * 128], kdb[i2][:, g * 128:(g + 1) * 128], k.ident[:, :]),
                          R=[f"a_kdb{i2}_0", f"a_kdb{i2}_1", "ident"], W=["ps5"])
                yield
                sc.op("dve", lambda e: e.tensor_copy(kd[:, :, t * 128:(t + 1) * 128], ptk[:, 0:256].rearrange("p (c n) -> p c n", c=2)),
                      R=["ps5"], W=["aK"])
            run_pipelined((tile(t) for t in range(NT)), depth=2, skew=4)
        attention(k, "a", [(p, qb) for qb in range(NB) for p in range(4)], NT,
                  qk_fn=lambda u, kt, i: (kd[i * 64:i * 64 + 64, u[0] // 2, kt * 128:(kt + 1) * 128],
                                          qT[i * 64:i * 64 + 64, u[0], u[1] * 512:(u[1] + 1) * 512]),
                  v_fn=lambda u, kt, i: va[:, kt, u[0] // 2, :],
                  dv=64, scale=0.125,
                  out_fn=lambda u: k.oA_d[2 * u[0]:2 * u[0] + 2, :, u[1] * 512:(u[1] + 1) * 512].rearrange("h d t -> d h t"),
                  aug=True, Rres=["aQ", "aK", "aV", "aV1"], nh=2, qk_dup=1)


def phase3(k):
    nc, sc = k.nc, k.sc
    with scope(k) as es:
        sb = lambda name, shape, dt=F32: es.enter_context(nc.sbuf_tensor(name, list(shape), dt))
        qlT = sb("b_qlT", [128, 3, S], BF16)
        kvT = sb("b_kvT", [128, 2, S], BF16)
        kpe = sb("b_kpe", [128, S], BF16)
        rT = [sb(f"b_rT{i}", [128, 2, 512]) for i in range(2)]
        ta = [sb(f"b_ta{i}", [128, 512]) for i in range(2)]
        tb = [sb(f"b_tb{i}", [128, 512]) for i in range(2)]
        with scope(k) as es2:
            sb2 = lambda name, shape, dt=F32: es2.enter_context(nc.sbuf_tensor(name, list(shape), dt))
            wB = sb2("b_w", [128, 8, 672], BF16)
            sc.dma("pool", lambda e: e.dma_start(out=wB[:, :, :], in_=k.w_in[:, 768:1440].rearrange("(c p) n -> p c n", p=128)), W=["b_w"])
            wks = sb2("b_wks", [128, 8, 96], BF16)
            sc.op("pool", lambda e: e.memset(wks[:, :, 0:64], 0.0), W=["b_wks0"])
            sc.dma("pool", lambda e: e.dma_start(out=wks[:, :, 64:96], in_=k.w_kr_sw[:, :].rearrange("(c p) n -> p c n", p=128)), W=["b_wks1"])
            gql = sb2("b_gql", [128, 384])
            gkv = sb2("b_gkv", [128, 256])
            sc.dma("sp", lambda e: e.dma_start(out=gql[:, :], in_=bcast_rows(k.mla_qn[0, :])), W=["gains"])
            sc.dma("sp", lambda e: e.dma_start(out=gkv[:, :], in_=bcast_rows(k.mla_kvn[0, :])), W=["gains"])
            hb = [sb2(f"b_hb{i}", [128, 4, D], BF16) for i in range(2)]
            sq = [sb2(f"b_sq{i}", [128, 512]) for i in range(2)]
            st = [sb2(f"b_st{i}", [128, 8]) for i in range(2)]
            qn = [sb2(f"b_qn{i}", [128, 384]) for i in range(2)]
            kn = [sb2(f"b_kn{i}", [128, 256]) for i in range(2)]
            qlb = [sb2(f"b_qlb{i}", [128, 384], BF16) for i in range(2)]
            kvb = [sb2(f"b_kvb{i}", [128, 256], BF16) for i in range(2)]
            ptq = psbf(k, 4)
            ptk = psbf(k, 5)
            for b in range(NB):
                hbb = hb[b % 2]
                blk = slice(b * 512, (b + 1) * 512)
                sc.dma("sp", lambda e, b=b, hbb=hbb: e.dma_start(out=hbb[:, :, :], in_=k.h0T_d[4 * b:4 * b + 4, :, :].rearrange("t p f -> p t f")),
                       R=[("dram", "h0T", 4 * b + j) for j in range(4)], W=[f"b_hb{b % 2}"])
                rTb = rT[b % 2]
                sc.dma("sp", lambda e, b=b, rTb=rTb: e.dma_start(out=rTb[:, :, :], in_=k.c_ropeT[:, :, b * 512:(b + 1) * 512]), W=[f"b_rT{b % 2}"])
                for j in range(4):
                    t = 4 * b + j
                    i2 = t % 2
                    pa, pb = 2 * i2, 2 * i2 + 1
                    tg = f"b1_{i2}_"
                    mmacc(k, psb(k, pa), [(hbb[:, j, c * 128:(c + 1) * 128], wB[:, c, 0:512]) for c in range(8)],
                          R=[f"b_hb{b % 2}", "b_w"], W=[f"ps{pa}"])
                    mmacc(k, psb(k, pb)[:, 0:128], [(hbb[:, j, c * 128:(c + 1) * 128], wB[:, c, 512:640]) for c in range(8)],
                          R=[f"b_hb{b % 2}", "b_w"], W=[f"ps{pb}"])
                    s_, st_ = sq[i2], st[i2]
                    sc.op("act", lambda e, s_=s_, st_=st_, pa=pa: e.activation(out=s_[:, 0:384], in_=psb(k, pa)[:, 0:384], func=AF.Square, accum_out=st_[:, 0:1]),
                          R=[f"ps{pa}"], W=[tg + "sq", tg + "s0"])
                    sc.op("act", lambda e, s_=s_, st_=st_, pa=pa: e.activation(out=s_[:, 384:512], in_=psb(k, pa)[:, 384:512], func=AF.Square, accum_out=st_[:, 1:2]),
                          R=[f"ps{pa}"], W=[tg + "sq2", tg + "s1"])
                    sc.op("act", lambda e, s_=s_, st_=st_, pb=pb: e.activation(out=s_[:, 0:128], in_=psb(k, pb)[:, 0:128], func=AF.Square, accum_out=st_[:, 2:3]),
                          R=[f"ps{pb}", tg + "sq"], W=[tg + "sq", tg + "s2"])
                    sc.op("dve", lambda e, st_=st_: e.tensor_scalar(st_[:, 3:4], st_[:, 0:1], 1.0 / 384, RMS_EPS, ALU.mult, ALU.add),
                          R=[tg + "s0"], W=[tg + "s3"])
                    sc.op("dve", lambda e, st_=st_: e.tensor_tensor(st_[:, 4:5], st_[:, 1:2], st_[:, 2:3], ALU.add),
                          R=[tg + "s1", tg + "s2"], W=[tg + "s4"])
                    sc.op("dve", lambda e, st_=st_: e.tensor_scalar(st_[:, 4:5], st_[:, 4:5], 1.0 / 256, RMS_EPS, ALU.mult, ALU.add),
                          R=[tg + "s4"], W=[tg + "s4"])
                    sc.op("pool", lambda e, st_=st_: e.tensor_tensor(st_[:, 5:7], st_[:, 3:5], k.neghalf[:, 0:2], ALU.pow),
                          R=[tg + "s3", tg + "s4", "neghalf"], W=[tg + "r"])
                    sc.op("dve", lambda e, i2=i2, st_=st_, pa=pa: e.tensor_scalar(qn[i2][:, :], psb(k, pa)[:, 0:384], st_[:, 5:6], None, ALU.mult),
                          R=[f"ps{pa}", tg + "r"], W=[tg + "qn"])
                    sc.op("pool", lambda e, i2=i2: e.tensor_tensor(qlb[i2][:, :], qn[i2][:, :], gql[:, :], ALU.mult),
                          R=[tg + "qn", "gains"], W=[tg + "qlb"])
                    sc.op("dve", lambda e, i2=i2, st_=st_, pa=pa: e.tensor_scalar(kn[i2][:, 0:128], psb(k, pa)[:, 384:512], st_[:, 6:7], None, ALU.mult),
                          R=[f"ps{pa}", tg + "r"], W=[tg + "kn0"])
                    sc.op("dve", lambda e, i2=i2, st_=st_, pb=pb: e.tensor_scalar(kn[i2][:, 128:256], psb(k, pb)[:, 0:128], st_[:, 6:7], None, ALU.mult),
                          R=[f"ps{pb}", tg + "r"], W=[tg + "kn1"])
                    sc.op("pool", lambda e, i2=i2: e.tensor_tensor(kvb[i2][:, :], kn[i2][:, :], gkv[:, :], ALU.mult),
                          R=[tg + "kn0", tg + "kn1", "gains"], W=[tg + "kvb"])
                    for c in range(3):
                        sc.op("pe", lambda e, c=c, i2=i2: e.transpose(ptq[:, c * 128:(c + 1) * 128], qlb[i2][:, c * 128:(c + 1) * 128], k.ident[:, :]),
                              R=[tg + "qlb", "ident"], W=["ps4"])
                    sc.op("dve", lambda e, t=t: e.tensor_copy(qlT[:, :, t * 128:(t + 1) * 128], ptq[:, 0:384].rearrange("p (c n) -> p c n", c=3)),
                          R=["ps4"], W=["b_qlT"])
                    for c in range(2):
                        sc.op("pe", lambda e, c=c, i2=i2: e.transpose(ptk[:, c * 128:(c + 1) * 128], kvb[i2][:, c * 128:(c + 1) * 128], k.ident[:, :]),
                              R=[tg + "kvb", "ident"], W=["ps5"])
                    sc.op("act", lambda e, t=t: e.copy(kvT[:, :, t * 128:(t + 1) * 128], ptk[:, 0:256].rearrange("p (c n) -> p c n", c=2)),
                          R=["ps5"], W=["b_kvT"])
                mmacc(k, psb(k, 6)[0:96, :], [(wB[:, c, 576:672], hbb[:, :, c * 128:(c + 1) * 128]) for c in range(8)],
                      R=[f"b_hb{b % 2}", "b_w"], W=["ps6"])
                mmacc(k, psb(k, 7)[0:96, :], [(wks[:, c, 0:96], hbb[:, :, c * 128:(c + 1) * 128]) for c in range(8)],
                      R=[f"b_hb{b % 2}", "b_wks0", "b_wks1"], W=["ps7"])
                a_, b_ = ta[b % 2], tb[b % 2]
                sc.op("dve", lambda e, a_=a_, rTb=rTb: e.tensor_tensor(a_[64:96, :], psb(k, 6)[64:96, :], rTb[64:96, 0, :], ALU.mult),
                      R=["ps6", f"b_rT{b % 2}"], W=[f"b_ta{b % 2}"])
                sc.op("dve", lambda e, b_=b_, rTb=rTb: e.tensor_tensor(b_[64:96, :], psb(k, 7)[64:96, :], rTb[64:96, 1, :], ALU.mult),
                      R=["ps7", f"b_rT{b % 2}"], W=[f"b_tb{b % 2}"])
                sc.op("pool", lambda e, a_=a_, b_=b_, blk=blk: e.tensor_tensor(kpe[64:96, blk], a_[64:96, :], b_[64:96, :], ALU.add),
                      R=[f"b_ta{b % 2}", f"b_tb{b % 2}"], W=["b_kpe"])
        wqb = sb("b_wqb", [128, 3, 768], BF16)
        wqs = sb("b_wqs", [128, 3, 768], BF16)
        wkv = sb("b_wkv", [128, 2, 1024], BF16)
        sc.dma("pool", lambda e: e.dma_start(out=wqb[:, :, :], in_=k.w_qb[:, :].rearrange("(c p) n -> p c n", p=128)), W=["b_wqb"])
        sc.dma("pool", lambda e: e.dma_start(out=wqs[:, :, :], in_=k.w_qb_sw[:, :].rearrange("(c p) n -> p c n", p=128)), W=["b_wqs"])
        sc.dma("pool", lambda e: e.dma_start(out=wkv[:, :, :], in_=k.w_kvb[:, :].rearrange("(c p) n -> p c n", p=128)), W=["b_wkv"])
        qT = sb("b_qT", [128, 4, S], BF16)
        kT = sb("b_kT", [128, 4, S], BF16)
        vb = sb("b_v", [128, NT, 4, 65], BF16)
        sc.op("pool", lambda e: e.memset(vb[:, :, :, 64:65], 1.0), W=["bV1"])
        ta4 = [sb(f"b_ta4{i}", [128, 512]) for i in range(4)]
        tb4 = [sb(f"b_tb4{i}", [128, 512]) for i in range(4)]
        for half in range(2):
            def blockgen(b, half=half):
                blk = slice(b * 512, (b + 1) * 512)
                rTb = rT[b % 2]
                sc.dma("sp", lambda e: e.dma_start(out=rTb[:, :, :], in_=k.c_ropeT[:, :, b * 512:(b + 1) * 512]), W=[f"b_rT{b % 2}"])
                yield
                for hl in range(4):
                    h = 4 * half + hl
                    pq, pswp, pn = hl % 2, 2 + hl % 2, 4 + hl % 2
                    x4 = (b % 2) * 2 + hl % 2
                    a_, b_ = ta4[x4], tb4[x4]
                    mmacc(k, psb(k, pq)[0:96, :], [(wqb[:, c, h * 96:(h + 1) * 96], qlT[:, c, blk]) for c in range(3)],
                          R=["b_wqb", "b_qlT"], W=[f"ps{pq}"])
                    mmacc(k, psb(k, pswp)[0:96, :], [(wqs[:, c, h * 96:(h + 1) * 96], qlT[:, c, blk]) for c in range(3)],
                          R=["b_wqs", "b_qlT"], W=[f"ps{pswp}"])
                    mmacc(k, psb(k, pn)[0:64, :], [(wkv[:, c, h * 128:h * 128 + 64], kvT[:, c, blk]) for c in range(2)],
                          R=["b_wkv", "b_kvT"], W=[f"ps{pn}"])
                    yield
                    sc.op("act", lambda e, hl=hl, pq=pq: e.copy(qT[0:64, hl, blk], psb(k, pq)[0:64, :]), R=[f"ps{pq}"], W=["bQ"])
                    sc.op("dve", lambda e, a_=a_, pq=pq: e.tensor_tensor(a_[64:96, :], psb(k, pq)[64:96, :], rTb[64:96, 0, :], ALU.mult),
                          R=[f"ps{pq}", f"b_rT{b % 2}"], W=[f"b_ta4{x4}"])
                    sc.op("dve", lambda e, b_=b_, pswp=pswp: e.tensor_tensor(b_[64:96, :], psb(k, pswp)[64:96, :], rTb[64:96, 1, :], ALU.mult),
                          R=[f"ps{pswp}", f"b_rT{b % 2}"], W=[f"b_tb4{x4}"])
                    sc.op("act", lambda e, hl=hl, pn=pn: e.copy(kT[0:64, hl, blk], psb(k, pn)[0:64, :]), R=[f"ps{pn}"], W=["bK"])
                    sc.op("act", lambda e, hl=hl: e.copy(kT[64:96, hl, blk], kpe[64:96, blk]), R=["b_kpe"], W=["bK"])
                    yield
                    sc.op("dve", lambda e, a_=a_, b_=b_, hl=hl: e.tensor_tensor(qT[64:96, hl, blk], a_[64:96, :], b_[64:96, :], ALU.add),
                          R=[f"b_ta4{x4}", f"b_tb4{x4}"], W=["bQ"])
                for j in range(4):
                    t = 4 * b + j
                    pv_ = 6 + t % 2
                    wv = [wkv[:, c, :].rearrange("p (h n) -> p h n", n=128)[:, 4 * half:4 * half + 4, 64:128] for c in range(2)]
                    mmacc(k, psb(k, pv_)[:, 0:256], [(kvT[:, c, t * 128:(t + 1) * 128], wv[c]) for c in range(2)],
                          R=["b_wkv", "b_kvT"], W=[f"ps{pv_}"])
                    yield
                    sc.op("dve", lambda e, t=t, pv_=pv_: e.tensor_copy(vb[:, t, :, 0:64], psb(k, pv_)[:, 0:256].rearrange("p (h d) -> p h d", h=4)),
                          R=[f"ps{pv_}"], W=["bV"])
            run_pipelined((blockgen(b) for b in range(NB)), depth=2, skew=5)
            attention(k, f"b{half}", [(hl, qb) for qb in range(NB) for hl in range(4)], NT,
                      qk_fn=lambda u, kt, i: (kT[0:96, u[0], kt * 128:(kt + 1) * 128], qT[0:96, u[0], u[1] * 512:(u[1] + 1) * 512]),
                      v_fn=lambda u, kt, i: vb[:, kt, u[0], :],
                      dv=64, scale=96.0 ** -0.5,
                      out_fn=lambda u, half=half: k.oB_d[4 * half + u[0], :, u[1] * 512:(u[1] + 1) * 512],
                      aug=True, Rres=["bQ", "bK", "bV", "bV1"], nh=1, qk_dup=1)


def phase4(k):
    nc, sc = k.nc, k.sc
    with scope(k) as es:
        sb = lambda name, shape, dt=F32: es.enter_context(nc.sbuf_tensor(name, list(shape), dt))
        qm = sb("c_q", [128, 4, S], BF16)
        mkT = sb("c_mk", [128, 4, MEM], BF16)
        mv = sb("c_mv", [128, 2, 4, 128], BF16)
        with scope(k) as es2:
            sb2 = lambda name, shape, dt=F32: es2.enter_context(nc.sbuf_tensor(name, list(shape), dt))
            wq = sb2("c_wq", [128, 8, 512], BF16)
            wm = sb2("c_wm", [128, 8, D], BF16)
            sc.dma("pool", lambda e: e.dma_start(out=wq[:, :, :], in_=k.w_in[:, 1440:1952].rearrange("(c p) n -> p c n", p=128)), W=["c_wq"])
            sc.dma("pool", lambda e: e.dma_start(out=wm[:, :, :], in_=k.w_mkv[:, :].rearrange("(c p) n -> p c n", p=128)), W=["c_wm"])
            memf = sb2("c_memf", [128, 2, D])
            memb = sb2("c_memb", [128, 2, D], BF16)
            memT = sb2("c_memT", [128, 8, MEM], BF16)
            sc.dma("sp", lambda e: e.dma_start(out=memf[:, :, :], in_=k.mem[:, :].rearrange("(t p) d -> p t d", p=128)), W=["c_memf"])
            sc.op("act", lambda e: e.copy(memb[:, :, :], memf[:, :, :]), R=["c_memf"], W=["c_memb"])
            pt4 = psbf(k, 4)
            pt5 = psbf(k, 5)
            for t in range(2):
                pt = pt4 if t == 0 else pt5
                for c in range(8):
                    sc.op("pe", lambda e, t=t, c=c, pt=pt: e.transpose(pt[:, c * 128:(c + 1) * 128], memb[:, t, c * 128:(c + 1) * 128], k.ident[:, :]),
                          R=["c_memb", "ident"], W=[f"ps{4 + t}"])
                sc.op("dve", lambda e, t=t, pt=pt: e.tensor_copy(memT[:, :, t * 128:(t + 1) * 128], pt[:, :].rearrange("p (c n) -> p c n", c=8)),
                      R=[f"ps{4 + t}"], W=["c_memT"])
            for h in range(4):
                pb_ = h % 2
                mmacc(k, psb(k, pb_)[:, 0:MEM], [(wm[:, c, h * 128:(h + 1) * 128], memT[:, c, :]) for c in range(8)],
                      R=["c_wm", "c_memT"], W=[f"ps{pb_}"])
                sc.op("act", lambda e, h=h, pb_=pb_: e.copy(mkT[:, h, :], psb(k, pb_)[:, 0:MEM]), R=[f"ps{pb_}"], W=["cK"])
            for t in range(2):
                mmacc(k, psb(k, 2 + t), [(memT[:, c, t * 128:(t + 1) * 128], wm[:, c, 512:1024]) for c in range(8)],
                      R=["c_wm", "c_memT"], W=[f"ps{2 + t}"])
                sc.op("dve", lambda e, t=t: e.tensor_copy(mv[:, t, :, :], psb(k, 2 + t).rearrange("p (h d) -> p h d", h=4)),
                      R=[f"ps{2 + t}"], W=["cV"])
            hb = [sb2(f"c_hb{i}", [128, 4, D], BF16) for i in range(2)]
            for b in range(NB):
                hbb = hb[b % 2]
                blk = slice(b * 512, (b + 1) * 512)
                sc.dma("sp", lambda e, b=b, hbb=hbb: e.dma_start(out=hbb[:, :, :], in_=k.h0T_d[4 * b:4 * b + 4, :, :].rearrange("t p f -> p t f")),
                       R=[("dram", "h0T", 4 * b + j) for j in range(4)], W=[f"c_hb{b % 2}"])
                for h in range(4):
                    pb_ = 4 + h % 2 if h < 2 else 6 + h % 2
                    mmacc(k, psb(k, pb_), [(wq[:, c, h * 128:(h + 1) * 128], hbb[:, :, c * 128:(c + 1) * 128]) for c in range(8)],
                          R=["c_wq", f"c_hb{b % 2}"], W=[f"ps{pb_}"])
                    eng = "act" if h % 2 == 0 else "dve"
                    if eng == "act":
                        sc.op("act", lambda e, h=h, blk=blk, pb_=pb_: e.copy(qm[:, h, blk], psb(k, pb_)), R=[f"ps{pb_}"], W=["cQ"])
                    else:
                        sc.op("dve", lambda e, h=h, blk=blk, pb_=pb_: e.tensor_copy(qm[:, h, blk], psb(k, pb_)), R=[f"ps{pb_}"], W=["cQ"])
        attention(k, "c", [(h, qb) for qb in range(NB) for h in range(4)], 2,
                  qk_fn=lambda u, kt, i: (mkT[:, u[0], kt * 128:(kt + 1) * 128], qm[:, u[0], u[1] * 512:(u[1] + 1) * 512]),
                  v_fn=lambda u, kt, i: mv[:, kt, u[0], :],
                  dv=128, scale=128.0 ** -0.5, out_fn=lambda u: k.oC_d[u[0], :, u[1] * 512:(u[1] + 1) * 512],
                  aug=False, Rres=["cQ", "cK", "cV"], nh=1)


def phase5(k):
    nc, sc = k.nc, k.sc
    BIG = float(1 << 22)
    with scope(k) as es:
        sb = lambda name, shape, dt=F32: es.enter_context(nc.sbuf_tensor(name, list(shape), dt))
        wg = sb("m_wg", [128, 8, 3 * D], BF16)
        for i in range(3):
            sc.dma("pool", lambda e, i=i: e.dma_start(out=wg[:, :, i * D:(i + 1) * D],
                                                     in_=k.w_in[:, OFF_G + i * D:OFF_G + (i + 1) * D].rearrange("(c p) n -> p c n", p=128)),
                   W=[f"m_wg{i}"])
        bg = sb("m_bg", [128, 3 * D])
        sc.dma("sp", lambda e: e.dma_start(out=bg[:, :], in_=bcast_rows(k.b_gate[0, :])), W=["m_bg"])
        wa = sb("m_wa", [64, 8, D], BF16)
        wb_ = sb("m_wb", [64, 8, D], BF16)
        wc = sb("m_wc", [128, 4, D], BF16)
        wo = sb("m_wo", [128, 8, D], BF16)
        sc.dma("pool", lambda e: e.dma_start(out=wa[:, :, :], in_=k.w_br_a[:, :].rearrange("(h p) n -> p h n", p=64)), W=["m_wa"])
        sc.dma("pool", lambda e: e.dma_start(out=wb_[:, :, :], in_=k.w_br_b[:, :].rearrange("(h p) n -> p h n", p=64)), W=["m_wb"])
        sc.dma("pool", lambda e: e.dma_start(out=wc[:, :, :], in_=k.w_br_c[:, :].rearrange("(h p) n -> p h n", p=128)), W=["m_wc"])
        sc.dma("pool", lambda e: e.dma_start(out=wo[:, :, :], in_=k.w_out[:, :].rearrange("(c p) n -> p c n", p=128)), W=["m_wo"])
        wr = sb("m_wr", [128, 8, NE])
        sc.dma("sp", lambda e: e.dma_start(out=wr[:, :, :], in_=k.w_router[:, :].rearrange("(c p) n -> p c n", p=128)), W=["m_wr"])
        brr = sb("m_br", [128, NE])
        sc.dma("sp", lambda e: e.dma_start(out=brr[:, :], in_=bcast_rows(k.b_router[0, :])), W=["m_br"])
        g1 = sb("m_g1", [128, D])
        b1 = sb("m_b1", [128, D])
        sc.dma("sp", lambda e: e.dma_start(out=g1[:, :], in_=bcast_rows(k.ln1_g[0, :])), W=["lnconst"])
        sc.dma("sp", lambda e: e.dma_start(out=b1[:, :], in_=bcast_rows(k.ln1_b[0, :])), W=["lnconst"])
        tri = sb("m_tri", [128, 128], BF16)
        ecap = sb("m_ecap", [128, NE])
        sc.dma("sp", lambda e: e.dma_start(out=tri[:, :], in_=k.c_tri[:, :]), W=["m_tri"])
        sc.dma("sp", lambda e: e.dma_start(out=ecap[:, :], in_=k.c_ecap[:, :]), W=["m_ecap"])
        base = sb("m_base", [128, NE])
        sc.op("pool", lambda e: e.memset(base[:, :], 0.0), W=["m_base"])
        NB2 = 2
        hT = [sb(f"m_hT{i}", [128, D], BF16) for i in range(NB2)]
        oa = [sb(f"m_oa{i}", [64, 8, 128], BF16) for i in range(NB2)]
        ob = [sb(f"m_ob{i}", [64, 8, 128], BF16) for i in range(NB2)]
        oc = [sb(f"m_oc{i}", [128, 4, 128], BF16) for i in range(NB2)]
        h0 = [sb(f"m_h0{i}", [128, D]) for i in range(NB2)]
        gs = [[sb(f"m_gs{i}_{g}", [128, 512]) for g in range(2)] for i in range(NB2)]
        mg = [sb(f"m_mg{i}", [128, D]) for i in range(NB2)]
        mgb = [sb(f"m_mgb{i}", [128, D], BF16) for i in range(NB2)]
        mT = [sb(f"m_mT{i}", [128, D], BF16) for i in range(NB2)]
        h1 = [sb(f"m_h1{i}", [128, D]) for i in range(NB2)]
        h1b = [sb(f"m_h1b{i}", [128, D], BF16) for i in range(NB2)]
        h1T = sb("m_h1T", [128, D])
        scr = [dict(st=sb(f"m_st{i}", [128, 12]), mv=sb(f"m_mv{i}", [128, 2]), rs=sb(f"m_rs{i}", [128, 2])) for i in range(NB2)]
        lg = sb("m_lg", [128, NE])
        v8 = sb("m_v8", [128, 8])
        sm = sb("m_sm", [128, 16])
        ex = sb("m_ex", [128, 4])
        oh = sb("m_oh", [128, 4, NE])
        prod = sb("m_prod", [128, 4, NE])
        mask = sb("m_mask", [128, NE])
        maskb = sb("m_maskb", [128, NE], BF16)
        rank = sb("m_rank", [128, NE])
        rk = sb("m_rk", [128, 4])
        ek = sb("m_ek", [128, 4])
        vl = sb("m_vl", [128, 4])
        sl = sb("m_sl", [128, 4])
        rtd = sb("m_rtd", [128, 8])
        brs = [(oa, wa, 8, 64, k.oA_d), (ob, wb_, 8, 64, k.oB_d), (oc, wc, 4, 128, k.oC_d)]

        def tile(t):
            i2 = t % NB2
            tk = slice(t * 128, (t + 1) * 128)
            T = f"m{i2}_"
            sc.dma("sp", lambda e: e.dma_start(out=hT[i2][:, :], in_=k.h0T_d[t, :, :]), R=[("dram", "h0T", t)], W=[T + "hT"])
            sc.dma("sp", lambda e: e.dma_start(out=h0[i2][:, :], in_=k.h0_d[tk, :]), R=[("dram", "h0", t)], W=[T + "h0"])
            for bi, (ot, _, nh, kr_, od) in enumerate(brs):
                tagn = "abc"[bi]
                rd = [r for r in sc.lastw if isinstance(r, tuple) and r[0] == "dram" and str(r[1]).startswith(tagn) and r[3] == t // 4]
                sc.dma("sp", lambda e, ot=ot, od=od: e.dma_start(out=ot[i2][:, :, :], in_=od[:, :, tk].rearrange("h d t -> d h t")),
                       R=rd, W=[T + "o" + tagn])
            yield
            pbase = 4 * i2
            for bi, (ot, wbr, nh, kr_, od) in enumerate(brs):
                tagn = "abc"[bi]
                for half in range(2):
                    gi = half
                    col = slice(bi * D + half * 512, bi * D + half * 512 + 512)
                    hs = slice(half * 512, (half + 1) * 512)
                    pg, pbr = pbase + gi, pbase + 2 + gi
                    g_ = gs[i2][gi]
                    mmacc(k, psb(k, pg), [(hT[i2][:, c * 128:(c + 1) * 128], wg[:, c, col]) for c in range(8)],
                          R=[T + "hT", f"m_wg{bi}"], W=[f"ps{pg}"])
                    mmacc(k, psb(k, pbr), [(ot[i2][0:kr_, h, :], wbr[0:kr_, h, hs]) for h in range(nh)],
                          R=[T + "o" + tagn, f"m_w{tagn}"], W=[f"ps{pbr}"])
                    sc.op("dve", lambda e, g_=g_, pg=pg, col=col: e.tensor_tensor(g_[:, :], psb(k, pg), bg[:, col], ALU.add),
                          R=[f"ps{pg}", "m_bg"], W=[T + f"gs{gi}"])
                    sc.op("act", lambda e, g_=g_: e.activation(out=g_[:, :], in_=g_[:, :], func=AF.Sigmoid),
                          R=[T + f"gs{gi}"], W=[T + f"gs{gi}"])
                    yield
                    if bi == 0:
                        sc.op("dve", lambda e, g_=g_, pbr=pbr, hs=hs: e.tensor_tensor(mg[i2][:, hs], g_[:, :], psb(k, pbr), ALU.mult),
                              R=[T + f"gs{gi}", f"ps{pbr}"], W=[T + f"mg{half}"])
                    else:
                        sc.op("dve", lambda e, g_=g_, pbr=pbr: e.tensor_tensor(g_[:, :], g_[:, :], psb(k, pbr), ALU.mult),
                              R=[T + f"gs{gi}", f"ps{pbr}"], W=[T + f"gs{gi}"])
                        sc.op("dve", lambda e, g_=g_, hs=hs: e.tensor_tensor(mg[i2][:, hs], mg[i2][:, hs], g_[:, :], ALU.add),
                              R=[T + f"gs{gi}", T + f"mg{half}"], W=[T + f"mg{half}"])
            yield
            sc.op("act", lambda e: e.copy(mgb[i2][:, :], mg[i2][:, :]), R=[T + "mg0", T + "mg1"], W=[T + "mgb"])
            yield
            ptb = psbf(k, pbase)
            for c in range(8):
                sc.op("pe", lambda e, c=c: e.transpose(ptb[:, c * 128:(c + 1) * 128], mgb[i2][:, c * 128:(c + 1) * 128], k.ident[:, :]),
                      R=[T + "mgb", "ident"], W=[f"ps{pbase}"])
            yield
            sc.op("dve", lambda e: e.tensor_copy(mT[i2][:, :], ptb[:, :]), R=[f"ps{pbase}"], W=[T + "mT"])
            yield
            for half in range(2):
                hs = slice(half * 512, (half + 1) * 512)
                pb_ = pbase + 1 + half
                mmacc(k, psb(k, pb_), [(mT[i2][:, c * 128:(c + 1) * 128], wo[:, c, hs]) for c in range(8)],
                      R=[T + "mT", "m_wo"], W=[f"ps{pb_}"])
            yield
            tag = T + "ln_"
            for half in range(2):
                hs = slice(half * 512, (half + 1) * 512)
                pb_ = pbase + 1 + half
                sc.op("dve", lambda e, hs=hs, pb_=pb_: e.scalar_tensor_tensor(mg[i2][:, hs], h0[i2][:, hs], ALPHA, psb(k, pb_), ALU.mult, ALU.add),
                      R=[f"ps{pb_}", T + "h0"], W=[tag + "z", T + f"mg{half}"])
            yield
            for _ in layer_norm_tile(k, tag, mg[i2], g1, b1, h1[i2], scr[i2], stages=True):
                yield
            sc._record(sc.lastw[tag + "o"], [T + "mg0", T + "mg1"], [])
            sc.dma("sp", lambda e: e.dma_start(out=k.h1_d[tk, :], in_=h1[i2][:, :]), R=[tag + "o"], W=[("dram", "h1", t)])
            sc.op("act", lambda e: e.copy(h1b[i2][:, :], h1[i2][:, :]), R=[tag + "o"], W=[T + "h1b"])
            for c in range(8):
                sc.op("pe", lambda e, c=c: e.transpose(k.pp[pbase // 2 + 1][:, c * 128:(c + 1) * 128], h1[i2][:, c * 128:(c + 1) * 128], k.identf[:, :]),
                      R=[tag + "o", "identf"], W=[f"ps{pbase + 2}", f"ps{pbase + 3}"])
            yield
            sc.op("dve", lambda e: e.tensor_copy(h1T[:, :], k.pp[pbase // 2 + 1][:, :]), R=[f"ps{pbase + 2}", f"ps{pbase + 3}"], W=["m_h1T"])
            yield
            pl_ = pbase
            mmacc(k, psb(k, pl_)[:, 0:NE], [(h1T[:, c * 128:(c + 1) * 128], wr[:, c, :]) for c in range(8)],
                  R=["m_h1T", "m_wr"], W=[f"ps{pl_}"])
            yield
            sc.op("dve", lambda e: e.tensor_tensor(lg[:, :], psb(k, pl_)[:, 0:NE], brr[:, :], ALU.add), R=[f"ps{pl_}", "m_br"], W=["m_lg"])
            sc.op("dve", lambda e: e.max(v8[:, :], lg[:, :]), R=["m_lg"], W=["m_v8"])
            sc.op("dve", lambda e: e.tensor_scalar_mul(sm[:, 0:1], v8[:, 0:1], -1.0), R=["m_v8"], W=["m_sm0"])
            for kk in range(4):
                sc.op("dve", lambda e, kk=kk: e.tensor_scalar(oh[:, kk, :], lg[:, :], v8[:, kk:kk + 1], None, ALU.is_equal),
                      R=["m_lg", "m_v8"], W=[f"m_oh{kk}"])
            ohr = [f"m_oh{kk}" for kk in range(4)]
            sc.op("act", lambda e: e.activation(out=ex[:, :], in_=v8[:, 0:4], func=AF.Exp, bias=sm[:, 0:1], accum_out=sm[:, 1:2]),
                  R=["m_v8", "m_sm0"], W=["m_ex", "m_sm1"])
            sc.op("dve", lambda e: e.tensor_tensor(mask[:, :], oh[:, 0, :], oh[:, 1, :], ALU.add), R=ohr, W=["m_mask"])
            sc.op("dve", lambda e: e.tensor_tensor(mask[:, :], mask[:, :], oh[:, 2, :], ALU.add), R=ohr + ["m_mask"], W=["m_mask"])
            sc.op("dve", lambda e: e.tensor_tensor(maskb[:, :], mask[:, :], oh[:, 3, :], ALU.add), R=ohr + ["m_mask"], W=["m_maskb"])
            yield
            sc.op("pe", lambda e: e.matmul(psb(k, pl_)[:, 64:64 + NE], tri[:, :], maskb[:, :], start=True, stop=True),
                  R=["m_maskb", "m_tri"], W=[f"ps{pl_}b"])
            sc.op("pe", lambda e: e.matmul(psb(k, pl_)[:, 128:128 + NE], k.ones_bf[:, :], maskb[:, :], start=True, stop=True),
                  R=["m_maskb", "ones_bf"], W=[f"ps{pl_}c"])
            yield
            sc.op("dve", lambda e: e.tensor_tensor(rank[:, :], psb(k, pl_)[:, 64:64 + NE], base[:, :], ALU.add), R=[f"ps{pl_}b", "m_base"], W=["m_rank"])
            sc.op("dve", lambda e: e.tensor_tensor(base[:, :], psb(k, pl_)[:, 128:128 + NE], base[:, :], ALU.add), R=[f"ps{pl_}c", "m_base", "m_rank"], W=["m_base"])
            sc._record(sc.lastw["m_base"], [f"ps{pl_}"], [])
            sc.op("dve", lambda e: e.tensor_tensor(prod[:, :, :], oh[:, :, :], rank[:, :].unsqueeze(1).broadcast_to([128, 4, NE]), ALU.mult),
                  R=ohr + ["m_rank"], W=["m_prod"])
            sc.op("dve", lambda e: e.reduce_sum(rk[:, :], prod[:, :, :], AX.X), R=["m_prod"], W=["m_rk"])
            sc.op("dve", lambda e: e.tensor_tensor(prod[:, :, :], oh[:, :, :], ecap[:, :].unsqueeze(1).broadcast_to([128, 4, NE]), ALU.mult),
                  R=ohr + ["m_ecap", "m_rk"], W=["m_prod"])
            sc.op("dve", lambda e: e.reduce_sum(ek[:, :], prod[:, :, :], AX.X), R=["m_prod"], W=["m_ek"])
            sc.op("dve", lambda e: e.tensor_single_scalar(vl[:, :], rk[:, :], float(CAP), ALU.is_lt), R=["m_rk"], W=["m_vl"])
            sc.op("dve", lambda e: e.tensor_tensor(sl[:, :], rk[:, :], ek[:, :], ALU.add), R=["m_rk", "m_ek"], W=["m_sl"])
            sc.op("dve", lambda e: e.tensor_scalar_add(sl[:, :], sl[:, :], -BIG), R=["m_sl"], W=["m_sl"])
            sc.op("dve", lambda e: e.tensor_tensor(sl[:, :], sl[:, :], vl[:, :], ALU.mult), R=["m_sl", "m_vl"], W=["m_sl"])
            sc.op("dve", lambda e: e.tensor_scalar_add(rtd[:, 0:4], sl[:, :], BIG), R=["m_sl"], W=["m_rtd0"])
            sc.op("dve", lambda e: e.tensor_copy(k.slots[:, t, :], rtd[:, 0:4]), R=["m_rtd0"], W=[("slots", t)])
            sc.op("dve", lambda e: e.reciprocal(sm[:, 2:3], sm[:, 1:2]), R=["m_sm1"], W=["m_sm2"])
            sc.op("dve", lambda e: e.tensor_scalar(ex[:, :], ex[:, :], sm[:, 2:3], None, ALU.mult), R=["m_ex", "m_sm2"], W=["m_ex"])
            sc.op("dve", lambda e: e.tensor_tensor(k.wts[:, t, :], ex[:, :], vl[:, :], ALU.mult), R=["m_ex", "m_vl"], W=[("wts", t)])
            yield
            if k.dbg:
                sc.op("dve", lambda e: e.tensor_copy(rtd[:, 4:8], k.wts[:, t, :]), R=[("wts", t)], W=["m_rtd1"])
                sc.dma("sp", lambda e: e.dma_start(out=k.rt_d[tk, :], in_=rtd[:, :]), R=["m_rtd0", "m_rtd1"], W=[("dram", "rt", t)])
            for kk in range(4):
                sc.dma("pool", lambda e, kk=kk: e.indirect_dma_start(
                    out=k.xs_d[:, :], out_offset=bass.IndirectOffsetOnAxis(ap=k.slots[:, t, kk:kk + 1], axis=0),
                    in_=h1b[i2][:, :], in_offset=None, bounds_check=sc.bc_reg, oob_is_err=False),
                    R=[("slots", t), T + "h1b"], W=[("dram", "xs", t, kk)])
        run_pipelined((tile(t) for t in range(NT)), depth=2, skew=8)


def phase6(k):
    nc, sc = k.nc, k.sc
    with scope(k) as es:
        sb = lambda name, shape, dt=F32: es.enter_context(nc.sbuf_tensor(name, list(shape), dt))
        w1 = [sb(f"e_w1{i}", [128, 8, 2 * D], BF16) for i in range(2)]
        w2 = [sb(f"e_w2{i}", [128, 8, D], BF16) for i in range(2)]
        bo = [sb(f"e_bo{i}", [128, D]) for i in range(2)]
        bi_ = sb("e_bi", [128, NE * 16])
        sc.dma("sp", lambda e: e.dma_start(out=bi_[:, :], in_=k.b_ein[:, :]), W=["e_bi"])
        bi1 = sb("e_bi1", [128, NE * 16])
        sc.op("dve", lambda e: e.tensor_scalar_add(bi1[:, :], bi_[:, :], 1.0), R=["e_bi"], W=["e_bi1"])
        CH = 3
        NCH = CAPT // CH
        N = CH * 128
        xs = [sb(f"e_xs{i}", [128, CH, D], BF16) for i in range(2)]
        xT = [sb(f"e_xT{i}", [128, 8, N], BF16) for i in range(2)]
        aT = [sb(f"e_aT{i}", [128, 8, N], BF16) for i in range(2)]
        t1 = [sb(f"e_t1{i}", [128, N]) for i in range(2)]
        t2 = [sb(f"e_t2{i}", [128, N]) for i in range(2)]
        sg = [sb(f"e_sg{i}", [128, N]) for i in range(2)]
        ys = [sb(f"e_ys{i}", [128, D]) for i in range(2)]
        pt = [psbf(k, 4), psbf(k, 5)]
        xs_reads = [r for r in sc.lastw if isinstance(r, tuple) and r[0] == "dram" and r[1] == "xs"]
        gchunk = 0

        def load_xs(g):
            s0_ = (g // NCH) * CAP + (g % NCH) * N
            sc.dma("sp", lambda e: e.dma_start(out=xs[g % 2][:, :, :], in_=k.xs_d[s0_:s0_ + N, :].rearrange("(t p) d -> p t d", p=128)),
                   R=xs_reads if g == 0 else [], W=[f"e_xs{g % 2}"])
        for e_ in range(NE):
            wi = e_ % 2
            for hf in range(2):
                sc.dma("pool", lambda e, e_=e_, wi=wi, hf=hf: e.dma_start(
                    out=w1[wi][:, :, hf * D:(hf + 1) * D], in_=k.w_ein[e_, :, hf * D:(hf + 1) * D].rearrange("(c p) n -> p c n", p=128)),
                    W=[f"e_w1{wi}_{hf}"])
            sc.dma("pool", lambda e, e_=e_, wi=wi: e.dma_start(out=w2[wi][:, :, :], in_=k.w_eout[e_, :, :].rearrange("(c p) n -> p c n", p=128)),
                   W=[f"e_w2{wi}"])
            sc.dma("pool", lambda e, e_=e_, wi=wi: e.dma_start(out=bo[wi][:, :], in_=bcast_rows(k.b_eout[e_, :])), W=[f"e_bo{wi}"])
            for ch in range(NCH):
                ci = gchunk % 2
                gchunk += 1
                s0 = e_ * CAP + ch * N
                if gchunk == 1:
                    load_xs(0)
                if gchunk < NE * NCH:
                    load_xs(gchunk)
                for st in range(CH):
                    for c in range(8):
                        sc.op("pe", lambda e, st=st, c=c, ci=ci: e.transpose(pt[st % 2][:, c * 128:(c + 1) * 128], xs[ci][:, st, c * 128:(c + 1) * 128], k.ident[:, :]),
                              R=[f"e_xs{ci}", "ident"], W=[f"ps{4 + st % 2}"])
                    sc.op("dve" if st % 2 == 0 else "act",
                          (lambda e, st=st, ci=ci: e.tensor_copy(xT[ci][:, :, st * 128:(st + 1) * 128], pt[st % 2][:, :].rearrange("p (c n) -> p c n", c=8)))
                          if st % 2 == 0 else
                          (lambda e, st=st, ci=ci: e.copy(xT[ci][:, :, st * 128:(st + 1) * 128], pt[st % 2][:, :].rearrange("p (c n) -> p c n", c=8))),
                          R=[f"ps{4 + st % 2}"], W=[f"e_xT{ci}_{st}"])
                xTr = [f"e_xT{ci}_{st}" for st in range(CH)]
                for j in range(8):
                    ji = j % 2
                    pg, pl = ji, 2 + ji
                    mmacc(k, psb(k, pg)[:, 0:N], [(w1[wi][:, c, j * 128:(j + 1) * 128], xT[ci][:, c, :]) for c in range(8)],
                          R=xTr + [f"e_w1{wi}_0"], W=[f"ps{pg}"])
                    mmacc(k, psb(k, pl)[:, 0:N], [(w1[wi][:, c, D + j * 128:D + (j + 1) * 128], xT[ci][:, c, :]) for c in range(8)],
                          R=xTr + [f"e_w1{wi}_1"], W=[f"ps{pl}"])
                    bg_ = bi_[:, e_ * 16 + j:e_ * 16 + j + 1]
                    bl_ = bi1[:, e_ * 16 + 8 + j:e_ * 16 + 8 + j + 1]
                    sc.op("dve", lambda e, ji=ji, pg=pg, bg_=bg_: e.tensor_scalar(t1[ji][:, :], psb(k, pg)[:, 0:N], bg_, 7.0, ALU.add, ALU.min),
                          R=[f"ps{pg}", "e_bi"], W=[f"e_t1{ji}"])
                    sc.op("act", lambda e, ji=ji: e.activation(out=sg[ji][:, :], in_=t1[ji][:, :], func=AF.Sigmoid, scale=1.702),
                          R=[f"e_t1{ji}"], W=[f"e_sg{ji}"])
                    sc.op("dve", lambda e, ji=ji, pl=pl, bl_=bl_: e.tensor_scalar(t2[ji][:, :], psb(k, pl)[:, 0:N], bl_, 8.0, ALU.add, ALU.min),
                          R=[f"ps{pl}", "e_bi1"], W=[f"e_t2{ji}"])
                    sc.op("dve", lambda e, ji=ji: e.tensor_tensor(t1[ji][:, :], t1[ji][:, :], sg[ji][:, :], ALU.mult),
                          R=[f"e_t1{ji}", f"e_sg{ji}"], W=[f"e_t1{ji}"])
                    sc.op("dve", lambda e, ji=ji, j=j, ci=ci: e.scalar_tensor_tensor(aT[ci][:, j, :], t2[ji][:, :], -6.0, t1[ji][:, :], ALU.max, ALU.mult),
                          R=[f"e_t1{ji}", f"e_t2{ji}"], W=[f"e_aT{ci}_{j}"])
                aTr = [f"e_aT{ci}_{j}" for j in range(8)]
                for st in range(CH):
                    yi = st % 2
                    for half in range(2):
                        pb_ = 6 + half
                        mmacc(k, psb(k, pb_), [(aT[ci][:, j, st * 128:(st + 1) * 128], w2[wi][:, j, half * 512:(half + 1) * 512]) for j in range(8)],
                              R=aTr + [f"e_w2{wi}"], W=[f"ps{pb_}"])
                        sc.op("dve", lambda e, yi=yi, half=half, pb_=pb_, wi=wi: e.tensor_tensor(
                            ys[yi][:, half * 512:(half + 1) * 512], psb(k, pb_), bo[wi][:, half * 512:(half + 1) * 512], ALU.add),
                            R=[f"ps{pb_}", f"e_bo{wi}"], W=[f"e_ys{yi}_{half}"])
                    r0 = s0 + st * 128
                    sc.dma("sp", lambda e, yi=yi, r0=r0: e.dma_start(out=k.ys_d[r0:r0 + 128, :], in_=ys[yi][:, :]),
                           R=[f"e_ys{yi}_0", f"e_ys{yi}_1"], W=[("dram", "ys", r0)])


def phase7(k):
    nc, sc = k.nc, k.sc
    with scope(k) as es:
        sb = lambda name, shape, dt=F32: es.enter_context(nc.sbuf_tensor(name, list(shape), dt))
        g2 = sb("f_g2", [128, D])
        b2 = sb("f_b2", [128, D])
        sc.dma("sp", lambda e: e.dma_start(out=g2[:, :], in_=bcast_rows(k.ln2_g[0, :])), W=["lnconst"])
        sc.dma("sp", lambda e: e.dma_start(out=b2[:, :], in_=bcast_rows(k.ln2_b[0, :])), W=["lnconst"])
        NB2 = 3
        h1 = [sb(f"f_h1{i}", [128, D]) for i in range(NB2)]
        yk = [[sb(f"f_yk{i}_{kk}", [128, D]) for kk in range(4)] for i in range(NB2)]
        acc = [sb(f"f_acc{i}", [128, D]) for i in range(NB2)]
        out = [sb(f"f_out{i}", [128, D]) for i in range(NB2)]
        scr = [dict(st=sb(f"f_st{i}", [128, 12]), mv=sb(f"f_mv{i}", [128, 2]), rs=sb(f"f_rs{i}", [128, 2])) for i in range(NB2)]
        ys_w = [r for r in sc.lastw if isinstance(r, tuple) and r[0] == "dram" and r[1] == "ys"]
        for i in range(NB2):
            for kk in range(4):
                sc.op("pool", lambda e, i=i, kk=kk: e.memset(yk[i][kk][:, :], 0.0), W=[f"f_yk{i}_{kk}"])
        NB2 = 3

        def tile(t):
            i2 = t % NB2
            tk = slice(t * 128, (t + 1) * 128)
            tag = f"f_{i2}_"
            sc.dma("sp", lambda e: e.dma_start(out=h1[i2][:, :], in_=k.h1_d[tk, :]), R=[("dram", "h1", t)], W=[f"f_h1{i2}"])
            for kk in range(4):
                sc.dma("pool", lambda e, kk=kk: e.indirect_dma_start(
                    out=yk[i2][kk][:, :], out_offset=None, in_=k.ys_d[:, :],
                    in_offset=bass.IndirectOffsetOnAxis(ap=k.slots[:, t, kk:kk + 1], axis=0),
                    bounds_check=sc.bc_reg, oob_is_err=False),
                    R=(ys_w if t == 0 else []) + [("slots", t)], W=[f"f_yk{i2}_{kk}"])
            yield
            sc.op("dve", lambda e: e.scalar_tensor_tensor(acc[i2][:, :], yk[i2][0][:, :], k.wts[:, t, 0:1], h1[i2][:, :], ALU.mult, ALU.add),
                  R=[f"f_yk{i2}_0", ("wts", t), f"f_h1{i2}"], W=[tag + "z"])
            sc.op("dve", lambda e: e.scalar_tensor_tensor(acc[i2][:, :], h1[i2][:, :], ALPHA - 1.0, acc[i2][:, :], ALU.mult, ALU.add),
                  R=[tag + "z", f"f_h1{i2}"], W=[tag + "z"])
            for kk in range(1, 4):
                sc.op("dve", lambda e, kk=kk: e.scalar_tensor_tensor(acc[i2][:, :], yk[i2][kk][:, :], k.wts[:, t, kk:kk + 1], acc[i2][:, :], ALU.mult, ALU.add),
                      R=[f"f_yk{i2}_{kk}", ("wts", t), tag + "z"], W=[tag + "z"])
            yield
            for _ in layer_norm_tile(k, tag, acc[i2], g2, b2, out[i2], scr[i2], stages=True):
                yield
            sc.dma("sp", lambda e: e.dma_start(out=k.y[tk, :], in_=out[i2][:, :]), R=[tag + "o"], W=[("y", t)])
        run_pipelined((tile(t) for t in range(NT)), depth=3, skew=2)


def host_consts():
    c = {}
    c["c_ident"] = np.eye(128, dtype=np.float32).astype(ml_dtypes.bfloat16)
    c["c_identf"] = np.eye(128, dtype=np.float32)
    t = np.arange(S)
    row, col = t // 64, t % 64

    def tab(half):
        inv = (10000.0 ** (-np.arange(half, dtype=np.float32) / half)).astype(np.float32)
        ar = row.astype(np.float32)[:, None] * inv[None, :]
        ac = col.astype(np.float32)[:, None] * inv[None, :]
        cs = np.concatenate([np.cos(ar), np.cos(ar), np.cos(ac), np.cos(ac)], axis=1)
        sn = np.concatenate([-np.sin(ar), np.sin(ar), -np.sin(ac), np.sin(ac)], axis=1)
        return cs.astype(np.float32), sn.astype(np.float32)
    c64, s64 = tab(16)
    r64 = np.stack([c64, s64], 0).reshape(2, NT, 128, 64).transpose(2, 0, 1, 3)
    c["c_rope64"] = np.ascontiguousarray(r64)
    c32, s32 = tab(8)
    rT = np.stack([c32.T, s32.T], 0)
    rT = np.tile(rT[None], (4, 1, 1, 1)).transpose(0, 2, 1, 3).reshape(128, 2, S)
    c["c_ropeT"] = np.ascontiguousarray(rT)
    sel = np.zeros((128, 128), np.float32)
    sel[64, :] = 1.0
    c["c_sel"] = sel
    c["c_tri"] = np.triu(np.ones((128, 128), np.float32), 1).astype(ml_dtypes.bfloat16)
    c["c_ecap"] = np.tile((np.arange(NE, dtype=np.float32) * CAP)[None, :], (128, 1))
    return c


def swap_perm(n_heads, hd, rope_off, rope_dim):
    perm = np.arange(n_heads * hd)
    q = rope_dim // 4
    for h in range(n_heads):
        b = h * hd + rope_off
        for blk in range(2):
            o = b + blk * 2 * q
            perm[o:o + q] = np.arange(o + q, o + 2 * q)
            perm[o + q:o + 2 * q] = np.arange(o, o + q)
    return perm


_CACHE = {}


def kernel(**inputs):
    upto = inputs.pop("_upto", 99)
    dbg = tuple(inputs.pop("_dbg", ()))
    ncores = inputs.pop("_ncores", 8)
    key = (upto, dbg)
    if key not in _CACHE:
        _CACHE[key] = build(upto, dbg)
    nc, kk = _CACHE[key]
    f = lambda a: np.ascontiguousarray(np.asarray(a, dtype=np.float32))
    shared = {}
    shared["ln_in_g"] = f(inputs["ln_in_g"]).reshape(1, D)
    shared["ln_in_b"] = f(inputs["ln_in_b"]).reshape(1, D)
    shared["w_in_proj"] = f(inputs["w_in_proj"][0])
    shared["w_kr_sw"] = np.ascontiguousarray(shared["w_in_proj"][:, OFF_KR:OFF_KR + 32][:, swap_perm(1, 32, 0, 32)])
    shared["b_gate"] = f(inputs["b_gate"][0]).reshape(1, 3 * D)
    shared["gqa_q_norm"] = np.tile(f(inputs["gqa_q_norm"][0]), 8).reshape(1, 512)
    shared["gqa_k_norm"] = np.tile(f(inputs["gqa_k_norm"][0]), 2).reshape(1, 128)
    shared["mla_q_norm"] = f(inputs["mla_q_norm"][0]).reshape(1, 384)
    shared["mla_kv_norm"] = f(inputs["mla_kv_norm"][0]).reshape(1, 256)
    wqb = f(inputs["w_mla_qb"][0])
    shared["w_mla_qb"] = wqb
    shared["w_mla_qb_sw"] = np.ascontiguousarray(wqb[:, swap_perm(8, 96, 64, 32)])
    shared["w_mla_kvb"] = f(inputs["w_mla_kvb"][0])
    shared["w_mem_kv"] = f(inputs["w_mem_kv"][0])
    shared["w_br_gqa"] = f(inputs["w_br_gqa"][0])
    shared["w_br_mla"] = f(inputs["w_br_mla"][0])
    shared["w_br_mem"] = f(inputs["w_br_mem"][0])
    shared["w_out"] = f(inputs["w_out"][0])
    shared["ln1_g"] = f(inputs["ln1_g"][0]).reshape(1, D)
    shared["ln1_b"] = f(inputs["ln1_b"][0]).reshape(1, D)
    shared["w_router"] = f(inputs["w_router"][0])
    shared["b_router"] = f(inputs["b_router"][0]).reshape(1, NE)
    shared["w_exp_in"] = f(inputs["w_exp_in"][0])
    shared["b_exp_in"] = np.ascontiguousarray(
        f(inputs["b_exp_in"][0]).reshape(NE, 16, 128).transpose(2, 0, 1).reshape(128, NE * 16))
    shared["w_exp_out"] = f(inputs["w_exp_out"][0])
    shared["b_exp_out"] = f(inputs["b_exp_out"][0])
    shared["ln2_g"] = f(inputs["ln2_g"][0]).reshape(1, D)
    shared["ln2_b"] = f(inputs["ln2_b"][0]).reshape(1, D)
    shared.update(host_consts())
    x = f(inputs["x"])
    mem = f(inputs["mem"])
    in_maps = []
    for c in range(ncores):
        m = {n: shared[n] for n in kk.inputs if n in shared}
        m["x"] = x[c]
        if "mem" in kk.inputs:
            m["mem"] = mem[c]
        in_maps.append(m)
    res = run_bass_kernel_spmd(nc, in_maps, core_ids=list(range(ncores)))
    if dbg:
        return res.results
    return np.stack([r["y"] for r in res.results], axis=0)
```

```python
import numpy as np
import ml_dtypes
from contextlib import ExitStack, contextmanager

import concourse.bass as bass
import concourse.mybir as mybir
from concourse.bass_utils import run_bass_kernel_spmd

F32 = mybir.dt.float32
BF16 = mybir.dt.bfloat16
I32 = mybir.dt.int32
U32 = mybir.dt.uint32
ALU = mybir.AluOpType
AF = mybir.ActivationFunctionType
AX = mybir.AxisListType

S = 4096
D = 1024
NT = S // 128
NB = S // 512
MEM = 256
NE = 32
TOPK = 4
CAPT = 6
CAP = CAPT * 128
NSLOT = NE * CAP
ALPHA = 2.0 ** 0.25
RMS_EPS = 1e-6
LN_EPS = 1e-5
OFF_QA, OFF_KA, OFF_VA, OFF_QL, OFF_KVL, OFF_KR, OFF_QM, OFF_G = 0, 512, 640, 768, 1152, 1408, 1440, 1952
INW = 5024

ENGS = ("pe", "act", "dve", "pool", "sp")


class Sched:
    EPOCH = 30000

    def __init__(self, nc, es, ndma=None):
        self.nc, self.es = nc, es
        self.prog = {e: [] for e in ENGS}
        self.esem = {e: es.enter_context(nc.semaphore(f"e_{e}_0")) for e in ENGS}
        self.eold = {e: set() for e in ENGS}
        self.nep = {e: 0 for e in ENGS}
        self.ecnt = {e: 0 for e in ENGS}
        self.seen = {e: {} for e in ENGS}
        self.lastw, self.readers = {}, {}
        ndma = ndma or {"sp": 28, "pool": 24, "act": 6}
        self.dsem = {q: [es.enter_context(nc.semaphore(f"d_{q}_{i}")) for i in range(n)] for q, n in ndma.items()}
        self.duse = {q: [0] * n for q, n in ndma.items()}
        self.dnext = {q: 0 for q in ndma}
        self.ninstr = 0

    def _deps(self, e, R, W):
        deps = {}

        def add(tok):
            if tok is None:
                return
            s, v = tok
            if deps.get(id(s), (None, 0))[1] < v:
                deps[id(s)] = (s, v)
        for r in R:
            add(self.lastw.get(r))
        for w in W:
            add(self.lastw.get(w))
            for tok in self.readers.get(w, {}).values():
                add(tok)
        waits = []
        for k, (s, v) in deps.items():
            if id(s) in self.eold[e]:
                continue
            if s is self.esem[e] and e == "pe":
                continue
            if self.seen[e].get(k, 0) >= v:
                continue
            self.seen[e][k] = v
            waits.append((s, v))
        return waits

    def _record(self, tok, R, W):
        for w in W:
            self.lastw[w] = tok
            self.readers[w] = {}
        for r in R:
            d = self.readers.setdefault(r, {})
            if d.get(id(tok[0]), (None, 0))[1] < tok[1]:
                d[id(tok[0])] = tok

    def op(self, e, fn, R=(), W=()):
        waits = self._deps(e, R, W)
        if self.ecnt[e] >= self.EPOCH:
            self.eold[e].add(id(self.esem[e]))
            self.nep[e] += 1
            self.esem[e] = self.es.enter_context(self.nc.semaphore(f"e_{e}_{self.nep[e]}"))
            self.ecnt[e] = 0
        self.ecnt[e] += 1
        sem = self.esem[e]
        tok = (sem, self.ecnt[e])

        def emit(eng, waits=waits, fn=fn, sem=sem):
            for s, v in waits:
                eng.wait_ge(s, v)
            fn(eng).then_inc(sem, 1)
        self.prog[e].append(emit)
        self._record(tok, R, W)
        self.ninstr += 1 + len(waits)
        return tok

    def dma(self, q, fn, R=(), W=()):
        waits = self._deps(q, R, W)
        i = self.dnext[q]
        self.dnext[q] = (i + 1) % len(self.dsem[q])
        sem = self.dsem[q][i]
        prev = self.duse[q][i]
        if prev and self.seen[q].get(id(sem), 0) < 16 * prev:
            self.seen[q][id(sem)] = 16 * prev
            waits.append((sem, 16 * prev))
        self.duse[q][i] = prev + 1
        tok = (sem, 16 * (prev + 1))

        def emit(eng, waits=waits, fn=fn, sem=sem):
            for s, v in waits:
                eng.wait_ge(s, v)
            fn(eng).then_inc(sem, 16)
        self.prog[q].append(emit)
        self._record(tok, R, W)
        self.ninstr += 1 + len(waits)
        return tok

    def barrier(self):
        toks = [(self.esem[e], self.ecnt[e]) for e in ENGS if self.ecnt[e] > 0]
        for q in self.dsem:
            for i, s in enumerate(self.dsem[q]):
                if self.duse[q][i]:
                    toks.append((s, 16 * self.duse[q][i]))
        for e in ENGS:
            waits = [(s, v) for (s, v) in toks if self.seen[e].get(id(s), 0) < v]
            for s, v in waits:
                self.seen[e][id(s)] = v

            def emit(eng, waits=waits):
                for s, v in waits:
                    eng.wait_ge(s, v)
            self.prog[e].append(emit)
            self.ninstr += len(waits)

    def fence(self, e, toks):
        best = {}
        for s, v in toks:
            if best.get(id(s), (None, 0))[1] < v:
                best[id(s)] = (s, v)

        def emit(eng, best=best):
            for s, v in best.values():
                eng.wait_ge(s, v)
        self.prog[e].append(emit)

    def run(self):
        nc = self.nc
        with nc.Block() as block:
            @block.tensor
            def _(eng):
                for f in self.prog["pe"]:
                    f(eng)

            @block.scalar
            def _(eng):
                for f in self.prog["act"]:
                    f(eng)

            @block.vector
            def _(eng):
                for f in self.prog["dve"]:
                    f(eng)

            @block.gpsimd
            def _(eng):
                self.bc_reg = eng.to_reg(NSLOT - 1)
                for f in self.prog["pool"]:
                    f(eng)

            @block.sync
            def _(eng):
                for f in self.prog["sp"]:
                    f(eng)


@contextmanager
def scope(k):
    with ExitStack() as es:
        yield es
        k.sc.barrier()


def run_pipelined(gens, depth, skew=1):
    gens = iter(gens)
    active, rnd, done = [], 0, False
    while True:
        if not done and len(active) < depth and rnd % skew == 0:
            g = next(gens, None)
            if g is None:
                done = True
            else:
                active.append(g)
        if done and not active:
            break
        for g in list(active):
            try:
                next(g)
            except StopIteration:
                active.remove(g)
        rnd += 1


def bcast_rows(ap, n=128):
    return ap.partition_broadcast(n)


class K:
    pass


def build(upto=99, dbg=()):
    nc = bass.Bass("TRN2", target_bir_lowering=False)
    k = K()
    k.nc = nc
    k.inputs = []
    k.dbg = dbg

    def dt_in(name, shape, dt=F32):
        k.inputs.append(name)
        return nc.dram_tensor(name, list(shape), dt, kind="ExternalInput")
    k.x = dt_in("x", [S, D])
    k.ln_in_g = dt_in("ln_in_g", [1, D])
    k.ln_in_b = dt_in("ln_in_b", [1, D])
    k.c_ident = dt_in("c_ident", [128, 128], BF16)
    k.c_identf = dt_in("c_identf", [128, 128])
    if upto >= 2:
        k.mem = dt_in("mem", [MEM, D])
        k.w_in = dt_in("w_in_proj", [D, INW])
        k.w_kr_sw = dt_in("w_kr_sw", [D, 32])
        k.gqa_qn = dt_in("gqa_q_norm", [1, 512])
        k.gqa_kn = dt_in("gqa_k_norm", [1, 128])
        k.mla_qn = dt_in("mla_q_norm", [1, 384])
        k.mla_kvn = dt_in("mla_kv_norm", [1, 256])
        k.w_qb = dt_in("w_mla_qb", [384, 768])
        k.w_qb_sw = dt_in("w_mla_qb_sw", [384, 768])
        k.w_kvb = dt_in("w_mla_kvb", [256, 1024])
        k.w_mkv = dt_in("w_mem_kv", [D, D])
        k.c_rope64 = dt_in("c_rope64", [128, 2, NT, 64])
        k.c_ropeT = dt_in("c_ropeT", [128, 2, S])
        k.c_sel = dt_in("c_sel", [128, 128])
    if upto >= 5:
        k.b_gate = dt_in("b_gate", [1, 3 * D])
        k.w_br_a = dt_in("w_br_gqa", [512, D])
        k.w_br_b = dt_in("w_br_mla", [512, D])
        k.w_br_c = dt_in("w_br_mem", [512, D])
        k.w_out = dt_in("w_out", [D, D])
        k.ln1_g = dt_in("ln1_g", [1, D])
        k.ln1_b = dt_in("ln1_b", [1, D])
        k.w_router = dt_in("w_router", [D, NE])
        k.b_router = dt_in("b_router", [1, NE])
        k.c_tri = dt_in("c_tri", [128, 128], BF16)
        k.c_ecap = dt_in("c_ecap", [128, NE])
    if upto >= 6:
        k.w_ein = dt_in("w_exp_in", [NE, D, 2 * D])
        k.b_ein = dt_in("b_exp_in", [128, NE * 16])
        k.w_eout = dt_in("w_exp_out", [NE, D, D])
        k.b_eout = dt_in("b_exp_out", [NE, D])
        k.ln2_g = dt_in("ln2_g", [1, D])
        k.ln2_b = dt_in("ln2_b", [1, D])
    k.y = nc.dram_tensor("y", [S, D], F32, kind="ExternalOutput")
    sk = lambda name: ("ExternalOutput" if name in dbg else "Internal")
    k.h0_d = nc.dram_tensor("h0_d", [S, D], F32, kind=sk("h0_d"))
    k.h0T_d = nc.dram_tensor("h0T_d", [NT, 128, D], BF16, kind=sk("h0T_d"))
    k.oA_d = nc.dram_tensor("oA_d", [8, 64, S], BF16, kind=sk("oA_d"))
    k.oB_d = nc.dram_tensor("oB_d", [8, 64, S], BF16, kind=sk("oB_d"))
    k.oC_d = nc.dram_tensor("oC_d", [4, 128, S], BF16, kind=sk("oC_d"))
    k.h1_d = nc.dram_tensor("h1_d", [S, D], F32, kind=sk("h1_d"))
    k.xs_d = nc.dram_tensor("xs_d", [NSLOT, D], BF16, kind=sk("xs_d"))
    k.ys_d = nc.dram_tensor("ys_d", [NSLOT, D], F32, kind=sk("ys_d"))
    k.rt_d = nc.dram_tensor("rt_d", [S, 8], F32, kind=sk("rt_d"))

    with ExitStack() as es:
        sc = Sched(nc, es)
        k.sc = sc
        k.pp = [es.enter_context(nc.psum_tensor(f"pp{i}", [128, 1024], F32)) for i in range(4)]
        k.ppb = [p.bitcast(BF16) for p in k.pp]
        sb = lambda name, shape, dt=F32: es.enter_context(nc.sbuf_tensor(name, list(shape), dt))
        k.ident = sb("ident", [128, 128], BF16)
        k.identf = sb("identf", [128, 128], F32)
        k.neghalf = sb("neghalf", [128, 8], F32)
        k.ones_bf = sb("ones_bf", [128, 128], BF16)
        k.ones_f = sb("ones_f", [128, 128], F32)
        k.slots = sb("slots_all", [128, NT, 4], I32)
        k.wts = sb("wts_all", [128, NT, 4], F32)
        sc.dma("sp", lambda e: e.dma_start(out=k.ident[:, :], in_=k.c_ident[:, :]), W=["ident"])
        sc.dma("sp", lambda e: e.dma_start(out=k.identf[:, :], in_=k.c_identf[:, :]), W=["identf"])
        if upto >= 2:
            k.sel = sb("sel", [128, 128], F32)
            sc.dma("sp", lambda e: e.dma_start(out=k.sel[:, :], in_=k.c_sel[:, :]), W=["sel"])
        sc.op("pool", lambda e: e.memset(k.neghalf[:, :], -0.5), W=["neghalf"])
        sc.op("pool", lambda e: e.memset(k.ones_bf[:, :], 1.0), W=["ones_bf"])
        sc.op("pool", lambda e: e.memset(k.ones_f[:, :], 1.0), W=["ones_f"])

        phase1(k)
        if upto >= 2:
            phase2(k)
        if upto >= 3:
            phase3(k)
        if upto >= 4:
            phase4(k)
        if upto >= 5:
            phase5(k)
        if upto >= 6:
            phase6(k)
            phase7(k)
        toks = [t for r, t in sc.lastw.items() if isinstance(r, tuple) and r[0] in ("y", "dram")]
        sc.fence("sp", toks)
        sc.run()
    k.ninstr = sc.ninstr
    return nc, k


def psb(k, i):
    return k.pp[i // 2][:, (i % 2) * 512:(i % 2 + 1) * 512]


def psbf(k, i):
    return k.ppb[i // 2][:, (i % 2) * 1024:(i % 2 + 1) * 1024]


def layer_norm_tile(k, tag, z, g_rep, b_rep, out, scr, stages=False):
    g = _ln_gen(k, tag, z, g_rep, b_rep, out, scr)
    if stages:
        return g
    for _ in g:
        pass


def _ln_gen(k, tag, z, g_rep, b_rep, out, scr):
    sc = k.sc
    st, mv, rs = scr["st"], scr["mv"], scr["rs"]
    for h in range(2):
        sc.op("dve", lambda e, h=h: e.bn_stats(st[:, h * 6:(h + 1) * 6], z[:, h * 512:(h + 1) * 512]),
              R=[tag + "z"], W=[tag + f"st{h}"])
    sc.op("dve", lambda e: e.bn_aggr(mv[:, 0:2], st[:, 0:12]), R=[tag + "st0", tag + "st1"], W=[tag + "mv"])
    sc.op("dve", lambda e: e.tensor_scalar_add(rs[:, 0:1], mv[:, 1:2], LN_EPS), R=[tag + "mv"], W=[tag + "rs0"])
    yield
    sc.op("pool", lambda e: e.tensor_tensor(rs[:, 1:2], rs[:, 0:1], k.neghalf[:, 0:1], ALU.pow),
          R=[tag + "rs0", "neghalf"], W=[tag + "rs"])
    yield
    sc.op("dve", lambda e: e.tensor_scalar(out[:, :], z[:, :], mv[:, 0:1], rs[:, 1:2], ALU.subtract, ALU.mult),
          R=[tag + "z", tag + "mv", tag + "rs"], W=[tag + "o"])
    sc.op("dve", lambda e: e.tensor_tensor(out[:, :], out[:, :], g_rep[:, :], ALU.mult),
          R=[tag + "o", "lnconst"], W=[tag + "o"])
    sc.op("dve", lambda e: e.tensor_tensor(out[:, :], out[:, :], b_rep[:, :], ALU.add),
          R=[tag + "o", "lnconst"], W=[tag + "o"])
    yield


def phase1(k):
    nc, sc = k.nc, k.sc
    with scope(k) as es:
        sb = lambda name, shape, dt=F32: es.enter_context(nc.sbuf_tensor(name, list(shape), dt))
        g_rep = sb("p1_g", [128, D])
        b_rep = sb("p1_b", [128, D])
        sc.dma("sp", lambda e: e.dma_start(out=g_rep[:, :], in_=bcast_rows(k.ln_in_g[0, :])), W=["lnconst"])
        sc.dma("sp", lambda e: e.dma_start(out=b_rep[:, :], in_=bcast_rows(k.ln_in_b[0, :])), W=["lnconst"])
        NBUF = 3
        xt = [sb(f"p1_x{i}", [128, D]) for i in range(NBUF)]
        ht = [sb(f"p1_h{i}", [128, D]) for i in range(NBUF)]
        hb = [sb(f"p1_hb{i}", [128, D], BF16) for i in range(NBUF)]
        hT = [sb(f"p1_hT{i}", [128, D], BF16) for i in range(NBUF)]
        scr = [dict(st=sb(f"p1_st{i}", [128, 12]), mv=sb(f"p1_mv{i}", [128, 2]), rs=sb(f"p1_rs{i}", [128, 2]))
               for i in range(NBUF)]
        def tile(t):
            i = t % NBUF
            tag = f"p1_{i}_"
            sc.dma("sp", lambda e: e.dma_start(out=xt[i][:, :], in_=k.x[t * 128:(t + 1) * 128, :]), W=[tag + "z"])
            yield
            for _ in layer_norm_tile(k, tag, xt[i], g_rep, b_rep, ht[i], scr[i], stages=True):
                yield
            sc.dma("sp", lambda e: e.dma_start(out=k.h0_d[t * 128:(t + 1) * 128, :], in_=ht[i][:, :]),
                   R=[tag + "o"], W=[("dram", "h0", t)])
            sc.op("act", lambda e: e.copy(hb[i][:, :], ht[i][:, :]), R=[tag + "o"], W=[tag + "hb"])
            yield
            pst = psbf(k, t % 2)
            for c in range(8):
                sc.op("pe", lambda e, c=c: e.transpose(pst[:, c * 128:(c + 1) * 128], hb[i][:, c * 128:(c + 1) * 128], k.ident[:, :]),
                      R=[tag + "hb", "ident"], W=[f"ps{t % 2}"])
            yield
            sc.op("dve", lambda e: e.tensor_copy(hT[i][:, :], pst[:, :]), R=[f"ps{t % 2}"], W=[tag + "hT"])
            yield
            sc.dma("sp", lambda e: e.dma_start(out=k.h0T_d[t, :, :], in_=hT[i][:, :]), R=[tag + "hT"], W=[("dram", "h0T", t)])
        run_pipelined((tile(t) for t in range(NT)), depth=3, skew=3)


def mmacc(k, out, pairs, R, W):
    n = len(pairs)
    for i, (l, r) in enumerate(pairs):
        k.sc.op("pe", lambda e, l=l, r=r, i=i: e.matmul(out, l, r, start=(i == 0), stop=(i == n - 1)), R=R, W=W)


def attention(k, tag, units, nkt, qk_fn, v_fn, dv, scale, out_fn, aug, Rres, nh):
    nc, sc = k.nc, k.sc
    W = 512 * nh
    with scope(k) as es:
        sb = lambda name, shape, dt=F32: es.enter_context(nc.sbuf_tensor(name, list(shape), dt))
        pt = [sb(f"{tag}_pt{i}", [128, 1024], BF16) for i in range(2)]
        osb = [sb(f"{tag}_osb{i}", [128, W]) for i in range(2)]
        rec = [sb(f"{tag}_rec{i}", [128, W]) for i in range(2)]
        onb = [sb(f"{tag}_onb{i}", [128, W], BF16) for i in range(2)]
        den = [sb(f"{tag}_den{i}", [1, 512]) for i in range(2)] if not aug else None
        G = nkt // 2 if nh == 1 else nkt
        gs = [(ui, g) for ui in range(len(units)) for g in range(G)]
        M = dv + 1 if aug else dv
        slot_of = {}
        pending = []

        def kts(g):
            return (2 * g, 2 * g + 1) if nh == 1 else (g, g)

        def qk(idx):
            ui, g = gs[idx]
            u = units[ui]
            sl = idx % 2
            slot_of[idx] = sl
            for i in range(2):
                l, r = qk_fn(u, kts(g)[i], i)
                sc.op("pe", lambda e, l=l, r=r, b=2 * sl + i: e.matmul(psb(k, b), l, r, start=True, stop=True),
                      R=Rres, W=[f"ps{2 * sl + i}"])
            sc.op("act", lambda e, sl=sl: e.activation(out=pt[sl][:, :], in_=k.pp[sl][:, :], func=AF.Exp, scale=scale),
                  R=[f"ps{2 * sl}", f"ps{2 * sl + 1}"], W=[f"{tag}_pt{sl}"])

        def accb(ui, i):
            return 4 + ui % 2 if nh == 1 else 4 + 2 * (ui % 2) + i

        def epi2(ui, I):
            u = units[ui]
            s2 = ui % 2
            sl = (I + 1) % 2
            for i in range(nh):
                b = 2 * sl + i
                if aug:
                    sc.op("pe", lambda e, b=b, i=i: e.matmul(psb(k, b)[0:dv, :], k.sel[0:M, 0:dv], osb[s2][0:M, i * 512:(i + 1) * 512], start=True, stop=True),
                          R=[f"{tag}_osb{s2}", "sel"], W=[f"ps{b}"])
                else:
                    sc.op("pe", lambda e, b=b: e.matmul(psb(k, b)[0:dv, :], k.ones_f[0:1, 0:dv], den[s2][0:1, :], start=True, stop=True),
                          R=[f"{tag}_den{s2}", "ones_f"], W=[f"ps{b}"])
            sc.op("dve", lambda e: e.reciprocal(rec[s2][0:dv, :], k.pp[sl][0:dv, 0:W]),
                  R=[f"ps{2 * sl}", f"ps{2 * sl + 1}"][:nh], W=[f"{tag}_rec{s2}"])
            sc.op("dve", lambda e: e.tensor_tensor(onb[s2][0:dv, :], osb[s2][0:dv, :], rec[s2][0:dv, :], ALU.mult),
                  R=[f"{tag}_osb{s2}", f"{tag}_rec{s2}"], W=[f"{tag}_onb{s2}"])
            src = onb[s2][0:dv, :] if nh == 1 else onb[s2][0:dv, :].rearrange("p (h t) -> p h t", h=2)
            sc.dma("sp", lambda e, src=src: e.dma_start(out=out_fn(u), in_=src),
                   R=[f"{tag}_onb{s2}"], W=[("dram", tag, u[0], u[1])])

        def pv(idx):
            ui, g = gs[idx]
            u = units[ui]
            sl = slot_of.pop(idx)
            s2 = ui % 2
            for i in range(2):
                kt = kts(g)[i]
                ob = accb(ui, i)
                first = (kt == 0) if nh == 2 else (kt == 0)
                last = (kt == nkt - 1)
                vv = v_fn(u, kt, i)
                sc.op("pe", lambda e, ob=ob, vv=vv, i=i, first=first, last=last: e.matmul(
                    psb(k, ob)[0:vv.shape[1], :], vv, pt[sl][:, i * 512:(i + 1) * 512], start=first, stop=last),
                    R=[f"{tag}_pt{sl}"] + Rres, W=[f"ps{ob}"])
                if not aug:
                    sc.op("pe", lambda e, kt=kt, i=i, first=first, last=last: e.matmul(
                        psb(k, 6 + ui % 2)[0:1, :], k.ones_bf[:, 0:1], pt[sl][:, i * 512:(i + 1) * 512], start=first, stop=last),
                        R=[f"{tag}_pt{sl}"], W=[f"ps{6 + ui % 2}"])
            if g == G - 1:
                if nh == 1:
                    sc.op("dve", lambda e: e.tensor_copy(osb[s2][0:M, :], psb(k, accb(ui, 0))[0:M, :]),
                          R=[f"ps{accb(ui, 0)}"], W=[f"{tag}_osb{s2}"])
                else:
                    sc.op("dve", lambda e: e.tensor_copy(osb[s2][0:M, :], k.pp[2 + ui % 2][0:M, :]),
                          R=[f"ps{accb(ui, 0)}", f"ps{accb(ui, 1)}"], W=[f"{tag}_osb{s2}"])
                if not aug:
                    sc.op("dve", lambda e: e.tensor_copy(den[s2][0:1, :], psb(k, 6 + ui % 2)[0:1, :]),
                          R=[f"ps{6 + ui % 2}"], W=[f"{tag}_den{s2}"])
                pending.append((idx + 2, ui))

        n = len(gs)
        for idx in range(n + 1):
            if idx < n:
                qk(idx)
            while pending and pending[0][0] <= idx:
                epi2(pending.pop(0)[1], idx)
            if idx >= 1:
                pv(idx - 1)
        while pending:
            epi2(pending.pop(0)[1], n)


def rms_rope_tm(k, tag, src, srcres, nh, gain_rep, r64, t, scr, out_bf):
    sc = k.sc
    w = nh * 64
    sq, ss, r, qn, t1, t2 = scr["sq"], scr["ss"], scr["r"], scr["qn"], scr["t1"], scr["t2"]
    v3 = lambda ap: ap.rearrange("p (h d) -> p h d", h=nh)
    v5 = lambda ap: ap.rearrange("p (h b two j) -> p h b two j", h=nh, b=2, two=2, j=16)
    Ct = r64[:, 0, t, :].unsqueeze(1).broadcast_to([128, nh, 64])
    St = r64[:, 1, t, :].rearrange("p (b two j) -> p b two j", b=2, two=2, j=16)
    sc.op("act", lambda e: e.activation(out=sq[:, 0:w], in_=src, func=AF.Square), R=[srcres], W=[tag + "sq"])
    yield
    sc.op("dve", lambda e: e.reduce_sum(ss[:, 0:nh], v3(sq[:, 0:w]), AX.X), R=[tag + "sq"], W=[tag + "ss"])
    sc.op("dve", lambda e: e.tensor_scalar(ss[:, 0:nh], ss[:, 0:nh], 1.0 / 64, RMS_EPS, ALU.mult, ALU.add),
          R=[tag + "ss"], W=[tag + "ss"])
    yield
    sc.op("pool", lambda e: e.tensor_tensor(r[:, 0:nh], ss[:, 0:nh], k.neghalf[:, 0:nh], ALU.pow),
          R=[tag + "ss", "neghalf"], W=[tag + "r"])
    yield
    sc.op("dve", lambda e: e.tensor_tensor(v3(qn[:, 0:w]), v3(src), r[:, 0:nh].unsqueeze(2).broadcast_to([128, nh, 64]), ALU.mult),
          R=[srcres, tag + "r"], W=[tag + "qn"])
    sc.op("dve", lambda e: e.tensor_tensor(qn[:, 0:w], qn[:, 0:w], gain_rep[:, 0:w], ALU.mult),
          R=[tag + "qn", "gains"], W=[tag + "qn"])
    sc.op("dve", lambda e: e.tensor_tensor(v3(t1[:, 0:w]), v3(qn[:, 0:w]), Ct, ALU.mult),
          R=[tag + "qn", "r64"], W=[tag + "t1"])
    for half in range(2):
        sc.op("dve", lambda e, half=half: e.tensor_tensor(
            v5(t2[:, 0:w])[:, :, :, half, :], v5(qn[:, 0:w])[:, :, :, 1 - half, :],
            St[:, :, half, :].unsqueeze(1).broadcast_to([128, nh, 2, 16]), ALU.mult),
            R=[tag + "qn", "r64"], W=[tag + f"t2{half}"])
    sc.op("dve", lambda e: e.tensor_tensor(out_bf, t1[:, 0:w], t2[:, 0:w], ALU.add),
          R=[tag + "t1", tag + "t20", tag + "t21"], W=[tag + "out"])
    yield


def phase2(k):
    nc, sc = k.nc, k.sc
    with scope(k) as es:
        sb = lambda name, shape, dt=F32: es.enter_context(nc.sbuf_tensor(name, list(shape), dt))
        qT = sb("a_qT", [128, 4, S], BF16)
        kd = sb("a_kd", [128, 2, S], BF16)
        va = sb("a_v", [128, NT, 2, 128], BF16)
        with scope(k) as es2:
            sb2 = lambda name, shape, dt=F32: es2.enter_context(nc.sbuf_tensor(name, list(shape), dt))
            wA = sb2("a_w", [128, 8, 768], BF16)
            sc.dma("pool", lambda e: e.dma_start(out=wA[:, :, :], in_=k.w_in[:, 0:768].rearrange("(c p) n -> p c n", p=128)),
                   W=["a_w"])
            gq = sb2("a_gq", [128, 512])
            gk = sb2("a_gk", [128, 128])
            r64 = sb2("a_r64", [128, 2, NT, 64])
            sc.dma("sp", lambda e: e.dma_start(out=gq[:, :], in_=bcast_rows(k.gqa_qn[0, :])), W=["gains"])
            sc.dma("sp", lambda e: e.dma_start(out=gk[:, :], in_=bcast_rows(k.gqa_kn[0, :])), W=["gains"])
            sc.dma("sp", lambda e: e.dma_start(out=r64[:, :, :, :], in_=k.c_rope64[:, :, :, :]), W=["r64"])
            sc.op("pool", lambda e: e.memset(va[:, :, :, 64:128], 0.0), W=["aV1"])
            sc.op("pool", lambda e: e.memset(va[:, :, :, 64:65], 1.0), W=["aV1"])
            hb = [sb2(f"a_hb{i}", [128, 4, D], BF16) for i in range(2)]
            mk = lambda i, w: dict(sq=sb2(f"a_sq{w}{i}", [128, w]), ss=sb2(f"a_ss{w}{i}", [128, 8]), r=sb2(f"a_r{w}{i}", [128, 8]),
                                   qn=sb2(f"a_qn{w}{i}", [128, w]), t1=sb2(f"a_t1{w}{i}", [128, w]), t2=sb2(f"a_t2{w}{i}", [128, w]))
            scq = [mk(i, 512) for i in range(2)]
            sck = [mk(i, 128) for i in range(2)]
            qr = [sb2(f"a_qr{i}", [128, 512], BF16) for i in range(2)]
            kr = [sb2(f"a_kr{i}", [128, 128], BF16) for i in range(2)]
            kdb = [sb2(f"a_kdb{i}", [128, 256], BF16) for i in range(2)]
            ptq = psbf(k, 4)
            ptk = psbf(k, 5)
            def load_hb(b):
                sc.dma("sp", lambda e: e.dma_start(out=hb[b % 2][:, :, :], in_=k.h0T_d[4 * b:4 * b + 4, :, :].rearrange("t p f -> p t f")),
                       R=[("dram", "h0T", 4 * b + j) for j in range(4)], W=[f"a_hb{b % 2}"])

            def tile(t):
                b, j = t // 4, t % 4
                hbb = hb[b % 2]
                if t == 0:
                    load_hb(0)
                if j == 0 and b + 1 < NB:
                    load_hb(b + 1)
                i2 = t % 2
                pa, pb = 2 * i2, 2 * i2 + 1
                mmacc(k, psb(k, pa), [(hbb[:, j, c * 128:(c + 1) * 128], wA[:, c, 0:512]) for c in range(8)],
                      R=[f"a_hb{b % 2}", "a_w"], W=[f"ps{pa}"])
                mmacc(k, psb(k, pb)[:, 0:256], [(hbb[:, j, c * 128:(c + 1) * 128], wA[:, c, 512:768]) for c in range(8)],
                      R=[f"a_hb{b % 2}", "a_w"], W=[f"ps{pb}"])
                yield
                tq, tk = f"a_q{i2}_", f"a_k{i2}_"
                gq_ = rms_rope_tm(k, tq, psb(k, pa), f"ps{pa}", 8, gq, r64, t, scq[i2], qr[i2][:, :])
                gk_ = rms_rope_tm(k, tk, psb(k, pb)[:, 0:128], f"ps{pb}", 2, gk, r64, t, sck[i2], kr[i2][:, :])
                sc.op("act", lambda e: e.copy(va[:, t, :, 0:64], psb(k, pb)[:, 128:256].rearrange("p (g d) -> p g d", g=2)),
                      R=[f"ps{pb}"], W=["aV"])
                for _ in zip(gq_, gk_):
                    yield
                for p in range(4):
                    sc.op("pe", lambda e, p=p: e.transpose(ptq[:, p * 128:(p + 1) * 128], qr[i2][:, p * 128:(p + 1) * 128], k.ident[:, :]),
                          R=[tq + "out", "ident"], W=["ps4"])
                kdv = kdb[i2][:, :].rearrange("p (g u d) -> p g u d", g=2, u=2, d=64)
                for u in range(2):
                    sc.op("act", lambda e, u=u: e.copy(kdv[:, :, u, :], kr[i2][:, :].rearrange("p (g d) -> p g d", g=2)),
                          R=[tk + "out"], W=[f"a_kdb{i2}_{u}"])
                yield
                sc.op("dve", lambda e: e.tensor_copy(qT[:, :, t * 128:(t + 1) * 128], ptq[:, 0:512].rearrange("p (c n) -> p c n", c=4)),
                      R=["ps4"], W=["aQ"])
                for g in range(2):
                    sc.op("pe", lambda e, g=g: e.transpose(ptk[:, g * 128:(g + 1) * 128], kdb[i2][:, g * 128:(g + 1) * 128], k.ident[:, :]),
                          R=[f"a_kdb{i2}_0", f"a_kdb{i2}_1", "ident"], W=["ps5"])
                yield
                sc.op("dve", lambda e: e.tensor_copy(kd[:, :, t * 128:(t + 1) * 128], ptk[:, 0:256].rearrange("p (c n) -> p c n", c=2)),
                      R=["ps5"], W=["aK"])
            run_pipelined((tile(t) for t in range(NT)), depth=2, skew=4)
        attention(k, "a", [(p, qb) for qb in range(NB) for p in range(4)], NT,
                  qk_fn=lambda u, kt, i: (kd[i * 64:i * 64 + 64, u[0] // 2, kt * 128:(kt + 1) * 128],
                                          qT[i * 64:i * 64 + 64, u[0], u[1] * 512:(u[1] + 1) * 512]),
                  v_fn=lambda u, kt, i: va[:, kt, u[0] // 2, :],
                  dv=64, scale=0.125,
                  out_fn=lambda u: k.oA_d[2 * u[0]:2 * u[0] + 2, :, u[1] * 512:(u[1] + 1) * 512].rearrange("h d t -> d h t"),
                  aug=True, Rres=["aQ", "aK", "aV", "aV1"], nh=2)


def phase3(k):
    nc, sc = k.nc, k.sc
    with scope(k) as es:
        sb = lambda name, shape, dt=F32: es.enter_context(nc.sbuf_tensor(name, list(shape), dt))
        qlT = sb("b_qlT", [128, 3, S], BF16)
        kvT = sb("b_kvT", [128, 2, S], BF16)
        kpe = sb("b_kpe", [128, S], BF16)
        rT = [sb(f"b_rT{i}", [128, 2, 512]) for i in range(2)]
        with scope(k) as es2:
            sb2 = lambda name, shape, dt=F32: es2.enter_context(nc.sbuf_tensor(name, list(shape), dt))
            ta = [sb2(f"b_ta{i}", [128, 512]) for i in range(2)]
            tb = [sb2(f"b_tb{i}", [128, 512]) for i in range(2)]
            wB = sb2("b_w", [128, 8, 672], BF16)
            sc.dma("pool", lambda e: e.dma_start(out=wB[:, :, :], in_=k.w_in[:, 768:1440].rearrange("(c p) n -> p c n", p=128)), W=["b_w"])
            wks = sb2("b_wks", [128, 8, 96], BF16)
            sc.op("pool", lambda e: e.memset(wks[:, :, 0:64], 0.0), W=["b_wks0"])
            sc.dma("pool", lambda e: e.dma_start(out=wks[:, :, 64:96], in_=k.w_kr_sw[:, :].rearrange("(c p) n -> p c n", p=128)), W=["b_wks1"])
            gql = sb2("b_gql", [128, 384])
            gkv = sb2("b_gkv", [128, 256])
            sc.dma("sp", lambda e: e.dma_start(out=gql[:, :], in_=bcast_rows(k.mla_qn[0, :])), W=["gains"])
            sc.dma("sp", lambda e: e.dma_start(out=gkv[:, :], in_=bcast_rows(k.mla_kvn[0, :])), W=["gains"])
            hb = [sb2(f"b_hb{i}", [128, 4, D], BF16) for i in range(2)]
            sq = [sb2(f"b_sq{i}", [128, 512]) for i in range(2)]
            st = [sb2(f"b_st{i}", [128, 8]) for i in range(2)]
            qn = [sb2(f"b_qn{i}", [128, 384]) for i in range(2)]
            kn = [sb2(f"b_kn{i}", [128, 256]) for i in range(2)]
            qlb = [sb2(f"b_qlb{i}", [128, 384], BF16) for i in range(2)]
            kvb = [sb2(f"b_kvb{i}", [128, 256], BF16) for i in range(2)]
            ptq = psbf(k, 4)
            ptk = psbf(k, 5)
            for b in range(NB):
                hbb = hb[b % 2]
                blk = slice(b * 512, (b + 1) * 512)
                sc.dma("sp", lambda e, b=b, hbb=hbb: e.dma_start(out=hbb[:, :, :], in_=k.h0T_d[4 * b:4 * b + 4, :, :].rearrange("t p f -> p t f")),
                       R=[("dram", "h0T", 4 * b + j) for j in range(4)], W=[f"b_hb{b % 2}"])
                rTb = rT[b % 2]
                sc.dma("sp", lambda e, b=b, rTb=rTb: e.dma_start(out=rTb[:, :, :], in_=k.c_ropeT[:, :, b * 512:(b + 1) * 512]), W=[f"b_rT{b % 2}"])
                for j in range(4):
                    t = 4 * b + j
                    i2 = t % 2
                    pa, pb = 2 * i2, 2 * i2 + 1
                    tg = f"b1_{i2}_"
                    mmacc(k, psb(k, pa), [(hbb[:, j, c * 128:(c + 1) * 128], wB[:, c, 0:512]) for c in range(8)],
                          R=[f"b_hb{b % 2}", "b_w"], W=[f"ps{pa}"])
                    mmacc(k, psb(k, pb)[:, 0:128], [(hbb[:, j, c * 128:(c + 1) * 128], wB[:, c, 512:640]) for c in range(8)],
                          R=[f"b_hb{b % 2}", "b_w"], W=[f"ps{pb}"])
                    s_, st_ = sq[i2], st[i2]
                    sc.op("act", lambda e, s_=s_, st_=st_, pa=pa: e.activation(out=s_[:, 0:384], in_=psb(k, pa)[:, 0:384], func=AF.Square, accum_out=st_[:, 0:1]),
                          R=[f"ps{pa}"], W=[tg + "sq", tg + "s0"])
                    sc.op("act", lambda e, s_=s_, st_=st_, pa=pa: e.activation(out=s_[:, 384:512], in_=psb(k, pa)[:, 384:512], func=AF.Square, accum_out=st_[:, 1:2]),
                          R=[f"ps{pa}"], W=[tg + "sq2", tg + "s1"])
                    sc.op("act", lambda e, s_=s_, st_=st_, pb=pb: e.activation(out=s_[:, 0:128], in_=psb(k, pb)[:, 0:128], func=AF.Square, accum_out=st_[:, 2:3]),
                          R=[f"ps{pb}", tg + "sq"], W=[tg + "sq", tg + "s2"])
                    sc.op("dve", lambda e, st_=st_: e.tensor_scalar(st_[:, 3:4], st_[:, 0:1], 1.0 / 384, RMS_EPS, ALU.mult, ALU.add),
                          R=[tg + "s0"], W=[tg + "s3"])
                    sc.op("dve", lambda e, st_=st_: e.tensor_tensor(st_[:, 4:5], st_[:, 1:2], st_[:, 2:3], ALU.add),
                          R=[tg + "s1", tg + "s2"], W=[tg + "s4"])
                    sc.op("dve", lambda e, st_=st_: e.tensor_scalar(st_[:, 4:5], st_[:, 4:5], 1.0 / 256, RMS_EPS, ALU.mult, ALU.add),
                          R=[tg + "s4"], W=[tg + "s4"])
                    sc.op("pool", lambda e, st_=st_: e.tensor_tensor(st_[:, 5:7], st_[:, 3:5], k.neghalf[:, 0:2], ALU.pow),
                          R=[tg + "s3", tg + "s4", "neghalf"], W=[tg + "r"])
                    sc.op("dve", lambda e, i2=i2, st_=st_, pa=pa: e.tensor_scalar(qn[i2][:, :], psb(k, pa)[:, 0:384], st_[:, 5:6], None, ALU.mult),
                          R=[f"ps{pa}", tg + "r"], W=[tg + "qn"])
                    sc.op("pool", lambda e, i2=i2: e.tensor_tensor(qlb[i2][:, :], qn[i2][:, :], gql[:, :], ALU.mult),
                          R=[tg + "qn", "gains"], W=[tg + "qlb"])
                    sc.op("dve", lambda e, i2=i2, st_=st_, pa=pa: e.tensor_scalar(kn[i2][:, 0:128], psb(k, pa)[:, 384:512], st_[:, 6:7], None, ALU.mult),
                          R=[f"ps{pa}", tg + "r"], W=[tg + "kn0"])
                    sc.op("dve", lambda e, i2=i2, st_=st_, pb=pb: e.tensor_scalar(kn[i2][:, 128:256], psb(k, pb)[:, 0:128], st_[:, 6:7], None, ALU.mult),
                          R=[f"ps{pb}", tg + "r"], W=[tg + "kn1"])
                    sc.op("pool", lambda e, i2=i2: e.tensor_tensor(kvb[i2][:, :], kn[i2][:, :], gkv[:, :], ALU.mult),
                          R=[tg + "kn0", tg + "kn1", "gains"], W=[tg + "kvb"])
                    for c in range(3):
                        sc.op("pe", lambda e, c=c, i2=i2: e.transpose(ptq[:, c * 128:(c + 1) * 128], qlb[i2][:, c * 128:(c + 1) * 128], k.ident[:, :]),
                              R=[tg + "qlb", "ident"], W=["ps4"])
                    sc.op("dve", lambda e, t=t: e.tensor_copy(qlT[:, :, t * 128:(t + 1) * 128], ptq[:, 0:384].rearrange("p (c n) -> p c n", c=3)),
                          R=["ps4"], W=["b_qlT"])
                    for c in range(2):
                        sc.op("pe", lambda e, c=c, i2=i2: e.transpose(ptk[:, c * 128:(c + 1) * 128], kvb[i2][:, c * 128:(c + 1) * 128], k.ident[:, :]),
                              R=[tg + "kvb", "ident"], W=["ps5"])
                    sc.op("act", lambda e, t=t: e.copy(kvT[:, :, t * 128:(t + 1) * 128], ptk[:, 0:256].rearrange("p (c n) -> p c n", c=2)),
                          R=["ps5"], W=["b_kvT"])
                mmacc(k, psb(k, 6)[0:96, :], [(wB[:, c, 576:672], hbb[:, :, c * 128:(c + 1) * 128]) for c in range(8)],
                      R=[f"b_hb{b % 2}", "b_w"], W=["ps6"])
                mmacc(k, psb(k, 7)[0:96, :], [(wks[:, c, 0:96], hbb[:, :, c * 128:(c + 1) * 128]) for c in range(8)],
                      R=[f"b_hb{b % 2}", "b_wks0", "b_wks1"], W=["ps7"])
                a_, b_ = ta[b % 2], tb[b % 2]
                sc.op("dve", lambda e, a_=a_, rTb=rTb: e.tensor_tensor(a_[64:96, :], psb(k, 6)[64:96, :], rTb[64:96, 0, :], ALU.mult),
                      R=["ps6", f"b_rT{b % 2}"], W=[f"b_ta{b % 2}"])
                sc.op("dve", lambda e, b_=b_, rTb=rTb: e.tensor_tensor(b_[64:96, :], psb(k, 7)[64:96, :], rTb[64:96, 1, :], ALU.mult),
                      R=["ps7", f"b_rT{b % 2}"], W=[f"b_tb{b % 2}"])
                sc.op("pool", lambda e, a_=a_, b_=b_, blk=blk: e.tensor_tensor(kpe[64:96, blk], a_[64:96, :], b_[64:96, :], ALU.add),
                      R=[f"b_ta{b % 2}", f"b_tb{b % 2}"], W=["b_kpe"])
        wqb = sb("b_wqb", [128, 3, 768], BF16)
        wqs = sb("b_wqs", [128, 3, 768], BF16)
        wkv = sb("b_wkv", [128, 2, 1024], BF16)
        sc.dma("pool", lambda e: e.dma_start(out=wqb[:, :, :], in_=k.w_qb[:, :].rearrange("(c p) n -> p c n", p=128)), W=["b_wqb"])
        sc.dma("pool", lambda e: e.dma_start(out=wqs[:, :, :], in_=k.w_qb_sw[:, :].rearrange("(c p) n -> p c n", p=128)), W=["b_wqs"])
        sc.dma("pool", lambda e: e.dma_start(out=wkv[:, :, :], in_=k.w_kvb[:, :].rearrange("(c p) n -> p c n", p=128)), W=["b_wkv"])
        qT = sb("b_qT", [128, 4, S], BF16)
        kT = sb("b_kT", [128, 4, S], BF16)
        vb = sb("b_v", [128, NT, 4, 128], BF16)
        sc.op("pool", lambda e: e.memset(vb[:, :, :, 64:128], 0.0), W=["bV1"])
        sc.op("pool", lambda e: e.memset(vb[:, :, :, 64:65], 1.0), W=["bV1"])
        sc.op("pool", lambda e: e.memset(qT[96:128, :, :], 0.0), W=["bV1"])
        sc.op("pool", lambda e: e.memset(kT[96:128, :, :], 0.0), W=["bV1"])
        ta4 = [sb(f"b_ta4{i}", [128, 512]) for i in range(4)]
        tb4 = [sb(f"b_tb4{i}", [128, 512]) for i in range(4)]
        for half in range(2):
            def blockgen(b, half=half):
                blk = slice(b * 512, (b + 1) * 512)
                rTb = rT[b % 2]
                sc.dma("sp", lambda e: e.dma_start(out=rTb[:, :, :], in_=k.c_ropeT[:, :, b * 512:(b + 1) * 512]), W=[f"b_rT{b % 2}"])
                yield
                for hl in range(4):
                    h = 4 * half + hl
                    pq, pswp, pn = hl % 2, 2 + hl % 2, 4 + hl % 2
                    x4 = (b % 2) * 2 + hl % 2
                    a_, b_ = ta4[x4], tb4[x4]
                    mmacc(k, psb(k, pq)[0:96, :], [(wqb[:, c, h * 96:(h + 1) * 96], qlT[:, c, blk]) for c in range(3)],
                          R=["b_wqb", "b_qlT"], W=[f"ps{pq}"])
                    mmacc(k, psb(k, pswp)[0:96, :], [(wqs[:, c, h * 96:(h + 1) * 96], qlT[:, c, blk]) for c in range(3)],
                          R=["b_wqs", "b_qlT"], W=[f"ps{pswp}"])
                    mmacc(k, psb(k, pn)[0:64, :], [(wkv[:, c, h * 128:h * 128 + 64], kvT[:, c, blk]) for c in range(2)],
                          R=["b_wkv", "b_kvT"], W=[f"ps{pn}"])
                    yield
                    sc.op("act", lambda e, hl=hl, pq=pq: e.copy(qT[0:64, hl, blk], psb(k, pq)[0:64, :]), R=[f"ps{pq}"], W=["bQ"])
                    sc.op("dve", lambda e, a_=a_, pq=pq: e.tensor_tensor(a_[64:96, :], psb(k, pq)[64:96, :], rTb[64:96, 0, :], ALU.mult),
                          R=[f"ps{pq}", f"b_rT{b % 2}"], W=[f"b_ta4{x4}"])
                    sc.op("dve", lambda e, b_=b_, pswp=pswp: e.tensor_tensor(b_[64:96, :], psb(k, pswp)[64:96, :], rTb[64:96, 1, :], ALU.mult),
                          R=[f"ps{pswp}", f"b_rT{b % 2}"], W=[f"b_tb4{x4}"])
                    sc.op("act", lambda e, hl=hl, pn=pn: e.copy(kT[0:64, hl, blk], psb(k, pn)[0:64, :]), R=[f"ps{pn}"], W=["bK"])
                    sc.op("act", lambda e, hl=hl: e.copy(kT[64:96, hl, blk], kpe[64:96, blk]), R=["b_kpe"], W=["bK"])
                    yield
                    sc.op("dve", lambda e, a_=a_, b_=b_, hl=hl: e.tensor_tensor(qT[64:96, hl, blk], a_[64:96, :], b_[64:96, :], ALU.add),
                          R=[f"b_ta4{x4}", f"b_tb4{x4}"], W=["bQ"])
                for j in range(4):
                    t = 4 * b + j
                    pv_ = 6 + t % 2
                    wv = [wkv[:, c, :].rearrange("p (h n) -> p h n", n=128)[:, 4 * half:4 * half + 4, 64:128] for c in range(2)]
                    mmacc(k, psb(k, pv_)[:, 0:256], [(kvT[:, c, t * 128:(t + 1) * 128], wv[c]) for c in range(2)],
                          R=["b_wkv", "b_kvT"], W=[f"ps{pv_}"])
                    yield
                    sc.op("dve", lambda e, t=t, pv_=pv_: e.tensor_copy(vb[:, t, :, 0:64], psb(k, pv_)[:, 0:256].rearrange("p (h d) -> p h d", h=4)),
                          R=[f"ps{pv_}"], W=["bV"])
            run_pipelined((blockgen(b) for b in range(NB)), depth=2, skew=5)
            attention(k, f"b{half}", [(hl, qb) for qb in range(NB) for hl in range(4)], NT,
                      qk_fn=lambda u, kt, i: (kT[:, u[0], kt * 128:(kt + 1) * 128], qT[:, u[0], u[1] * 512:(u[1] + 1) * 512]),
                      v_fn=lambda u, kt, i: vb[:, kt, u[0], :],
                      dv=64, scale=96.0 ** -0.5,
                      out_fn=lambda u, half=half: k.oB_d[4 * half + u[0], :, u[1] * 512:(u[1] + 1) * 512],
                      aug=True, Rres=["bQ", "bK", "bV", "bV1"], nh=1)


def phase4(k):
    nc, sc = k.nc, k.sc
    with scope(k) as es:
        sb = lambda name, shape, dt=F32: es.enter_context(nc.sbuf_tensor(name, list(shape), dt))
        qm = sb("c_q", [128, 4, S], BF16)
        mkT = sb("c_mk", [128, 4, MEM], BF16)
        mv = sb("c_mv", [128, 2, 4, 128], BF16)
        with scope(k) as es2:
            sb2 = lambda name, shape, dt=F32: es2.enter_context(nc.sbuf_tensor(name, list(shape), dt))
            wq = sb2("c_wq", [128, 8, 512], BF16)
            wm = sb2("c_wm", [128, 8, D], BF16)
            sc.dma("pool", lambda e: e.dma_start(out=wq[:, :, :], in_=k.w_in[:, 1440:1952].rearrange("(c p) n -> p c n", p=128)), W=["c_wq"])
            sc.dma("pool", lambda e: e.dma_start(out=wm[:, :, :], in_=k.w_mkv[:, :].rearrange("(c p) n -> p c n", p=128)), W=["c_wm"])
            memf = sb2("c_memf", [128, 2, D])
            memb = sb2("c_memb", [128, 2, D], BF16)
            memT = sb2("c_memT", [128, 8, MEM], BF16)
            sc.dma("sp", lambda e: e.dma_start(out=memf[:, :, :], in_=k.mem[:, :].rearrange("(t p) d -> p t d", p=128)), W=["c_memf"])
            sc.op("act", lambda e: e.copy(memb[:, :, :], memf[:, :, :]), R=["c_memf"], W=["c_memb"])
            pt4 = psbf(k, 4)
            pt5 = psbf(k, 5)
            for t in range(2):
                pt = pt4 if t == 0 else pt5
                for c in range(8):
                    sc.op("pe", lambda e, t=t, c=c, pt=pt: e.transpose(pt[:, c * 128:(c + 1) * 128], memb[:, t, c * 128:(c + 1) * 128], k.ident[:, :]),
                          R=["c_memb", "ident"], W=[f"ps{4 + t}"])
                sc.op("dve", lambda e, t=t, pt=pt: e.tensor_copy(memT[:, :, t * 128:(t + 1) * 128], pt[:, :].rearrange("p (c n) -> p c n", c=8)),
                      R=[f"ps{4 + t}"], W=["c_memT"])
            for h in range(4):
                pb_ = h % 2
                mmacc(k, psb(k, pb_)[:, 0:MEM], [(wm[:, c, h * 128:(h + 1) * 128], memT[:, c, :]) for c in range(8)],
                      R=["c_wm", "c_memT"], W=[f"ps{pb_}"])
                sc.op("act", lambda e, h=h, pb_=pb_: e.copy(mkT[:, h, :], psb(k, pb_)[:, 0:MEM]), R=[f"ps{pb_}"], W=["cK"])
            for t in range(2):
                mmacc(k, psb(k, 2 + t), [(memT[:, c, t * 128:(t + 1) * 128], wm[:, c, 512:1024]) for c in range(8)],
                      R=["c_wm", "c_memT"], W=[f"ps{2 + t}"])
                sc.op("dve", lambda e, t=t: e.tensor_copy(mv[:, t, :, :], psb(k, 2 + t).rearrange("p (h d) -> p h d", h=4)),
                      R=[f"ps{2 + t}"], W=["cV"])
            hb = [sb2(f"c_hb{i}", [128, 4, D], BF16) for i in range(2)]
            for b in range(NB):
                hbb = hb[b % 2]
                blk = slice(b * 512, (b + 1) * 512)
                sc.dma("sp", lambda e, b=b, hbb=hbb: e.dma_start(out=hbb[:, :, :], in_=k.h0T_d[4 * b:4 * b + 4, :, :].rearrange("t p f -> p t f")),
                       R=[("dram", "h0T", 4 * b + j) for j in range(4)], W=[f"c_hb{b % 2}"])
                for h in range(4):
                    pb_ = 4 + h % 2 if h < 2 else 6 + h % 2
                    mmacc(k, psb(k, pb_), [(wq[:, c, h * 128:(h + 1) * 128], hbb[:, :, c * 128:(c + 1) * 128]) for c in range(8)],
                          R=["c_wq", f"c_hb{b % 2}"], W=[f"ps{pb_}"])
                    eng = "act" if h % 2 == 0 else "dve"
                    if eng == "act":
                        sc.op("act", lambda e, h=h, blk=blk, pb_=pb_: e.copy(qm[:, h, blk], psb(k, pb_)), R=[f"ps{pb_}"], W=["cQ"])
                    else:
                        sc.op("dve", lambda e, h=h, blk=blk, pb_=pb_: e.tensor_copy(qm[:, h, blk], psb(k, pb_)), R=[f"ps{pb_}"], W=["cQ"])
        attention(k, "c", [(h, qb) for qb in range(NB) for h in range(4)], 2,
                  qk_fn=lambda u, kt, i: (mkT[:, u[0], kt * 128:(kt + 1) * 128], qm[:, u[0], u[1] * 512:(u[1] + 1) * 512]),
                  v_fn=lambda u, kt, i: mv[:, kt, u[0], :],
                  dv=128, scale=128.0 ** -0.5, out_fn=lambda u: k.oC_d[u[0], :, u[1] * 512:(u[1] + 1) * 512],
                  aug=False, Rres=["cQ", "cK", "cV"], nh=1)


def phase5(k):
    nc, sc = k.nc, k.sc
    BIG = float(1 << 22)
    with scope(k) as es:
        sb = lambda name, shape, dt=F32: es.enter_context(nc.sbuf_tensor(name, list(shape), dt))
        wg = sb("m_wg", [128, 8, 3 * D], BF16)
        for i in range(3):
            sc.dma("pool", lambda e, i=i: e.dma_start(out=wg[:, :, i * D:(i + 1) * D],
                                                     in_=k.w_in[:, OFF_G + i * D:OFF_G + (i + 1) * D].rearrange("(c p) n -> p c n", p=128)),
                   W=[f"m_wg{i}"])
        bg = sb("m_bg", [128, 3 * D])
        sc.dma("sp", lambda e: e.dma_start(out=bg[:, :], in_=bcast_rows(k.b_gate[0, :])), W=["m_bg"])
        wa = sb("m_wa", [64, 8, D], BF16)
        wb_ = sb("m_wb", [64, 8, D], BF16)
        wc = sb("m_wc", [128, 4, D], BF16)
        wo = sb("m_wo", [128, 8, D], BF16)
        sc.dma("pool", lambda e: e.dma_start(out=wa[:, :, :], in_=k.w_br_a[:, :].rearrange("(h p) n -> p h n", p=64)), W=["m_wa"])
        sc.dma("pool", lambda e: e.dma_start(out=wb_[:, :, :], in_=k.w_br_b[:, :].rearrange("(h p) n -> p h n", p=64)), W=["m_wb"])
        sc.dma("pool", lambda e: e.dma_start(out=wc[:, :, :], in_=k.w_br_c[:, :].rearrange("(h p) n -> p h n", p=128)), W=["m_wc"])
        sc.dma("pool", lambda e: e.dma_start(out=wo[:, :, :], in_=k.w_out[:, :].rearrange("(c p) n -> p c n", p=128)), W=["m_wo"])
        wr = sb("m_wr", [128, 8, NE])
        sc.dma("sp", lambda e: e.dma_start(out=wr[:, :, :], in_=k.w_router[:, :].rearrange("(c p) n -> p c n", p=128)), W=["m_wr"])
        brr = sb("m_br", [128, NE])
        sc.dma("sp", lambda e: e.dma_start(out=brr[:, :], in_=bcast_rows(k.b_router[0, :])), W=["m_br"])
        g1 = sb("m_g1", [128, D])
        b1 = sb("m_b1", [128, D])
        sc.dma("sp", lambda e: e.dma_start(out=g1[:, :], in_=bcast_rows(k.ln1_g[0, :])), W=["lnconst"])
        sc.dma("sp", lambda e: e.dma_start(out=b1[:, :], in_=bcast_rows(k.ln1_b[0, :])), W=["lnconst"])
        tri = sb("m_tri", [128, 128], BF16)
        ecap = sb("m_ecap", [128, NE])
        sc.dma("sp", lambda e: e.dma_start(out=tri[:, :], in_=k.c_tri[:, :]), W=["m_tri"])
        sc.dma("sp", lambda e: e.dma_start(out=ecap[:, :], in_=k.c_ecap[:, :]), W=["m_ecap"])
        base = sb("m_base", [128, NE])
        sc.op("pool", lambda e: e.memset(base[:, :], 0.0), W=["m_base"])
        NB2 = 2
        hT = [sb(f"m_hT{i}", [128, D], BF16) for i in range(NB2)]
        oa = [sb(f"m_oa{i}", [64, 8, 128], BF16) for i in range(NB2)]
        ob = [sb(f"m_ob{i}", [64, 8, 128], BF16) for i in range(NB2)]
        oc = [sb(f"m_oc{i}", [128, 4, 128], BF16) for i in range(NB2)]
        h0 = [sb(f"m_h0{i}", [128, D]) for i in range(NB2)]
        gs = [[sb(f"m_gs{i}_{g}", [128, 512]) for g in range(2)] for i in range(NB2)]
        mg = [sb(f"m_mg{i}", [128, D]) for i in range(NB2)]
        mgb = [sb(f"m_mgb{i}", [128, D], BF16) for i in range(NB2)]
        mT = [sb(f"m_mT{i}", [128, D], BF16) for i in range(NB2)]
        h1 = [sb(f"m_h1{i}", [128, D]) for i in range(NB2)]
        h1b = [sb(f"m_h1b{i}", [128, D], BF16) for i in range(NB2)]
        h1T = sb("m_h1T", [128, D])
        scr = [dict(st=sb(f"m_st{i}", [128, 12]), mv=sb(f"m_mv{i}", [128, 2]), rs=sb(f"m_rs{i}", [128, 2])) for i in range(NB2)]
        lg = sb("m_lg", [128, NE])
        v8 = sb("m_v8", [128, 8])
        sm = sb("m_sm", [128, 16])
        ex = sb("m_ex", [128, 4])
        oh = sb("m_oh", [128, 4, NE])
        prod = sb("m_prod", [128, 4, NE])
        mask = sb("m_mask", [128, NE])
        maskb = sb("m_maskb", [128, NE], BF16)
        rank = sb("m_rank", [128, NE])
        rk = sb("m_rk", [128, 4])
        ek = sb("m_ek", [128, 4])
        vl = sb("m_vl", [128, 4])
        sl = sb("m_sl", [128, 4])
        rtd = sb("m_rtd", [128, 8])
        brs = [(oa, wa, 8, 64, k.oA_d), (ob, wb_, 8, 64, k.oB_d), (oc, wc, 4, 128, k.oC_d)]

        def tile(t):
            i2 = t % NB2
            tk = slice(t * 128, (t + 1) * 128)
            T = f"m{i2}_"
            sc.dma("sp", lambda e: e.dma_start(out=hT[i2][:, :], in_=k.h0T_d[t, :, :]), R=[("dram", "h0T", t)], W=[T + "hT"])
            sc.dma("sp", lambda e: e.dma_start(out=h0[i2][:, :], in_=k.h0_d[tk, :]), R=[("dram", "h0", t)], W=[T + "h0"])
            for bi, (ot, _, nh, kr_, od) in enumerate(brs):
                tagn = "abc"[bi]
                rd = [r for r in sc.lastw if isinstance(r, tuple) and r[0] == "dram" and str(r[1]).startswith(tagn) and r[3] == t // 4]
                sc.dma("sp", lambda e, ot=ot, od=od: e.dma_start(out=ot[i2][:, :, :], in_=od[:, :, tk].rearrange("h d t -> d h t")),
                       R=rd, W=[T + "o" + tagn])
            yield
            pbase = 4 * i2
            for bi, (ot, wbr, nh, kr_, od) in enumerate(brs):
                tagn = "abc"[bi]
                for half in range(2):
                    gi = half
                    col = slice(bi * D + half * 512, bi * D + half * 512 + 512)
                    hs = slice(half * 512, (half + 1) * 512)
                    pg, pbr = pbase + gi, pbase + 2 + gi
                    g_ = gs[i2][gi]
                    mmacc(k, psb(k, pg), [(hT[i2][:, c * 128:(c + 1) * 128], wg[:, c, col]) for c in range(8)],
                          R=[T + "hT", f"m_wg{bi}"], W=[f"ps{pg}"])
                    mmacc(k, psb(k, pbr), [(ot[i2][0:kr_, h, :], wbr[0:kr_, h, hs]) for h in range(nh)],
                          R=[T + "o" + tagn, f"m_w{tagn}"], W=[f"ps{pbr}"])
                    sc.op("dve", lambda e, g_=g_, pg=pg, col=col: e.tensor_tensor(g_[:, :], psb(k, pg), bg[:, col], ALU.add),
                          R=[f"ps{pg}", "m_bg"], W=[T + f"gs{gi}"])
                    sc.op("act", lambda e, g_=g_: e.activation(out=g_[:, :], in_=g_[:, :], func=AF.Sigmoid),
                          R=[T + f"gs{gi}"], W=[T + f"gs{gi}"])
                    yield
                    if bi == 0:
                        sc.op("dve", lambda e, g_=g_, pbr=pbr, hs=hs: e.tensor_tensor(mg[i2][:, hs], g_[:, :], psb(k, pbr), ALU.mult),
                              R=[T + f"gs{gi}", f"ps{pbr}"], W=[T + f"mg{half}"])
                    else:
                        sc.op("dve", lambda e, g_=g_, pbr=pbr: e.tensor_tensor(g_[:, :], g_[:, :], psb(k, pbr), ALU.mult),
                              R=[T + f"gs{gi}", f"ps{pbr}"], W=[T + f"gs{gi}"])
                        sc.op("dve", lambda e, g_=g_, hs=hs: e.tensor_tensor(mg[i2][:, hs], mg[i2][:, hs], g_[:, :], ALU.add),
                              R=[T + f"gs{gi}", T + f"mg{half}"], W=[T + f"mg{half}"])
            yield
            sc.op("act", lambda e: e.copy(mgb[i2][:, :], mg[i2][:, :]), R=[T + "mg0", T + "mg1"], W=[T + "mgb"])
            yield
            ptb = psbf(k, pbase)
            for c in range(8):
                sc.op("pe", lambda e, c=c: e.transpose(ptb[:, c * 128:(c + 1) * 128], mgb[i2][:, c * 128:(c + 1) * 128], k.ident[:, :]),
                      R=[T + "mgb", "ident"], W=[f"ps{pbase}"])
            yield
            sc.op("dve", lambda e: e.tensor_copy(mT[i2][:, :], ptb[:, :]), R=[f"ps{pbase}"], W=[T + "mT"])
            yield
            for half in range(2):
                hs = slice(half * 512, (half + 1) * 512)
                pb_ = pbase + 1 + half
                mmacc(k, psb(k, pb_), [(mT[i2][:, c * 128:(c + 1) * 128], wo[:, c, hs]) for c in range(8)],
                      R=[T + "mT", "m_wo"], W=[f"ps{pb_}"])
            yield
            tag = T + "ln_"
            for half in range(2):
                hs = slice(half * 512, (half + 1) * 512)
                pb_ = pbase + 1 + half
                sc.op("dve", lambda e, hs=hs, pb_=pb_: e.scalar_tensor_tensor(mg[i2][:, hs], h0[i2][:, hs], ALPHA, psb(k, pb_), ALU.mult, ALU.add),
                      R=[f"ps{pb_}", T + "h0"], W=[tag + "z", T + f"mg{half}"])
            yield
            for _ in layer_norm_tile(k, tag, mg[i2], g1, b1, h1[i2], scr[i2], stages=True):
                yield
            sc._record(sc.lastw[tag + "o"], [T + "mg0", T + "mg1"], [])
            sc.dma("sp", lambda e: e.dma_start(out=k.h1_d[tk, :], in_=h1[i2][:, :]), R=[tag + "o"], W=[("dram", "h1", t)])
            sc.op("act", lambda e: e.copy(h1b[i2][:, :], h1[i2][:, :]), R=[tag + "o"], W=[T + "h1b"])
            for c in range(8):
                sc.op("pe", lambda e, c=c: e.transpose(k.pp[pbase // 2 + 1][:, c * 128:(c + 1) * 128], h1[i2][:, c * 128:(c + 1) * 128], k.identf[:, :]),
                      R=[tag + "o", "identf"], W=[f"ps{pbase + 2}", f"ps{pbase + 3}"])
            yield
            sc.op("dve", lambda e: e.tensor_copy(h1T[:, :], k.pp[pbase // 2 + 1][:, :]), R=[f"ps{pbase + 2}", f"ps{pbase + 3}"], W=["m_h1T"])
            yield
            pl_ = pbase
            mmacc(k, psb(k, pl_)[:, 0:NE], [(h1T[:, c * 128:(c + 1) * 128], wr[:, c, :]) for c in range(8)],
                  R=["m_h1T", "m_wr"], W=[f"ps{pl_}"])
            yield
            sc.op("dve", lambda e: e.tensor_tensor(lg[:, :], psb(k, pl_)[:, 0:NE], brr[:, :], ALU.add), R=[f"ps{pl_}", "m_br"], W=["m_lg"])
            sc.op("dve", lambda e: e.max(v8[:, :], lg[:, :]), R=["m_lg"], W=["m_v8"])
            sc.op("dve", lambda e: e.tensor_scalar_mul(sm[:, 0:1], v8[:, 0:1], -1.0), R=["m_v8"], W=["m_sm0"])
            for kk in range(4):
                sc.op("dve", lambda e, kk=kk: e.tensor_scalar(oh[:, kk, :], lg[:, :], v8[:, kk:kk + 1], None, ALU.is_equal),
                      R=["m_lg", "m_v8"], W=[f"m_oh{kk}"])
            ohr = [f"m_oh{kk}" for kk in range(4)]
            sc.op("act", lambda e: e.activation(out=ex[:, :], in_=v8[:, 0:4], func=AF.Exp, bias=sm[:, 0:1], accum_out=sm[:, 1:2]),
                  R=["m_v8", "m_sm0"], W=["m_ex", "m_sm1"])
            sc.op("dve", lambda e: e.tensor_tensor(mask[:, :], oh[:, 0, :], oh[:, 1, :], ALU.add), R=ohr, W=["m_mask"])
            sc.op("dve", lambda e: e.tensor_tensor(mask[:, :], mask[:, :], oh[:, 2, :], ALU.add), R=ohr + ["m_mask"], W=["m_mask"])
            sc.op("dve", lambda e: e.tensor_tensor(maskb[:, :], mask[:, :], oh[:, 3, :], ALU.add), R=ohr + ["m_mask"], W=["m_maskb"])
            yield
            sc.op("pe", lambda e: e.matmul(psb(k, pl_)[:, 64:64 + NE], tri[:, :], maskb[:, :], start=True, stop=True),
                  R=["m_maskb", "m_tri"], W=[f"ps{pl_}b"])
            sc.op("pe", lambda e: e.matmul(psb(k, pl_)[:, 128:128 + NE], k.ones_bf[:, :], maskb[:, :], start=True, stop=True),
                  R=["m_maskb", "ones_bf"], W=[f"ps{pl_}c"])
            yield
            sc.op("dve", lambda e: e.tensor_tensor(rank[:, :], psb(k, pl_)[:, 64:64 + NE], base[:, :], ALU.add), R=[f"ps{pl_}b", "m_base"], W=["m_rank"])
            sc.op("dve", lambda e: e.tensor_tensor(base[:, :], psb(k, pl_)[:, 128:128 + NE], base[:, :], ALU.add), R=[f"ps{pl_}c", "m_base", "m_rank"], W=["m_base"])
            sc._record(sc.lastw["m_base"], [f"ps{pl_}"], [])
            sc.op("dve", lambda e: e.tensor_tensor(prod[:, :, :], oh[:, :, :], rank[:, :].unsqueeze(1).broadcast_to([128, 4, NE]), ALU.mult),
                  R=ohr + ["m_rank"], W=["m_prod"])
            sc.op("dve", lambda e: e.reduce_sum(rk[:, :], prod[:, :, :], AX.X), R=["m_prod"], W=["m_rk"])
            sc.op("dve", lambda e: e.tensor_tensor(prod[:, :, :], oh[:, :, :], ecap[:, :].unsqueeze(1).broadcast_to([128, 4, NE]), ALU.mult),
                  R=ohr + ["m_ecap", "m_rk"], W=["m_prod"])
            sc.op("dve", lambda e: e.reduce_sum(ek[:, :], prod[:, :, :], AX.X), R=["m_prod"], W=["m_ek"])
            sc.op("dve", lambda e: e.tensor_single_scalar(vl[:, :], rk[:, :], float(CAP), ALU.is_lt), R=["m_rk"], W=["m_vl"])
            sc.op("dve", lambda e: e.tensor_tensor(sl[:, :], rk[:, :], ek[:, :], ALU.add), R=["m_rk", "m_ek"], W=["m_sl"])
            sc.op("dve", lambda e: e.tensor_scalar_add(sl[:, :], sl[:, :], -BIG), R=["m_sl"], W=["m_sl"])
            sc.op("dve", lambda e: e.tensor_tensor(sl[:, :], sl[:, :], vl[:, :], ALU.mult), R=["m_sl", "m_vl"], W=["m_sl"])
            sc.op("dve", lambda e: e.tensor_scalar_add(rtd[:, 0:4], sl[:, :], BIG), R=["m_sl"], W=["m_rtd0"])
            sc.op("dve", lambda e: e.tensor_copy(k.slots[:, t, :], rtd[:, 0:4]), R=["m_rtd0"], W=[("slots", t)])
            sc.op("dve", lambda e: e.reciprocal(sm[:, 2:3], sm[:, 1:2]), R=["m_sm1"], W=["m_sm2"])
            sc.op("dve", lambda e: e.tensor_scalar(ex[:, :], ex[:, :], sm[:, 2:3], None, ALU.mult), R=["m_ex", "m_sm2"], W=["m_ex"])
            sc.op("dve", lambda e: e.tensor_tensor(k.wts[:, t, :], ex[:, :], vl[:, :], ALU.mult), R=["m_ex", "m_vl"], W=[("wts", t)])
            yield
            if k.dbg:
                sc.op("dve", lambda e: e.tensor_copy(rtd[:, 4:8], k.wts[:, t, :]), R=[("wts", t)], W=["m_rtd1"])
                sc.dma("sp", lambda e: e.dma_start(out=k.rt_d[tk, :], in_=rtd[:, :]), R=["m_rtd0", "m_rtd1"], W=[("dram", "rt", t)])
            for kk in range(4):
                sc.dma("pool", lambda e, kk=kk: e.indirect_dma_start(
                    out=k.xs_d[:, :], out_offset=bass.IndirectOffsetOnAxis(ap=k.slots[:, t, kk:kk + 1], axis=0),
                    in_=h1b[i2][:, :], in_offset=None, bounds_check=sc.bc_reg, oob_is_err=False),
                    R=[("slots", t), T + "h1b"], W=[("dram", "xs", t, kk)])
        run_pipelined((tile(t) for t in range(NT)), depth=2, skew=8)


def phase6(k):
    nc, sc = k.nc, k.sc
    with scope(k) as es:
        sb = lambda name, shape, dt=F32: es.enter_context(nc.sbuf_tensor(name, list(shape), dt))
        w1 = [sb(f"e_w1{i}", [128, 8, 2 * D], BF16) for i in range(2)]
        w2 = [sb(f"e_w2{i}", [128, 8, D], BF16) for i in range(2)]
        bo = [sb(f"e_bo{i}", [128, D]) for i in range(2)]
        bi_ = sb("e_bi", [128, NE * 16])
        sc.dma("sp", lambda e: e.dma_start(out=bi_[:, :], in_=k.b_ein[:, :]), W=["e_bi"])
        bi1 = sb("e_bi1", [128, NE * 16])
        sc.op("dve", lambda e: e.tensor_scalar_add(bi1[:, :], bi_[:, :], 1.0), R=["e_bi"], W=["e_bi1"])
        CH = 3
        NCH = CAPT // CH
        N = CH * 128
        xs = [sb(f"e_xs{i}", [128, CH, D], BF16) for i in range(2)]
        xT = [sb(f"e_xT{i}", [128, 8, N], BF16) for i in range(2)]
        aT = [sb(f"e_aT{i}", [128, 8, N], BF16) for i in range(2)]
        t1 = [sb(f"e_t1{i}", [128, N]) for i in range(2)]
        t2 = [sb(f"e_t2{i}", [128, N]) for i in range(2)]
        sg = [sb(f"e_sg{i}", [128, N]) for i in range(2)]
        ys = [sb(f"e_ys{i}", [128, D]) for i in range(2)]
        pt = [psbf(k, 4), psbf(k, 5)]
        xs_reads = [r for r in sc.lastw if isinstance(r, tuple) and r[0] == "dram" and r[1] == "xs"]
        gchunk = 0

        def load_xs(g):
            s0_ = (g // NCH) * CAP + (g % NCH) * N
            sc.dma("sp", lambda e: e.dma_start(out=xs[g % 2][:, :, :], in_=k.xs_d[s0_:s0_ + N, :].rearrange("(t p) d -> p t d", p=128)),
                   R=xs_reads if g == 0 else [], W=[f"e_xs{g % 2}"])
        for e_ in range(NE):
            wi = e_ % 2
            for hf in range(2):
                sc.dma("pool", lambda e, e_=e_, wi=wi, hf=hf: e.dma_start(
                    out=w1[wi][:, :, hf * D:(hf + 1) * D], in_=k.w_ein[e_, :, hf * D:(hf + 1) * D].rearrange("(c p) n -> p c n", p=128)),
                    W=[f"e_w1{wi}_{hf}"])
            sc.dma("pool", lambda e, e_=e_, wi=wi: e.dma_start(out=w2[wi][:, :, :], in_=k.w_eout[e_, :, :].rearrange("(c p) n -> p c n", p=128)),
                   W=[f"e_w2{wi}"])
            sc.dma("pool", lambda e, e_=e_, wi=wi: e.dma_start(out=bo[wi][:, :], in_=bcast_rows(k.b_eout[e_, :])), W=[f"e_bo{wi}"])
            for ch in range(NCH):
                ci = gchunk % 2
                gchunk += 1
                s0 = e_ * CAP + ch * N
                if gchunk == 1:
                    load_xs(0)
                if gchunk < NE * NCH:
                    load_xs(gchunk)
                for st in range(CH):
                    for c in range(8):
                        sc.op("pe", lambda e, st=st, c=c, ci=ci: e.transpose(pt[st % 2][:, c * 128:(c + 1) * 128], xs[ci][:, st, c * 128:(c + 1) * 128], k.ident[:, :]),
                              R=[f"e_xs{ci}", "ident"], W=[f"ps{4 + st % 2}"])
                    sc.op("dve" if st % 2 == 0 else "act",
                          (lambda e, st=st, ci=ci: e.tensor_copy(xT[ci][:, :, st * 128:(st + 1) * 128], pt[st % 2][:, :].rearrange("p (c n) -> p c n", c=8)))
                          if st % 2 == 0 else
                          (lambda e, st=st, ci=ci: e.copy(xT[ci][:, :, st * 128:(st + 1) * 128], pt[st % 2][:, :].rearrange("p (c n) -> p c n", c=8))),
                          R=[f"ps{4 + st % 2}"], W=[f"e_xT{ci}_{st}"])
                xTr = [f"e_xT{ci}_{st}" for st in range(CH)]
                for j in range(8):
                    ji = j % 2
                    pg, pl = ji, 2 + ji
                    mmacc(k, psb(k, pg)[:, 0:N], [(w1[wi][:, c, j * 128:(j + 1) * 128], xT[ci][:, c, :]) for c in range(8)],
                          R=xTr + [f"e_w1{wi}_0"], W=[f"ps{pg}"])
                    mmacc(k, psb(k, pl)[:, 0:N], [(w1[wi][:, c, D + j * 128:D + (j + 1) * 128], xT[ci][:, c, :]) for c in range(8)],
                          R=xTr + [f"e_w1{wi}_1"], W=[f"ps{pl}"])
                    bg_ = bi_[:, e_ * 16 + j:e_ * 16 + j + 1]
                    bl_ = bi1[:, e_ * 16 + 8 + j:e_ * 16 + 8 + j + 1]
                    sc.op("dve", lambda e, ji=ji, pg=pg, bg_=bg_: e.tensor_scalar(t1[ji][:, :], psb(k, pg)[:, 0:N], bg_, 7.0, ALU.add, ALU.min),
                          R=[f"ps{pg}", "e_bi"], W=[f"e_t1{ji}"])
                    sc.op("act", lambda e, ji=ji: e.activation(out=sg[ji][:, :], in_=t1[ji][:, :], func=AF.Sigmoid, scale=1.702),
                          R=[f"e_t1{ji}"], W=[f"e_sg{ji}"])
                    sc.op("dve", lambda e, ji=ji, pl=pl, bl_=bl_: e.tensor_scalar(t2[ji][:, :], psb(k, pl)[:, 0:N], bl_, 8.0, ALU.add, ALU.min),
                          R=[f"ps{pl}", "e_bi1"], W=[f"e_t2{ji}"])
                    sc.op("dve", lambda e, ji=ji: e.tensor_tensor(t1[ji][:, :], t1[ji][:, :], sg[ji][:, :], ALU.mult),
                          R=[f"e_t1{ji}", f"e_sg{ji}"], W=[f"e_t1{ji}"])
                    sc.op("dve", lambda e, ji=ji, j=j, ci=ci: e.scalar_tensor_tensor(aT[ci][:, j, :], t2[ji][:, :], -6.0, t1[ji][:, :], ALU.max, ALU.mult),
                          R=[f"e_t1{ji}", f"e_t2{ji}"], W=[f"e_aT{ci}_{j}"])
                aTr = [f"e_aT{ci}_{j}" for j in range(8)]
                for st in range(CH):
                    yi = st % 2
                    for half in range(2):
                        pb_ = 6 + half
                        mmacc(k, psb(k, pb_), [(aT[ci][:, j, st * 128:(st + 1) * 128], w2[wi][:, j, half * 512:(half + 1) * 512]) for j in range(8)],
                              R=aTr + [f"e_w2{wi}"], W=[f"ps{pb_}"])
                        sc.op("dve", lambda e, yi=yi, half=half, pb_=pb_, wi=wi: e.tensor_tensor(
                            ys[yi][:, half * 512:(half + 1) * 512], psb(k, pb_), bo[wi][:, half * 512:(half + 1) * 512], ALU.add),
                            R=[f"ps{pb_}", f"e_bo{wi}"], W=[f"e_ys{yi}_{half}"])
                    r0 = s0 + st * 128
                    sc.dma("sp", lambda e, yi=yi, r0=r0: e.dma_start(out=k.ys_d[r0:r0 + 128, :], in_=ys[yi][:, :]),
                           R=[f"e_ys{yi}_0", f"e_ys{yi}_1"], W=[("dram", "ys", r0)])


def phase7(k):
    nc, sc = k.nc, k.sc
    with scope(k) as es:
        sb = lambda name, shape, dt=F32: es.enter_context(nc.sbuf_tensor(name, list(shape), dt))
        g2 = sb("f_g2", [128, D])
        b2 = sb("f_b2", [128, D])
        sc.dma("sp", lambda e: e.dma_start(out=g2[:, :], in_=bcast_rows(k.ln2_g[0, :])), W=["lnconst"])
        sc.dma("sp", lambda e: e.dma_start(out=b2[:, :], in_=bcast_rows(k.ln2_b[0, :])), W=["lnconst"])
        NB2 = 3
        h1 = [sb(f"f_h1{i}", [128, D]) for i in range(NB2)]
        yk = [[sb(f"f_yk{i}_{kk}", [128, D]) for kk in range(4)] for i in range(NB2)]
        acc = [sb(f"f_acc{i}", [128, D]) for i in range(NB2)]
        out = [sb(f"f_out{i}", [128, D]) for i in range(NB2)]
        scr = [dict(st=sb(f"f_st{i}", [128, 12]), mv=sb(f"f_mv{i}", [128, 2]), rs=sb(f"f_rs{i}", [128, 2])) for i in range(NB2)]
        ys_w = [r for r in sc.lastw if isinstance(r, tuple) and r[0] == "dram" and r[1] == "ys"]
        for i in range(NB2):
            for kk in range(4):
                sc.op("pool", lambda e, i=i, kk=kk: e.memset(yk[i][kk][:, :], 0.0), W=[f"f_yk{i}_{kk}"])
        NB2 = 3

        def tile(t):
            i2 = t % NB2
            tk = slice(t * 128, (t + 1) * 128)
            tag = f"f_{i2}_"
            sc.dma("sp", lambda e: e.dma_start(out=h1[i2][:, :], in_=k.h1_d[tk, :]), R=[("dram", "h1", t)], W=[f"f_h1{i2}"])
            for kk in range(4):
                sc.dma("pool", lambda e, kk=kk: e.indirect_dma_start(
                    out=yk[i2][kk][:, :], out_offset=None, in_=k.ys_d[:, :],
                    in_offset=bass.IndirectOffsetOnAxis(ap=k.slots[:, t, kk:kk + 1], axis=0),
                    bounds_check=sc.bc_reg, oob_is_err=False),
                    R=(ys_w if t == 0 else []) + [("slots", t)], W=[f"f_yk{i2}_{kk}"])
            yield
            sc.op("dve", lambda e: e.scalar_tensor_tensor(acc[i2][:, :], yk[i2][0][:, :], k.wts[:, t, 0:1], h1[i2][:, :], ALU.mult, ALU.add),
                  R=[f"f_yk{i2}_0", ("wts", t), f"f_h1{i2}"], W=[tag + "z"])
            sc.op("dve", lambda e: e.scalar_tensor_tensor(acc[i2][:, :], h1[i2][:, :], ALPHA - 1.0, acc[i2][:, :], ALU.mult, ALU.add),
                  R=[tag + "z", f"f_h1{i2}"], W=[tag + "z"])
            for kk in range(1, 4):
                sc.op("dve", lambda e, kk=kk: e.scalar_tensor_tensor(acc[i2][:, :], yk[i2][kk][:, :], k.wts[:, t, kk:kk + 1], acc[i2][:, :], ALU.mult, ALU.add),
                      R=[f"f_yk{i2}_{kk}", ("wts", t), tag + "z"], W=[tag + "z"])
            yield
            for _ in layer_norm_tile(k, tag, acc[i2], g2, b2, out[i2], scr[i2], stages=True):
                yield
            sc.dma("sp", lambda e: e.dma_start(out=k.y[tk, :], in_=out[i2][:, :]), R=[tag + "o"], W=[("y", t)])
        run_pipelined((tile(t) for t in range(NT)), depth=3, skew=2)


def host_consts():
    c = {}
    c["c_ident"] = np.eye(128, dtype=np.float32).astype(ml_dtypes.bfloat16)
    c["c_identf"] = np.eye(128, dtype=np.float32)
    t = np.arange(S)
    row, col = t // 64, t % 64

    def tab(half):
        inv = (10000.0 ** (-np.arange(half, dtype=np.float32) / half)).astype(np.float32)
        ar = row.astype(np.float32)[:, None] * inv[None, :]
        ac = col.astype(np.float32)[:, None] * inv[None, :]
        cs = np.concatenate([np.cos(ar), np.cos(ar), np.cos(ac), np.cos(ac)], axis=1)
        sn = np.concatenate([-np.sin(ar), np.sin(ar), -np.sin(ac), np.sin(ac)], axis=1)
        return cs.astype(np.float32), sn.astype(np.float32)
    c64, s64 = tab(16)
    r64 = np.stack([c64, s64], 0).reshape(2, NT, 128, 64).transpose(2, 0, 1, 3)
    c["c_rope64"] = np.ascontiguousarray(r64)
    c32, s32 = tab(8)
    rT = np.stack([c32.T, s32.T], 0)
    rT = np.tile(rT[None], (4, 1, 1, 1)).transpose(0, 2, 1, 3).reshape(128, 2, S)
    c["c_ropeT"] = np.ascontiguousarray(rT)
    sel = np.zeros((128, 128), np.float32)
    sel[64, :] = 1.0
    c["c_sel"] = sel
    c["c_tri"] = np.triu(np.ones((128, 128), np.float32), 1).astype(ml_dtypes.bfloat16)
    c["c_ecap"] = np.tile((np.arange(NE, dtype=np.float32) * CAP)[None, :], (128, 1))
    return c


def swap_perm(n_heads, hd, rope_off, rope_dim):
    perm = np.arange(n_heads * hd)
    q = rope_dim // 4
    for h in range(n_heads):
        b = h * hd + rope_off
        for blk in range(2):
            o = b + blk * 2 * q
            perm[o:o + q] = np.arange(o + q, o + 2 * q)
            perm[o + q:o + 2 * q] = np.arange(o, o + q)
    return perm


_CACHE = {}


def kernel(**inputs):
    upto = inputs.pop("_upto", 99)
    dbg = tuple(inputs.pop("_dbg", ()))
    ncores = inputs.pop("_ncores", 8)
    key = (upto, dbg)
    if key not in _CACHE:
        _CACHE[key] = build(upto, dbg)
    nc, kk = _CACHE[key]
    f = lambda a: np.ascontiguousarray(np.asarray(a, dtype=np.float32))
    shared = {}
    shared["ln_in_g"] = f(inputs["ln_in_g"]).reshape(1, D)
    shared["ln_in_b"] = f(inputs["ln_in_b"]).reshape(1, D)
    shared["w_in_proj"] = f(inputs["w_in_proj"][0])
    shared["w_kr_sw"] = np.ascontiguousarray(shared["w_in_proj"][:, OFF_KR:OFF_KR + 32][:, swap_perm(1, 32, 0, 32)])
    shared["b_gate"] = f(inputs["b_gate"][0]).reshape(1, 3 * D)
    shared["gqa_q_norm"] = np.tile(f(inputs["gqa_q_norm"][0]), 8).reshape(1, 512)
    shared["gqa_k_norm"] = np.tile(f(inputs["gqa_k_norm"][0]), 2).reshape(1, 128)
    shared["mla_q_norm"] = f(inputs["mla_q_norm"][0]).reshape(1, 384)
    shared["mla_kv_norm"] = f(inputs["mla_kv_norm"][0]).reshape(1, 256)
    wqb = f(inputs["w_mla_qb"][0])
    shared["w_mla_qb"] = wqb
    shared["w_mla_qb_sw"] = np.ascontiguousarray(wqb[:, swap_perm(8, 96, 64, 32)])
    shared["w_mla_kvb"] = f(inputs["w_mla_kvb"][0])
    shared["w_mem_kv"] = f(inputs["w_mem_kv"][0])
    shared["w_br_gqa"] = f(inputs["w_br_gqa"][0])
    shared["w_br_mla"] = f(inputs["w_br_mla"][0])
    shared["w_br_mem"] = f(inputs["w_br_mem"][0])
    shared["w_out"] = f(inputs["w_out"][0])
    shared["ln1_g"] = f(inputs["ln1_g"][0]).reshape(1, D)
    shared["ln1_b"] = f(inputs["ln1_b"][0]).reshape(1, D)
    shared["w_router"] = f(inputs["w_router"][0])
    shared["b_router"] = f(inputs["b_router"][0]).reshape(1, NE)
    shared["w_exp_in"] = f(inputs["w_exp_in"][0])
    shared["b_exp_in"] = np.ascontiguousarray(
        f(inputs["b_exp_in"][0]).reshape(NE, 16, 128).transpose(2, 0, 1).reshape(128, NE * 16))
    shared["w_exp_out"] = f(inputs["w_exp_out"][0])
    shared["b_exp_out"] = f(inputs["b_exp_out"][0])
    shared["ln2_g"] = f(inputs["ln2_g"][0]).reshape(1, D)
    shared["ln2_b"] = f(inputs["ln2_b"][0]).reshape(1, D)
    shared.update(host_consts())
    x = f(inputs["x"])
    mem = f(inputs["mem"])
    in_maps = []
    for c in range(ncores):
        m = {n: shared[n] for n in kk.inputs if n in shared}
        m["x"] = x[c]
        if "mem" in kk.inputs:
            m["mem"] = mem[c]
        in_maps.append(m)
    res = run_bass_kernel_spmd(nc, in_maps, core_ids=list(range(ncores)))
    if dbg:
        return res.results
    return np.stack([r["y"] for r in res.results], axis=0)
```

```python
import numpy as np
import ml_dtypes
from contextlib import ExitStack, contextmanager

import concourse.bass as bass
import concourse.mybir as mybir
from concourse.bass_utils import run_bass_kernel_spmd

F32 = mybir.dt.float32
BF16 = mybir.dt.bfloat16
I32 = mybir.dt.int32
U32 = mybir.dt.uint32
ALU = mybir.AluOpType
AF = mybir.ActivationFunctionType
AX = mybir.AxisListType

S = 4096
D = 1024
NT = S // 128
NB = S // 512
MEM = 256
NE = 32
TOPK = 4
CAPT = 6
CAP = CAPT * 128
NSLOT = NE * CAP
ALPHA = 2.0 ** 0.25
RMS_EPS = 1e-6
LN_EPS = 1e-5
OFF_QA, OFF_KA, OFF_VA, OFF_QL, OFF_KVL, OFF_KR, OFF_QM, OFF_G = 0, 512, 640, 768, 1152, 1408, 1440, 1952
INW = 5024

ENGS = ("pe", "act", "dve", "pool", "sp")


class Sched:
    EPOCH = 30000

    def __init__(self, nc, es, ndma=None):
        self.nc, self.es = nc, es
        self.prog = {e: [] for e in ENGS}
        self.esem = {e: es.enter_context(nc.semaphore(f"e_{e}_0")) for e in ENGS}
        self.eold = {e: set() for e in ENGS}
        self.nep = {e: 0 for e in ENGS}
        self.ecnt = {e: 0 for e in ENGS}
        self.seen = {e: {} for e in ENGS}
        self.lastw, self.readers = {}, {}
        ndma = ndma or {"sp": 28, "pool": 24, "act": 6}
        self.dsem = {q: [es.enter_context(nc.semaphore(f"d_{q}_{i}")) for i in range(n)] for q, n in ndma.items()}
        self.duse = {q: [0] * n for q, n in ndma.items()}
        self.dnext = {q: 0 for q in ndma}
        self.ninstr = 0

    def _deps(self, e, R, W):
        deps = {}

        def add(tok):
            if tok is None:
                return
            s, v = tok
            if deps.get(id(s), (None, 0))[1] < v:
                deps[id(s)] = (s, v)
        for r in R:
            add(self.lastw.get(r))
        for w in W:
            add(self.lastw.get(w))
            for tok in self.readers.get(w, {}).values():
                add(tok)
        waits = []
        for k, (s, v) in deps.items():
            if id(s) in self.eold[e]:
                continue
            if s is self.esem[e] and e == "pe":
                continue
            if self.seen[e].get(k, 0) >= v:
                continue
            self.seen[e][k] = v
            waits.append((s, v))
        return waits

    def _record(self, tok, R, W):
        for w in W:
            self.lastw[w] = tok
            self.readers[w] = {}
        for r in R:
            d = self.readers.setdefault(r, {})
            if d.get(id(tok[0]), (None, 0))[1] < tok[1]:
                d[id(tok[0])] = tok

    def op(self, e, fn, R=(), W=()):
        waits = self._deps(e, R, W)
        if self.ecnt[e] >= self.EPOCH:
            self.eold[e].add(id(self.esem[e]))
            self.nep[e] += 1
            self.esem[e] = self.es.enter_context(self.nc.semaphore(f"e_{e}_{self.nep[e]}"))
            self.ecnt[e] = 0
        self.ecnt[e] += 1
        sem = self.esem[e]
        tok = (sem, self.ecnt[e])

        def emit(eng, waits=waits, fn=fn, sem=sem):
            for s, v in waits:
                eng.wait_ge(s, v)
            fn(eng).then_inc(sem, 1)
        self.prog[e].append(emit)
        self._record(tok, R, W)
        self.ninstr += 1 + len(waits)
        return tok

    def dma(self, q, fn, R=(), W=()):
        waits = self._deps(q, R, W)
        i = self.dnext[q]
        self.dnext[q] = (i + 1) % len(self.dsem[q])
        sem = self.dsem[q][i]
        prev = self.duse[q][i]
        if prev and self.seen[q].get(id(sem), 0) < 16 * prev:
            self.seen[q][id(sem)] = 16 * prev
            waits.append((sem, 16 * prev))
        self.duse[q][i] = prev + 1
        tok = (sem, 16 * (prev + 1))

        def emit(eng, waits=waits, fn=fn, sem=sem):
            for s, v in waits:
                eng.wait_ge(s, v)
            fn(eng).then_inc(sem, 16)
        self.prog[q].append(emit)
        self._record(tok, R, W)
        self.ninstr += 1 + len(waits)
        return tok

    def barrier(self):
        toks = [(self.esem[e], self.ecnt[e]) for e in ENGS if self.ecnt[e] > 0]
        for q in self.dsem:
            for i, s in enumerate(self.dsem[q]):
                if self.duse[q][i]:
                    toks.append((s, 16 * self.duse[q][i]))
        for e in ENGS:
            waits = [(s, v) for (s, v) in toks if self.seen[e].get(id(s), 0) < v]
            for s, v in waits:
                self.seen[e][id(s)] = v

            def emit(eng, waits=waits):
                for s, v in waits:
                    eng.wait_ge(s, v)
            self.prog[e].append(emit)
            self.ninstr += len(waits)

    def fence(self, e, toks):
        best = {}
        for s, v in toks:
            if best.get(id(s), (None, 0))[1] < v:
                best[id(s)] = (s, v)

        def emit(eng, best=best):
            for s, v in best.values():
                eng.wait_ge(s, v)
        self.prog[e].append(emit)

    def run(self):
        nc = self.nc
        with nc.Block() as block:
            @block.tensor
            def _(eng):
                for f in self.prog["pe"]:
                    f(eng)

            @block.scalar
            def _(eng):
                for f in self.prog["act"]:
                    f(eng)

            @block.vector
            def _(eng):
                for f in self.prog["dve"]:
                    f(eng)

            @block.gpsimd
            def _(eng):
                self.bc_reg = eng.to_reg(NSLOT - 1)
                for f in self.prog["pool"]:
                    f(eng)

            @block.sync
            def _(eng):
                for f in self.prog["sp"]:
                    f(eng)


@contextmanager
def scope(k):
    with ExitStack() as es:
        yield es
        k.sc.barrier()


def run_pipelined(gens, depth, skew=1):
    gens = iter(gens)
    active, rnd, done = [], 0, False
    while True:
        if not done and len(active) < depth and rnd % skew == 0:
            g = next(gens, None)
            if g is None:
                done = True
            else:
                active.append(g)
        if done and not active:
            break
        for g in list(active):
            try:
                next(g)
            except StopIteration:
                active.remove(g)
        rnd += 1


def bcast_rows(ap, n=128):
    return ap.partition_broadcast(n)


class K:
    pass


def build(upto=99, dbg=()):
    nc = bass.Bass("TRN2", target_bir_lowering=False)
    k = K()
    k.nc = nc
    k.inputs = []
    k.dbg = dbg

    def dt_in(name, shape, dt=F32):
        k.inputs.append(name)
        return nc.dram_tensor(name, list(shape), dt, kind="ExternalInput")
    k.x = dt_in("x", [S, D])
    k.ln_in_g = dt_in("ln_in_g", [1, D])
    k.ln_in_b = dt_in("ln_in_b", [1, D])
    k.c_ident = dt_in("c_ident", [128, 128], BF16)
    k.c_identf = dt_in("c_identf", [128, 128])
    if upto >= 2:
        k.mem = dt_in("mem", [MEM, D])
        k.w_in = dt_in("w_in_proj", [D, INW])
        k.w_kr_sw = dt_in("w_kr_sw", [D, 32])
        k.gqa_qn = dt_in("gqa_q_norm", [1, 512])
        k.gqa_kn = dt_in("gqa_k_norm", [1, 128])
        k.mla_qn = dt_in("mla_q_norm", [1, 384])
        k.mla_kvn = dt_in("mla_kv_norm", [1, 256])
        k.w_qb = dt_in("w_mla_qb", [384, 768])
        k.w_qb_sw = dt_in("w_mla_qb_sw", [384, 768])
        k.w_kvb = dt_in("w_mla_kvb", [256, 1024])
        k.w_mkv = dt_in("w_mem_kv", [D, D])
        k.c_rope64 = dt_in("c_rope64", [128, 2, NT, 64])
        k.c_ropeT = dt_in("c_ropeT", [128, 2, S])
        k.c_sel = dt_in("c_sel", [128, 128])
    if upto >= 5:
        k.b_gate = dt_in("b_gate", [1, 3 * D])
        k.w_br_a = dt_in("w_br_gqa", [512, D])
        k.w_br_b = dt_in("w_br_mla", [512, D])
        k.w_br_c = dt_in("w_br_mem", [512, D])
        k.w_out = dt_in("w_out", [D, D])
        k.ln1_g = dt_in("ln1_g", [1, D])
        k.ln1_b = dt_in("ln1_b", [1, D])
        k.w_router = dt_in("w_router", [D, NE])
        k.b_router = dt_in("b_router", [1, NE])
        k.c_tri = dt_in("c_tri", [128, 128], BF16)
        k.c_ecap = dt_in("c_ecap", [128, NE])
    if upto >= 6:
        k.w_ein = dt_in("w_exp_in", [NE, D, 2 * D])
        k.b_ein = dt_in("b_exp_in", [128, NE * 16])
        k.w_eout = dt_in("w_exp_out", [NE, D, D])
        k.b_eout = dt_in("b_exp_out", [NE, D])
        k.ln2_g = dt_in("ln2_g", [1, D])
        k.ln2_b = dt_in("ln2_b", [1, D])
    k.y = nc.dram_tensor("y", [S, D], F32, kind="ExternalOutput")
    sk = lambda name: ("ExternalOutput" if name in dbg else "Internal")
    k.h0_d = nc.dram_tensor("h0_d", [S, D], F32, kind=sk("h0_d"))
    k.h0T_d = nc.dram_tensor("h0T_d", [NT, 128, D], BF16, kind=sk("h0T_d"))
    k.oA_d = nc.dram_tensor("oA_d", [8, 64, S], BF16, kind=sk("oA_d"))
    k.oB_d = nc.dram_tensor("oB_d", [8, 64, S], BF16, kind=sk("oB_d"))
    k.oC_d = nc.dram_tensor("oC_d", [4, 128, S], BF16, kind=sk("oC_d"))
    k.h1_d = nc.dram_tensor("h1_d", [S, D], F32, kind=sk("h1_d"))
    k.xs_d = nc.dram_tensor("xs_d", [NSLOT, D], BF16, kind=sk("xs_d"))
    k.ys_d = nc.dram_tensor("ys_d", [NSLOT, D], F32, kind=sk("ys_d"))
    k.rt_d = nc.dram_tensor("rt_d", [S, 8], F32, kind=sk("rt_d"))

    with ExitStack() as es:
        sc = Sched(nc, es)
        k.sc = sc
        k.pp = [es.enter_context(nc.psum_tensor(f"pp{i}", [128, 1024], F32)) for i in range(4)]
        k.ppb = [p.bitcast(BF16) for p in k.pp]
        sb = lambda name, shape, dt=F32: es.enter_context(nc.sbuf_tensor(name, list(shape), dt))
        k.ident = sb("ident", [128, 128], BF16)
        k.identf = sb("identf", [128, 128], F32)
        k.neghalf = sb("neghalf", [128, 8], F32)
        k.ones_bf = sb("ones_bf", [128, 128], BF16)
        k.ones_f = sb("ones_f", [128, 128], F32)
        k.slots = sb("slots_all", [128, NT, 4], I32)
        k.wts = sb("wts_all", [128, NT, 4], F32)
        sc.dma("sp", lambda e: e.dma_start(out=k.ident[:, :], in_=k.c_ident[:, :]), W=["ident"])
        sc.dma("sp", lambda e: e.dma_start(out=k.identf[:, :], in_=k.c_identf[:, :]), W=["identf"])
        if upto >= 2:
            k.sel = sb("sel", [128, 128], F32)
            sc.dma("sp", lambda e: e.dma_start(out=k.sel[:, :], in_=k.c_sel[:, :]), W=["sel"])
        sc.op("pool", lambda e: e.memset(k.neghalf[:, :], -0.5), W=["neghalf"])
        sc.op("pool", lambda e: e.memset(k.ones_bf[:, :], 1.0), W=["ones_bf"])
        sc.op("pool", lambda e: e.memset(k.ones_f[:, :], 1.0), W=["ones_f"])

        phase1(k)
        if upto >= 2:
            phase2(k)
        if upto >= 3:
            phase3(k)
        if upto >= 4:
            phase4(k)
        if upto >= 5:
            phase5(k)
        if upto >= 6:
            phase6(k)
            phase7(k)
        toks = [t for r, t in sc.lastw.items() if isinstance(r, tuple) and r[0] in ("y", "dram")]
        sc.fence("sp", toks)
        sc.run()
    k.ninstr = sc.ninstr
    return nc, k


def psb(k, i):
    return k.pp[i // 2][:, (i % 2) * 512:(i % 2 + 1) * 512]


def psbf(k, i):
    return k.ppb[i // 2][:, (i % 2) * 1024:(i % 2 + 1) * 1024]


def layer_norm_tile(k, tag, z, g_rep, b_rep, out, scr, stages=False):
    g = _ln_gen(k, tag, z, g_rep, b_rep, out, scr)
    if stages:
        return g
    for _ in g:
        pass


def _ln_gen(k, tag, z, g_rep, b_rep, out, scr):
    sc = k.sc
    st, mv, rs = scr["st"], scr["mv"], scr["rs"]
    for h in range(2):
        sc.op("dve", lambda e, h=h: e.bn_stats(st[:, h * 6:(h + 1) * 6], z[:, h * 512:(h + 1) * 512]),
              R=[tag + "z"], W=[tag + f"st{h}"])
    sc.op("dve", lambda e: e.bn_aggr(mv[:, 0:2], st[:, 0:12]), R=[tag + "st0", tag + "st1"], W=[tag + "mv"])
    sc.op("dve", lambda e: e.tensor_scalar_add(rs[:, 0:1], mv[:, 1:2], LN_EPS), R=[tag + "mv"], W=[tag + "rs0"])
    yield
    sc.op("pool", lambda e: e.tensor_tensor(rs[:, 1:2], rs[:, 0:1], k.neghalf[:, 0:1], ALU.pow),
          R=[tag + "rs0", "neghalf"], W=[tag + "rs"])
    yield
    sc.op("dve", lambda e: e.tensor_scalar(out[:, :], z[:, :], mv[:, 0:1], rs[:, 1:2], ALU.subtract, ALU.mult),
          R=[tag + "z", tag + "mv", tag + "rs"], W=[tag + "o"])
    sc.op("dve", lambda e: e.tensor_tensor(out[:, :], out[:, :], g_rep[:, :], ALU.mult),
          R=[tag + "o", "lnconst"], W=[tag + "o"])
    sc.op("dve", lambda e: e.tensor_tensor(out[:, :], out[:, :], b_rep[:, :], ALU.add),
          R=[tag + "o", "lnconst"], W=[tag + "o"])
    yield


def phase1(k):
    nc, sc = k.nc, k.sc
    with scope(k) as es:
        sb = lambda name, shape, dt=F32: es.enter_context(nc.sbuf_tensor(name, list(shape), dt))
        g_rep = sb("p1_g", [128, D])
        b_rep = sb("p1_b", [128, D])
        sc.dma("sp", lambda e: e.dma_start(out=g_rep[:, :], in_=bcast_rows(k.ln_in_g[0, :])), W=["lnconst"])
        sc.dma("sp", lambda e: e.dma_start(out=b_rep[:, :], in_=bcast_rows(k.ln_in_b[0, :])), W=["lnconst"])
        NBUF = 3
        xt = [sb(f"p1_x{i}", [128, D]) for i in range(NBUF)]
        ht = [sb(f"p1_h{i}", [128, D]) for i in range(NBUF)]
        hb = [sb(f"p1_hb{i}", [128, D], BF16) for i in range(NBUF)]
        hT = [sb(f"p1_hT{i}", [128, D], BF16) for i in range(NBUF)]
        scr = [dict(st=sb(f"p1_st{i}", [128, 12]), mv=sb(f"p1_mv{i}", [128, 2]), rs=sb(f"p1_rs{i}", [128, 2]))
               for i in range(NBUF)]
        def tile(t):
            i = t % NBUF
            tag = f"p1_{i}_"
            sc.dma("sp", lambda e: e.dma_start(out=xt[i][:, :], in_=k.x[t * 128:(t + 1) * 128, :]), W=[tag + "z"])
            yield
            for _ in layer_norm_tile(k, tag, xt[i], g_rep, b_rep, ht[i], scr[i], stages=True):
                yield
            sc.dma("sp", lambda e: e.dma_start(out=k.h0_d[t * 128:(t + 1) * 128, :], in_=ht[i][:, :]),
                   R=[tag + "o"], W=[("dram", "h0", t)])
            sc.op("act", lambda e: e.copy(hb[i][:, :], ht[i][:, :]), R=[tag + "o"], W=[tag + "hb"])
            yield
            pst = psbf(k, t % 2)
            for c in range(8):
                sc.op("pe", lambda e, c=c: e.transpose(pst[:, c * 128:(c + 1) * 128], hb[i][:, c * 128:(c + 1) * 128], k.ident[:, :]),
                      R=[tag + "hb", "ident"], W=[f"ps{t % 2}"])
            yield
            sc.op("dve", lambda e: e.tensor_copy(hT[i][:, :], pst[:, :]), R=[f"ps{t % 2}"], W=[tag + "hT"])
            yield
            sc.dma("sp", lambda e: e.dma_start(out=k.h0T_d[t, :, :], in_=hT[i][:, :]), R=[tag + "hT"], W=[("dram", "h0T", t)])
        run_pipelined((tile(t) for t in range(NT)), depth=3, skew=3)


def mmacc(k, out, pairs, R, W):
    n = len(pairs)
    for i, (l, r) in enumerate(pairs):
        k.sc.op("pe", lambda e, l=l, r=r, i=i: e.matmul(out, l, r, start=(i == 0), stop=(i == n - 1)), R=R, W=W)


def attention(k, tag, units, nkt, qk_fn, v_fn, dv, scale, out_fn, aug, Rres, nh):
    nc, sc = k.nc, k.sc
    W = 512 * nh
    with scope(k) as es:
        sb = lambda name, shape, dt=F32: es.enter_context(nc.sbuf_tensor(name, list(shape), dt))
        pt = [sb(f"{tag}_pt{i}", [128, 1024], BF16) for i in range(2)]
        osb = [sb(f"{tag}_osb{i}", [128, W]) for i in range(2)]
        rec = [sb(f"{tag}_rec{i}", [128, W]) for i in range(2)]
        onb = [sb(f"{tag}_onb{i}", [128, W], BF16) for i in range(2)]
        den = [sb(f"{tag}_den{i}", [1, 512]) for i in range(2)] if not aug else None
        G = nkt // 2 if nh == 1 else nkt
        gs = [(ui, g) for ui in range(len(units)) for g in range(G)]
        M = dv + 1 if aug else dv
        slot_of = {}
        pending = []

        def kts(g):
            return (2 * g, 2 * g + 1) if nh == 1 else (g, g)

        def qk(idx):
            ui, g = gs[idx]
            u = units[ui]
            sl = idx % 2
            slot_of[idx] = sl
            for i in range(2):
                l, r = qk_fn(u, kts(g)[i], i)
                sc.op("pe", lambda e, l=l, r=r, b=2 * sl + i: e.matmul(psb(k, b), l, r, start=True, stop=True),
                      R=Rres, W=[f"ps{2 * sl + i}"])
            sc.op("act", lambda e, sl=sl: e.activation(out=pt[sl][:, :], in_=k.pp[sl][:, :], func=AF.Exp, scale=scale),
                  R=[f"ps{2 * sl}", f"ps{2 * sl + 1}"], W=[f"{tag}_pt{sl}"])

        def accb(ui, i):
            return 4 + ui % 2 if nh == 1 else 4 + i

        def epi2(ui, I):
            u = units[ui]
            s2 = ui % 2
            if nh == 2:
                bcb, bc_ap = [6, 7], k.pp[3][0:dv, 0:W]
            elif aug:
                bcb, bc_ap = [6 + ui % 2], psb(k, 6 + ui % 2)[0:dv, :]
            else:
                sl = (I + 1) % 2
                bcb, bc_ap = [2 * sl], k.pp[sl][0:dv, 0:W]
            for i in range(nh):
                b = bcb[i]
                if aug:
                    sc.op("pe", lambda e, b=b, i=i: e.matmul(psb(k, b)[0:dv, :], k.sel[0:M, 0:dv], osb[s2][0:M, i * 512:(i + 1) * 512], start=True, stop=True),
                          R=[f"{tag}_osb{s2}", "sel"], W=[f"ps{b}"])
                else:
                    sc.op("pe", lambda e, b=b: e.matmul(psb(k, b)[0:dv, :], k.ones_f[0:1, 0:dv], den[s2][0:1, :], start=True, stop=True),
                          R=[f"{tag}_den{s2}", "ones_f"], W=[f"ps{b}"])
            sc.op("dve", lambda e: e.reciprocal(rec[s2][0:dv, :], bc_ap),
                  R=[f"ps{b}" for b in bcb], W=[f"{tag}_rec{s2}"])
            sc.op("dve", lambda e: e.tensor_tensor(onb[s2][0:dv, :], osb[s2][0:dv, :], rec[s2][0:dv, :], ALU.mult),
                  R=[f"{tag}_osb{s2}", f"{tag}_rec{s2}"], W=[f"{tag}_onb{s2}"])
            src = onb[s2][0:dv, :] if nh == 1 else onb[s2][0:dv, :].rearrange("p (h t) -> p h t", h=2)
            sc.dma("sp", lambda e, src=src: e.dma_start(out=out_fn(u), in_=src),
                   R=[f"{tag}_onb{s2}"], W=[("dram", tag, u[0], u[1])])

        def pv(idx):
            ui, g = gs[idx]
            u = units[ui]
            sl = slot_of.pop(idx)
            s2 = ui % 2
            for i in range(2):
                kt = kts(g)[i]
                ob = accb(ui, i)
                first = (kt == 0) if nh == 2 else (kt == 0)
                last = (kt == nkt - 1)
                vv = v_fn(u, kt, i)
                sc.op("pe", lambda e, ob=ob, vv=vv, i=i, first=first, last=last: e.matmul(
                    psb(k, ob)[0:vv.shape[1], :], vv, pt[sl][:, i * 512:(i + 1) * 512], start=first, stop=last),
                    R=[f"{tag}_pt{sl}"] + Rres, W=[f"ps{ob}"])
                if not aug:
                    sc.op("pe", lambda e, kt=kt, i=i, first=first, last=last: e.matmul(
                        psb(k, 6 + ui % 2)[0:1, :], k.ones_bf[:, 0:1], pt[sl][:, i * 512:(i + 1) * 512], start=first, stop=last),
                        R=[f"{tag}_pt{sl}"], W=[f"ps{6 + ui % 2}"])
            if g == G - 1:
                if nh == 1:
                    sc.op("dve", lambda e: e.tensor_copy(osb[s2][0:M, :], psb(k, accb(ui, 0))[0:M, :]),
                          R=[f"ps{accb(ui, 0)}"], W=[f"{tag}_osb{s2}"])
                else:
                    sc.op("dve", lambda e: e.tensor_copy(osb[s2][0:M, :], k.pp[2][0:M, :]),
                          R=[f"ps{accb(ui, 0)}", f"ps{accb(ui, 1)}"], W=[f"{tag}_osb{s2}"])
                if not aug:
                    sc.op("dve", lambda e: e.tensor_copy(den[s2][0:1, :], psb(k, 6 + ui % 2)[0:1, :]),
                          R=[f"ps{6 + ui % 2}"], W=[f"{tag}_den{s2}"])
                pending.append((idx + 2, ui))

        n = len(gs)
        for idx in range(n + 1):
            if idx < n:
                qk(idx)
            while pending and pending[0][0] <= idx:
                epi2(pending.pop(0)[1], idx)
            if idx >= 1:
                pv(idx - 1)
        while pending:
            epi2(pending.pop(0)[1], n)


def rms_rope_tm(k, tag, src, srcres, nh, gain_rep, r64, t, scr, out_bf):
    sc = k.sc
    w = nh * 64
    sq, ss, r, qn, t1, t2 = scr["sq"], scr["ss"], scr["r"], scr["qn"], scr["t1"], scr["t2"]
    v3 = lambda ap: ap.rearrange("p (h d) -> p h d", h=nh)
    v5 = lambda ap: ap.rearrange("p (h b two j) -> p h b two j", h=nh, b=2, two=2, j=16)
    Ct = r64[:, 0, t, :].unsqueeze(1).broadcast_to([128, nh, 64])
    St = r64[:, 1, t, :].rearrange("p (b two j) -> p b two j", b=2, two=2, j=16)
    sc.op("act", lambda e: e.activation(out=sq[:, 0:w], in_=src, func=AF.Square), R=[srcres], W=[tag + "sq"])
    yield
    sc.op("dve", lambda e: e.reduce_sum(ss[:, 0:nh], v3(sq[:, 0:w]), AX.X), R=[tag + "sq"], W=[tag + "ss"])
    sc.op("dve", lambda e: e.tensor_scalar(ss[:, 0:nh], ss[:, 0:nh], 1.0 / 64, RMS_EPS, ALU.mult, ALU.add),
          R=[tag + "ss"], W=[tag + "ss"])
    yield
    sc.op("pool", lambda e: e.tensor_tensor(r[:, 0:nh], ss[:, 0:nh], k.neghalf[:, 0:nh], ALU.pow),
          R=[tag + "ss", "neghalf"], W=[tag + "r"])
    yield
    sc.op("dve", lambda e: e.tensor_tensor(v3(qn[:, 0:w]), v3(src), r[:, 0:nh].unsqueeze(2).broadcast_to([128, nh, 64]), ALU.mult),
          R=[srcres, tag + "r"], W=[tag + "qn"])
    sc.op("dve", lambda e: e.tensor_tensor(qn[:, 0:w], qn[:, 0:w], gain_rep[:, 0:w], ALU.mult),
          R=[tag + "qn", "gains"], W=[tag + "qn"])
    sc.op("dve", lambda e: e.tensor_tensor(v3(t1[:, 0:w]), v3(qn[:, 0:w]), Ct, ALU.mult),
          R=[tag + "qn", "r64"], W=[tag + "t1"])
    for half in range(2):
        sc.op("dve", lambda e, half=half: e.tensor_tensor(
            v5(t2[:, 0:w])[:, :, :, half, :], v5(qn[:, 0:w])[:, :, :, 1 - half, :],
            St[:, :, half, :].unsqueeze(1).broadcast_to([128, nh, 2, 16]), ALU.mult),
            R=[tag + "qn", "r64"], W=[tag + f"t2{half}"])
    sc.op("dve", lambda e: e.tensor_tensor(out_bf, t1[:, 0:w], t2[:, 0:w], ALU.add),
          R=[tag + "t1", tag + "t20", tag + "t21"], W=[tag + "out"])
    yield


def phase2(k):
    nc, sc = k.nc, k.sc
    with scope(k) as es:
        sb = lambda name, shape, dt=F32: es.enter_context(nc.sbuf_tensor(name, list(shape), dt))
        qT = sb("a_qT", [128, 4, S], BF16)
        kd = sb("a_kd", [128, 2, S], BF16)
        va = sb("a_v", [128, NT, 2, 128], BF16)
        with scope(k) as es2:
            sb2 = lambda name, shape, dt=F32: es2.enter_context(nc.sbuf_tensor(name, list(shape), dt))
            wA = sb2("a_w", [128, 8, 768], BF16)
            sc.dma("pool", lambda e: e.dma_start(out=wA[:, :, :], in_=k.w_in[:, 0:768].rearrange("(c p) n -> p c n", p=128)),
                   W=["a_w"])
            gq = sb2("a_gq", [128, 512])
            gk = sb2("a_gk", [128, 128])
            r64 = sb2("a_r64", [128, 2, NT, 64])
            sc.dma("sp", lambda e: e.dma_start(out=gq[:, :], in_=bcast_rows(k.gqa_qn[0, :])), W=["gains"])
            sc.dma("sp", lambda e: e.dma_start(out=gk[:, :], in_=bcast_rows(k.gqa_kn[0, :])), W=["gains"])
            sc.dma("sp", lambda e: e.dma_start(out=r64[:, :, :, :], in_=k.c_rope64[:, :, :, :]), W=["r64"])
            sc.op("pool", lambda e: e.memset(va[:, :, :, 64:128], 0.0), W=["aV1"])
            sc.op("pool", lambda e: e.memset(va[:, :, :, 64:65], 1.0), W=["aV1"])
            hb = [sb2(f"a_hb{i}", [128, 4, D], BF16) for i in range(2)]
            mk = lambda i, w: dict(sq=sb2(f"a_sq{w}{i}", [128, w]), ss=sb2(f"a_ss{w}{i}", [128, 8]), r=sb2(f"a_r{w}{i}", [128, 8]),
                                   qn=sb2(f"a_qn{w}{i}", [128, w]), t1=sb2(f"a_t1{w}{i}", [128, w]), t2=sb2(f"a_t2{w}{i}", [128, w]))
            scq = [mk(i, 512) for i in range(2)]
            sck = [mk(i, 128) for i in range(2)]
            qr = [sb2(f"a_qr{i}", [128, 512], BF16) for i in range(2)]
            kr = [sb2(f"a_kr{i}", [128, 128], BF16) for i in range(2)]
            kdb = [sb2(f"a_kdb{i}", [128, 256], BF16) for i in range(2)]
            ptq = psbf(k, 4)
            ptk = psbf(k, 5)
            def load_hb(b):
                sc.dma("sp", lambda e: e.dma_start(out=hb[b % 2][:, :, :], in_=k.h0T_d[4 * b:4 * b + 4, :, :].rearrange("t p f -> p t f")),
                       R=[("dram", "h0T", 4 * b + j) for j in range(4)], W=[f"a_hb{b % 2}"])

            def tile(t):
                b, j = t // 4, t % 4
                hbb = hb[b % 2]
                if t == 0:
                    load_hb(0)
                if j == 0 and b + 1 < NB:
                    load_hb(b + 1)
                i2 = t % 2
                pa, pb = 2 * i2, 2 * i2 + 1
                mmacc(k, psb(k, pa), [(hbb[:, j, c * 128:(c + 1) * 128], wA[:, c, 0:512]) for c in range(8)],
                      R=[f"a_hb{b % 2}", "a_w"], W=[f"ps{pa}"])
                mmacc(k, psb(k, pb)[:, 0:256], [(hbb[:, j, c * 128:(c + 1) * 128], wA[:, c, 512:768]) for c in range(8)],
                      R=[f"a_hb{b % 2}", "a_w"], W=[f"ps{pb}"])
                yield
                tq, tk = f"a_q{i2}_", f"a_k{i2}_"
                gq_ = rms_rope_tm(k, tq, psb(k, pa), f"ps{pa}", 8, gq, r64, t, scq[i2], qr[i2][:, :])
                gk_ = rms_rope_tm(k, tk, psb(k, pb)[:, 0:128], f"ps{pb}", 2, gk, r64, t, sck[i2], kr[i2][:, :])
                sc.op("act", lambda e: e.copy(va[:, t, :, 0:64], psb(k, pb)[:, 128:256].rearrange("p (g d) -> p g d", g=2)),
                      R=[f"ps{pb}"], W=["aV"])
                for _ in zip(gq_, gk_):
                    yield
                for p in range(4):
                    sc.op("pe", lambda e, p=p: e.transpose(ptq[:, p * 128:(p + 1) * 128], qr[i2][:, p * 128:(p + 1) * 128], k.ident[:, :]),
                          R=[tq + "out", "ident"], W=["ps4"])
                kdv = kdb[i2][:, :].rearrange("p (g u d) -> p g u d", g=2, u=2, d=64)
                for u in range(2):
                    sc.op("act", lambda e, u=u: e.copy(kdv[:, :, u, :], kr[i2][:, :].rearrange("p (g d) -> p g d", g=2)),
                          R=[tk + "out"], W=[f"a_kdb{i2}_{u}"])
                yield
                sc.op("dve", lambda e: e.tensor_copy(qT[:, :, t * 128:(t + 1) * 128], ptq[:, 0:512].rearrange("p (c n) -> p c n", c=4)),
                      R=["ps4"], W=["aQ"])
                for g in range(2):
                    sc.op("pe", lambda e, g=g: e.transpose(ptk[:, g * 128:(g + 1) * 128], kdb[i2][:, g * 128:(g + 1) * 128], k.ident[:, :]),
                          R=[f"a_kdb{i2}_0", f"a_kdb{i2}_1", "ident"], W=["ps5"])
                yield
                sc.op("dve", lambda e: e.tensor_copy(kd[:, :, t * 128:(t + 1) * 128], ptk[:, 0:256].rearrange("p (c n) -> p c n", c=2)),
                      R=["ps5"], W=["aK"])
            run_pipelined((tile(t) for t in range(NT)), depth=2, skew=4)
        attention(k, "a", [(p, qb) for qb in range(NB) for p in range(4)], NT,
                  qk_fn=lambda u, kt, i: (kd[i * 64:i * 64 + 64, u[0] // 2, kt * 128:(kt + 1) * 128],
                                          qT[i * 64:i * 64 + 64, u[0], u[1] * 512:(u[1] + 1) * 512]),
                  v_fn=lambda u, kt, i: va[:, kt, u[0] // 2, :],
                  dv=64, scale=0.125,
                  out_fn=lambda u: k.oA_d[2 * u[0]:2 * u[0] + 2, :, u[1] * 512:(u[1] + 1) * 512].rearrange("h d t -> d h t"),
                  aug=True, Rres=["aQ", "aK", "aV", "aV1"], nh=2)


def phase3(k):
    nc, sc = k.nc, k.sc
    with scope(k) as es:
        sb = lambda name, shape, dt=F32: es.enter_context(nc.sbuf_tensor(name, list(shape), dt))
        qlT = sb("b_qlT", [128, 3, S], BF16)
        kvT = sb("b_kvT", [128, 2, S], BF16)
        kpe = sb("b_kpe", [128, S], BF16)
        rT = [sb(f"b_rT{i}", [128, 2, 512]) for i in range(2)]
        with scope(k) as es2:
            sb2 = lambda name, shape, dt=F32: es2.enter_context(nc.sbuf_tensor(name, list(shape), dt))
            ta = [sb2(f"b_ta{i}", [128, 512]) for i in range(2)]
            tb = [sb2(f"b_tb{i}", [128, 512]) for i in range(2)]
            wB = sb2("b_w", [128, 8, 672], BF16)
            sc.dma("pool", lambda e: e.dma_start(out=wB[:, :, :], in_=k.w_in[:, 768:1440].rearrange("(c p) n -> p c n", p=128)), W=["b_w"])
            wks = sb2("b_wks", [128, 8, 96], BF16)
            sc.op("pool", lambda e: e.memset(wks[:, :, 0:64], 0.0), W=["b_wks0"])
            sc.dma("pool", lambda e: e.dma_start(out=wks[:, :, 64:96], in_=k.w_kr_sw[:, :].rearrange("(c p) n -> p c n", p=128)), W=["b_wks1"])
            gql = sb2("b_gql", [128, 384])
            gkv = sb2("b_gkv", [128, 256])
            sc.dma("sp", lambda e: e.dma_start(out=gql[:, :], in_=bcast_rows(k.mla_qn[0, :])), W=["gains"])
            sc.dma("sp", lambda e: e.dma_start(out=gkv[:, :], in_=bcast_rows(k.mla_kvn[0, :])), W=["gains"])
            hb = [sb2(f"b_hb{i}", [128, 4, D], BF16) for i in range(2)]
            sq = [sb2(f"b_sq{i}", [128, 512]) for i in range(2)]
            st = [sb2(f"b_st{i}", [128, 8]) for i in range(2)]
            qn = [sb2(f"b_qn{i}", [128, 384]) for i in range(2)]
            kn = [sb2(f"b_kn{i}", [128, 256]) for i in range(2)]
            qlb = [sb2(f"b_qlb{i}", [128, 384], BF16) for i in range(2)]
            kvb = [sb2(f"b_kvb{i}", [128, 256], BF16) for i in range(2)]
            ptq = psbf(k, 4)
            ptk = psbf(k, 5)
            for b in range(NB):
                hbb = hb[b % 2]
                blk = slice(b * 512, (b + 1) * 512)
                sc.dma("sp", lambda e, b=b, hbb=hbb: e.dma_start(out=hbb[:, :, :], in_=k.h0T_d[4 * b:4 * b + 4, :, :].rearrange("t p f -> p t f")),
                       R=[("dram", "h0T", 4 * b + j) for j in range(4)], W=[f"b_hb{b % 2}"])
                rTb = rT[b % 2]
                sc.dma("sp", lambda e, b=b, rTb=rTb: e.dma_start(out=rTb[:, :, :], in_=k.c_ropeT[:, :, b * 512:(b + 1) * 512]), W=[f"b_rT{b % 2}"])
                for j in range(4):
                    t = 4 * b + j
                    i2 = t % 2
                    pa, pb = 2 * i2, 2 * i2 + 1
                    tg = f"b1_{i2}_"
                    mmacc(k, psb(k, pa), [(hbb[:, j, c * 128:(c + 1) * 128], wB[:, c, 0:512]) for c in range(8)],
                          R=[f"b_hb{b % 2}", "b_w"], W=[f"ps{pa}"])
                    mmacc(k, psb(k, pb)[:, 0:128], [(hbb[:, j, c * 128:(c + 1) * 128], wB[:, c, 512:640]) for c in range(8)],
                          R=[f"b_hb{b % 2}", "b_w"], W=[f"ps{pb}"])
                    s_, st_ = sq[i2], st[i2]
                    sc.op("act", lambda e, s_=s_, st_=st_, pa=pa: e.activation(out=s_[:, 0:384], in_=psb(k, pa)[:, 0:384], func=AF.Square, accum_out=st_[:, 0:1]),
                          R=[f"ps{pa}"], W=[tg + "sq", tg + "s0"])
                    sc.op("act", lambda e, s_=s_, st_=st_, pa=pa: e.activation(out=s_[:, 384:512], in_=psb(k, pa)[:, 384:512], func=AF.Square, accum_out=st_[:, 1:2]),
                          R=[f"ps{pa}"], W=[tg + "sq2", tg + "s1"])
                    sc.op("act", lambda e, s_=s_, st_=st_, pb=pb: e.activation(out=s_[:, 0:128], in_=psb(k, pb)[:, 0:128], func=AF.Square, accum_out=st_[:, 2:3]),
                          R=[f"ps{pb}", tg + "sq"], W=[tg + "sq", tg + "s2"])
                    sc.op("dve", lambda e, st_=st_: e.tensor_scalar(st_[:, 3:4], st_[:, 0:1], 1.0 / 384, RMS_EPS, ALU.mult, ALU.add),
                          R=[tg + "s0"], W=[tg + "s3"])
                    sc.op("dve", lambda e, st_=st_: e.tensor_tensor(st_[:, 4:5], st_[:, 1:2], st_[:, 2:3], ALU.add),
                          R=[tg + "s1", tg + "s2"], W=[tg + "s4"])
                    sc.op("dve", lambda e, st_=st_: e.tensor_scalar(st_[:, 4:5], st_[:, 4:5], 1.0 / 256, RMS_EPS, ALU.mult, ALU.add),
                          R=[tg + "s4"], W=[tg + "s4"])
                    sc.op("pool", lambda e, st_=st_: e.tensor_tensor(st_[:, 5:7], st_[:, 3:5], k.neghalf[:, 0:2], ALU.pow),
                          R=[tg + "s3", tg + "s4", "neghalf"], W=[tg + "r"])
                    sc.op("dve", lambda e, i2=i2, st_=st_, pa=pa: e.tensor_scalar(qn[i2][:, :], psb(k, pa)[:, 0:384], st_[:, 5:6], None, ALU.mult),
                          R=[f"ps{pa}", tg + "r"], W=[tg + "qn"])
                    sc.op("pool", lambda e, i2=i2: e.tensor_tensor(qlb[i2][:, :], qn[i2][:, :], gql[:, :], ALU.mult),
                          R=[tg + "qn", "gains"], W=[tg + "qlb"])
                    sc.op("dve", lambda e, i2=i2, st_=st_, pa=pa: e.tensor_scalar(kn[i2][:, 0:128], psb(k, pa)[:, 384:512], st_[:, 6:7], None, ALU.mult),
                          R=[f"ps{pa}", tg + "r"], W=[tg + "kn0"])
                    sc.op("dve", lambda e, i2=i2, st_=st_, pb=pb: e.tensor_scalar(kn[i2][:, 128:256], psb(k, pb)[:, 0:128], st_[:, 6:7], None, ALU.mult),
                          R=[f"ps{pb}", tg + "r"], W=[tg + "kn1"])
                    sc.op("pool", lambda e, i2=i2: e.tensor_tensor(kvb[i2][:, :], kn[i2][:, :], gkv[:, :], ALU.mult),
                          R=[tg + "kn0", tg + "kn1", "gains"], W=[tg + "kvb"])
                    for c in range(3):
                        sc.op("pe", lambda e, c=c, i2=i2: e.transpose(ptq[:, c * 128:(c + 1) * 128], qlb[i2][:, c * 128:(c + 1) * 128], k.ident[:, :]),
                              R=[tg + "qlb", "ident"], W=["ps4"])
                    sc.op("dve", lambda e, t=t: e.tensor_copy(qlT[:, :, t * 128:(t + 1) * 128], ptq[:, 0:384].rearrange("p (c n) -> p c n", c=3)),
                          R=["ps4"], W=["b_qlT"])
                    for c in range(2):
                        sc.op("pe", lambda e, c=c, i2=i2: e.transpose(ptk[:, c * 128:(c + 1) * 128], kvb[i2][:, c * 128:(c + 1) * 128], k.ident[:, :]),
                              R=[tg + "kvb", "ident"], W=["ps5"])
                    sc.op("act", lambda e, t=t: e.copy(kvT[:, :, t * 128:(t + 1) * 128], ptk[:, 0:256].rearrange("p (c n) -> p c n", c=2)),
                          R=["ps5"], W=["b_kvT"])
                mmacc(k, psb(k, 6)[0:96, :], [(wB[:, c, 576:672], hbb[:, :, c * 128:(c + 1) * 128]) for c in range(8)],
                      R=[f"b_hb{b % 2}", "b_w"], W=["ps6"])
                mmacc(k, psb(k, 7)[0:96, :], [(wks[:, c, 0:96], hbb[:, :, c * 128:(c + 1) * 128]) for c in range(8)],
                      R=[f"b_hb{b % 2}", "b_wks0", "b_wks1"], W=["ps7"])
                a_, b_ = ta[b % 2], tb[b % 2]
                sc.op("dve", lambda e, a_=a_, rTb=rTb: e.tensor_tensor(a_[64:96, :], psb(k, 6)[64:96, :], rTb[64:96, 0, :], ALU.mult),
                      R=["ps6", f"b_rT{b % 2}"], W=[f"b_ta{b % 2}"])
                sc.op("dve", lambda e, b_=b_, rTb=rTb: e.tensor_tensor(b_[64:96, :], psb(k, 7)[64:96, :], rTb[64:96, 1, :], ALU.mult),
                      R=["ps7", f"b_rT{b % 2}"], W=[f"b_tb{b % 2}"])
                sc.op("pool", lambda e, a_=a_, b_=b_, blk=blk: e.tensor_tensor(kpe[64:96, blk], a_[64:96, :], b_[64:96, :], ALU.add),
                      R=[f"b_ta{b % 2}", f"b_tb{b % 2}"], W=["b_kpe"])
        wqb = sb("b_wqb", [128, 3, 768], BF16)
        wqs = sb("b_wqs", [128, 3, 768], BF16)
        wkv = sb("b_wkv", [128, 2, 1024], BF16)
        sc.dma("pool", lambda e: e.dma_start(out=wqb[:, :, :], in_=k.w_qb[:, :].rearrange("(c p) n -> p c n", p=128)), W=["b_wqb"])
        sc.dma("pool", lambda e: e.dma_start(out=wqs[:, :, :], in_=k.w_qb_sw[:, :].rearrange("(c p) n -> p c n", p=128)), W=["b_wqs"])
        sc.dma("pool", lambda e: e.dma_start(out=wkv[:, :, :], in_=k.w_kvb[:, :].rearrange("(c p) n -> p c n", p=128)), W=["b_wkv"])
        qT = sb("b_qT", [128, 4, S], BF16)
        kT = sb("b_kT", [128, 4, S], BF16)
        vb = sb("b_v", [128, NT, 4, 128], BF16)
        sc.op("pool", lambda e: e.memset(vb[:, :, :, 64:128], 0.0), W=["bV1"])
        sc.op("pool", lambda e: e.memset(vb[:, :, :, 64:65], 1.0), W=["bV1"])
        sc.op("pool", lambda e: e.memset(qT[96:128, :, :], 0.0), W=["bV1"])
        sc.op("pool", lambda e: e.memset(kT[96:128, :, :], 0.0), W=["bV1"])
        ta4 = [sb(f"b_ta4{i}", [128, 512]) for i in range(4)]
        tb4 = [sb(f"b_tb4{i}", [128, 512]) for i in range(4)]
        for half in range(2):
            def blockgen(b, half=half):
                blk = slice(b * 512, (b + 1) * 512)
                rTb = rT[b % 2]
                sc.dma("sp", lambda e: e.dma_start(out=rTb[:, :, :], in_=k.c_ropeT[:, :, b * 512:(b + 1) * 512]), W=[f"b_rT{b % 2}"])
                yield
                for hl in range(4):
                    h = 4 * half + hl
                    pq, pswp, pn = hl % 2, 2 + hl % 2, 4 + hl % 2
                    x4 = (b % 2) * 2 + hl % 2
                    a_, b_ = ta4[x4], tb4[x4]
                    mmacc(k, psb(k, pq)[0:96, :], [(wqb[:, c, h * 96:(h + 1) * 96], qlT[:, c, blk]) for c in range(3)],
                          R=["b_wqb", "b_qlT"], W=[f"ps{pq}"])
                    mmacc(k, psb(k, pswp)[0:96, :], [(wqs[:, c, h * 96:(h + 1) * 96], qlT[:, c, blk]) for c in range(3)],
                          R=["b_wqs", "b_qlT"], W=[f"ps{pswp}"])
                    mmacc(k, psb(k, pn)[0:64, :], [(wkv[:, c, h * 128:h * 128 + 64], kvT[:, c, blk]) for c in range(2)],
                          R=["b_wkv", "b_kvT"], W=[f"ps{pn}"])
                    yield
                    sc.op("act", lambda e, hl=hl, pq=pq: e.copy(qT[0:64, hl, blk], psb(k, pq)[0:64, :]), R=[f"ps{pq}"], W=["bQ"])
                    sc.op("dve", lambda e, a_=a_, pq=pq: e.tensor_tensor(a_[64:96, :], psb(k, pq)[64:96, :], rTb[64:96, 0, :], ALU.mult),
                          R=[f"ps{pq}", f"b_rT{b % 2}"], W=[f"b_ta4{x4}"])
                    sc.op("dve", lambda e, b_=b_, pswp=pswp: e.tensor_tensor(b_[64:96, :], psb(k, pswp)[64:96, :], rTb[64:96, 1, :], ALU.mult),
                          R=[f"ps{pswp}", f"b_rT{b % 2}"], W=[f"b_tb4{x4}"])
                    sc.op("act", lambda e, hl=hl, pn=pn: e.copy(kT[0:64, hl, blk], psb(k, pn)[0:64, :]), R=[f"ps{pn}"], W=["bK"])
                    sc.op("act", lambda e, hl=hl: e.copy(kT[64:96, hl, blk], kpe[64:96, blk]), R=["b_kpe"], W=["bK"])
                    yield
                    sc.op("dve", lambda e, a_=a_, b_=b_, hl=hl: e.tensor_tensor(qT[64:96, hl, blk], a_[64:96, :], b_[64:96, :], ALU.add),
                          R=[f"b_ta4{x4}", f"b_tb4{x4}"], W=["bQ"])
                for j in range(4):
                    t = 4 * b + j
                    pv_ = 6 + t % 2
                    wv = [wkv[:, c, :].rearrange("p (h n) -> p h n", n=128)[:, 4 * half:4 * half + 4, 64:128] for c in range(2)]
                    mmacc(k, psb(k, pv_)[:, 0:256], [(kvT[:, c, t * 128:(t + 1) * 128], wv[c]) for c in range(2)],
                          R=["b_wkv", "b_kvT"], W=[f"ps{pv_}"])
                    yield
                    sc.op("dve", lambda e, t=t, pv_=pv_: e.tensor_copy(vb[:, t, :, 0:64], psb(k, pv_)[:, 0:256].rearrange("p (h d) -> p h d", h=4)),
                          R=[f"ps{pv_}"], W=["bV"])
            run_pipelined((blockgen(b) for b in range(NB)), depth=2, skew=5)
            attention(k, f"b{half}", [(hl, qb) for qb in range(NB) for hl in range(4)], NT,
                      qk_fn=lambda u, kt, i: (kT[:, u[0], kt * 128:(kt + 1) * 128], qT[:, u[0], u[1] * 512:(u[1] + 1) * 512]),
                      v_fn=lambda u, kt, i: vb[:, kt, u[0], :],
                      dv=64, scale=96.0 ** -0.5,
                      out_fn=lambda u, half=half: k.oB_d[4 * half + u[0], :, u[1] * 512:(u[1] + 1) * 512],
                      aug=True, Rres=["bQ", "bK", "bV", "bV1"], nh=1)


def phase4(k):
    nc, sc = k.nc, k.sc
    with scope(k) as es:
        sb = lambda name, shape, dt=F32: es.enter_context(nc.sbuf_tensor(name, list(shape), dt))
        qm = sb("c_q", [128, 4, S], BF16)
        mkT = sb("c_mk", [128, 4, MEM], BF16)
        mv = sb("c_mv", [128, 2, 4, 128], BF16)
        with scope(k) as es2:
            sb2 = lambda name, shape, dt=F32: es2.enter_context(nc.sbuf_tensor(name, list(shape), dt))
            wq = sb2("c_wq", [128, 8, 512], BF16)
            wm = sb2("c_wm", [128, 8, D], BF16)
            sc.dma("pool", lambda e: e.dma_start(out=wq[:, :, :], in_=k.w_in[:, 1440:1952].rearrange("(c p) n -> p c n", p=128)), W=["c_wq"])
            sc.dma("pool", lambda e: e.dma_start(out=wm[:, :, :], in_=k.w_mkv[:, :].rearrange("(c p) n -> p c n", p=128)), W=["c_wm"])
            memf = sb2("c_memf", [128, 2, D])
            memb = sb2("c_memb", [128, 2, D], BF16)
            memT = sb2("c_memT", [128, 8, MEM], BF16)
            sc.dma("sp", lambda e: e.dma_start(out=memf[:, :, :], in_=k.mem[:, :].rearrange("(t p) d -> p t d", p=128)), W=["c_memf"])
            sc.op("act", lambda e: e.copy(memb[:, :, :], memf[:, :, :]), R=["c_memf"], W=["c_memb"])
            pt4 = psbf(k, 4)
            pt5 = psbf(k, 5)
            for t in range(2):
                pt = pt4 if t == 0 else pt5
                for c in range(8):
                    sc.op("pe", lambda e, t=t, c=c, pt=pt: e.transpose(pt[:, c * 128:(c + 1) * 128], memb[:, t, c * 128:(c + 1) * 128], k.ident[:, :]),
                          R=["c_memb", "ident"], W=[f"ps{4 + t}"])
                sc.op("dve", lambda e, t=t, pt=pt: e.tensor_copy(memT[:, :, t * 128:(t + 1) * 128], pt[:, :].rearrange("p (c n) -> p c n", c=8)),
                      R=[f"ps{4 + t}"], W=["c_memT"])
            for h in range(4):
                pb_ = h % 2
                mmacc(k, psb(k, pb_)[:, 0:MEM], [(wm[:, c, h * 128:(h + 1) * 128], memT[:, c, :]) for c in range(8)],
                      R=["c_wm", "c_memT"], W=[f"ps{pb_}"])
                sc.op("act", lambda e, h=h, pb_=pb_: e.copy(mkT[:, h, :], psb(k, pb_)[:, 0:MEM]), R=[f"ps{pb_}"], W=["cK"])
            for t in range(2):
                mmacc(k, psb(k, 2 + t), [(memT[:, c, t * 128:(t + 1) * 128], wm[:, c, 512:1024]) for c in range(8)],
                      R=["c_wm", "c_memT"], W=[f"ps{2 + t}"])
                sc.op("dve", lambda e, t=t: e.tensor_copy(mv[:, t, :, :], psb(k, 2 + t).rearrange("p (h d) -> p h d", h=4)),
                      R=[f"ps{2 + t}"], W=["cV"])
            hb = [sb2(f"c_hb{i}", [128, 4, D], BF16) for i in range(2)]
            for b in range(NB):
                hbb = hb[b % 2]
                blk = slice(b * 512, (b + 1) * 512)
                sc.dma("sp", lambda e, b=b, hbb=hbb: e.dma_start(out=hbb[:, :, :], in_=k.h0T_d[4 * b:4 * b + 4, :, :].rearrange("t p f -> p t f")),
                       R=[("dram", "h0T", 4 * b + j) for j in range(4)], W=[f"c_hb{b % 2}"])
                for h in range(4):
                    pb_ = 4 + h % 2 if h < 2 else 6 + h % 2
                    mmacc(k, psb(k, pb_), [(wq[:, c, h * 128:(h + 1) * 128], hbb[:, :, c * 128:(c + 1) * 128]) for c in range(8)],
                          R=["c_wq", f"c_hb{b % 2}"], W=[f"ps{pb_}"])
                    eng = "act" if h % 2 == 0 else "dve"
                    if eng == "act":
                        sc.op("act", lambda e, h=h, blk=blk, pb_=pb_: e.copy(qm[:, h, blk], psb(k, pb_)), R=[f"ps{pb_}"], W=["cQ"])
                    else:
                        sc.op("dve", lambda e, h=h, blk=blk, pb_=pb_: e.tensor_copy(qm[:, h, blk], psb(k, pb_)), R=[f"ps{pb_}"], W=["cQ"])
        attention(k, "c", [(h, qb) for qb in range(NB) for h in range(4)], 2,
                  qk_fn=lambda u, kt, i: (mkT[:, u[0], kt * 128:(kt + 1) * 128], qm[:, u[0], u[1] * 512:(u[1] + 1) * 512]),
                  v_fn=lambda u, kt, i: mv[:, kt, u[0], :],
                  dv=128, scale=128.0 ** -0.5, out_fn=lambda u: k.oC_d[u[0], :, u[1] * 512:(u[1] + 1) * 512],
                  aug=False, Rres=["cQ", "cK", "cV"], nh=1)


def phase5(k):
    nc, sc = k.nc, k.sc
    BIG = float(1 << 22)
    with scope(k) as es:
        sb = lambda name, shape, dt=F32: es.enter_context(nc.sbuf_tensor(name, list(shape), dt))
        wg = sb("m_wg", [128, 8, 3 * D], BF16)
        for i in range(3):
            sc.dma("pool", lambda e, i=i: e.dma_start(out=wg[:, :, i * D:(i + 1) * D],
                                                     in_=k.w_in[:, OFF_G + i * D:OFF_G + (i + 1) * D].rearrange("(c p) n -> p c n", p=128)),
                   W=[f"m_wg{i}"])
        bg = sb("m_bg", [128, 3 * D])
        sc.dma("sp", lambda e: e.dma_start(out=bg[:, :], in_=bcast_rows(k.b_gate[0, :])), W=["m_bg"])
        wa = sb("m_wa", [64, 8, D], BF16)
        wb_ = sb("m_wb", [64, 8, D], BF16)
        wc = sb("m_wc", [128, 4, D], BF16)
        wo = sb("m_wo", [128, 8, D], BF16)
        sc.dma("pool", lambda e: e.dma_start(out=wa[:, :, :], in_=k.w_br_a[:, :].rearrange("(h p) n -> p h n", p=64)), W=["m_wa"])
        sc.dma("pool", lambda e: e.dma_start(out=wb_[:, :, :], in_=k.w_br_b[:, :].rearrange("(h p) n -> p h n", p=64)), W=["m_wb"])
        sc.dma("pool", lambda e: e.dma_start(out=wc[:, :, :], in_=k.w_br_c[:, :].rearrange("(h p) n -> p h n", p=128)), W=["m_wc"])
        sc.dma("pool", lambda e: e.dma_start(out=wo[:, :, :], in_=k.w_out[:, :].rearrange("(c p) n -> p c n", p=128)), W=["m_wo"])
        wr = sb("m_wr", [128, 8, NE])
        sc.dma("sp", lambda e: e.dma_start(out=wr[:, :, :], in_=k.w_router[:, :].rearrange("(c p) n -> p c n", p=128)), W=["m_wr"])
        brr = sb("m_br", [128, NE])
        sc.dma("sp", lambda e: e.dma_start(out=brr[:, :], in_=bcast_rows(k.b_router[0, :])), W=["m_br"])
        g1 = sb("m_g1", [128, D])
        b1 = sb("m_b1", [128, D])
        sc.dma("sp", lambda e: e.dma_start(out=g1[:, :], in_=bcast_rows(k.ln1_g[0, :])), W=["lnconst"])
        sc.dma("sp", lambda e: e.dma_start(out=b1[:, :], in_=bcast_rows(k.ln1_b[0, :])), W=["lnconst"])
        tri = sb("m_tri", [128, 128], BF16)
        ecap = sb("m_ecap", [128, NE])
        sc.dma("sp", lambda e: e.dma_start(out=tri[:, :], in_=k.c_tri[:, :]), W=["m_tri"])
        sc.dma("sp", lambda e: e.dma_start(out=ecap[:, :], in_=k.c_ecap[:, :]), W=["m_ecap"])
        base = sb("m_base", [128, NE])
        sc.op("pool", lambda e: e.memset(base[:, :], 0.0), W=["m_base"])
        NB2 = 2
        hT = [sb(f"m_hT{i}", [128, D], BF16) for i in range(NB2)]
        oa = [sb(f"m_oa{i}", [64, 8, 128], BF16) for i in range(NB2)]
        ob = [sb(f"m_ob{i}", [64, 8, 128], BF16) for i in range(NB2)]
        oc = [sb(f"m_oc{i}", [128, 4, 128], BF16) for i in range(NB2)]
        h0 = [sb(f"m_h0{i}", [128, D]) for i in range(NB2)]
        gs = [[sb(f"m_gs{i}_{g}", [128, 512]) for g in range(2)] for i in range(NB2)]
        mg = [sb(f"m_mg{i}", [128, D]) for i in range(NB2)]
        mgb = [sb(f"m_mgb{i}", [128, D], BF16) for i in range(NB2)]
        mT = [sb(f"m_mT{i}", [128, D], BF16) for i in range(NB2)]
        h1 = [sb(f"m_h1{i}", [128, D]) for i in range(NB2)]
        h1b = [sb(f"m_h1b{i}", [128, D], BF16) for i in range(NB2)]
        h1T = sb("m_h1T", [128, D])
        scr = [dict(st=sb(f"m_st{i}", [128, 12]), mv=sb(f"m_mv{i}", [128, 2]), rs=sb(f"m_rs{i}", [128, 2])) for i in range(NB2)]
        lg = sb("m_lg", [128, NE])
        v8 = sb("m_v8", [128, 8])
        sm = sb("m_sm", [128, 16])
        ex = sb("m_ex", [128, 4])
        oh = sb("m_oh", [128, 4, NE])
        prod = sb("m_prod", [128, 4, NE])
        mask = sb("m_mask", [128, NE])
        maskb = sb("m_maskb", [128, NE], BF16)
        rank = sb("m_rank", [128, NE])
        rk = sb("m_rk", [128, 4])
        ek = sb("m_ek", [128, 4])
        vl = sb("m_vl", [128, 4])
        sl = sb("m_sl", [128, 4])
        rtd = sb("m_rtd", [128, 8])
        brs = [(oa, wa, 8, 64, k.oA_d), (ob, wb_, 8, 64, k.oB_d), (oc, wc, 4, 128, k.oC_d)]

        def tile(t):
            i2 = t % NB2
            tk = slice(t * 128, (t + 1) * 128)
            T = f"m{i2}_"
            sc.dma("sp", lambda e: e.dma_start(out=hT[i2][:, :], in_=k.h0T_d[t, :, :]), R=[("dram", "h0T", t)], W=[T + "hT"])
            sc.dma("sp", lambda e: e.dma_start(out=h0[i2][:, :], in_=k.h0_d[tk, :]), R=[("dram", "h0", t)], W=[T + "h0"])
            for bi, (ot, _, nh, kr_, od) in enumerate(brs):
                tagn = "abc"[bi]
                rd = [r for r in sc.lastw if isinstance(r, tuple) and r[0] == "dram" and str(r[1]).startswith(tagn) and r[3] == t // 4]
                sc.dma("sp", lambda e, ot=ot, od=od: e.dma_start(out=ot[i2][:, :, :], in_=od[:, :, tk].rearrange("h d t -> d h t")),
                       R=rd, W=[T + "o" + tagn])
            yield
            pbase = 4 * i2
            for bi, (ot, wbr, nh, kr_, od) in enumerate(brs):
                tagn = "abc"[bi]
                for half in range(2):
                    gi = half
                    col = slice(bi * D + half * 512, bi * D + half * 512 + 512)
                    hs = slice(half * 512, (half + 1) * 512)
                    pg, pbr = pbase + gi, pbase + 2 + gi
                    g_ = gs[i2][gi]
                    mmacc(k, psb(k, pg), [(hT[i2][:, c * 128:(c + 1) * 128], wg[:, c, col]) for c in range(8)],
                          R=[T + "hT", f"m_wg{bi}"], W=[f"ps{pg}"])
                    mmacc(k, psb(k, pbr), [(ot[i2][0:kr_, h, :], wbr[0:kr_, h, hs]) for h in range(nh)],
                          R=[T + "o" + tagn, f"m_w{tagn}"], W=[f"ps{pbr}"])
                    sc.op("dve", lambda e, g_=g_, pg=pg, col=col: e.tensor_tensor(g_[:, :], psb(k, pg), bg[:, col], ALU.add),
                          R=[f"ps{pg}", "m_bg"], W=[T + f"gs{gi}"])
                    sc.op("act", lambda e, g_=g_: e.activation(out=g_[:, :], in_=g_[:, :], func=AF.Sigmoid),
                          R=[T + f"gs{gi}"], W=[T + f"gs{gi}"])
                    yield
                    if bi == 0:
                        sc.op("dve", lambda e, g_=g_, pbr=pbr, hs=hs: e.tensor_tensor(mg[i2][:, hs], g_[:, :], psb(k, pbr), ALU.mult),
                              R=[T + f"gs{gi}", f"ps{pbr}"], W=[T + f"mg{half}"])
                    else:
                        sc.op("dve", lambda e, g_=g_, pbr=pbr: e.tensor_tensor(g_[:, :], g_[:, :], psb(k, pbr), ALU.mult),
                              R=[T + f"gs{gi}", f"ps{pbr}"], W=[T + f"gs{gi}"])
                        sc.op("dve", lambda e, g_=g_, hs=hs: e.tensor_tensor(mg[i2][:, hs], mg[i2][:, hs], g_[:, :], ALU.add),
                              R=[T + f"gs{gi}", T + f"mg{half}"], W=[T + f"mg{half}"])
            yield
            sc.op("act", lambda e: e.copy(mgb[i2][:, :], mg[i2][:, :]), R=[T + "mg0", T + "mg1"], W=[T + "mgb"])
            yield
            ptb = psbf(k, pbase)
            for c in range(8):
                sc.op("pe", lambda e, c=c: e.transpose(ptb[:, c * 128:(c + 1) * 128], mgb[i2][:, c * 128:(c + 1) * 128], k.ident[:, :]),
                      R=[T + "mgb", "ident"], W=[f"ps{pbase}"])
            yield
            sc.op("dve", lambda e: e.tensor_copy(mT[i2][:, :], ptb[:, :]), R=[f"ps{pbase}"], W=[T + "mT"])
            yield
            for half in range(2):
                hs = slice(half * 512, (half + 1) * 512)
                pb_ = pbase + 1 + half
                mmacc(k, psb(k, pb_), [(mT[i2][:, c * 128:(c + 1) * 128], wo[:, c, hs]) for c in range(8)],
                      R=[T + "mT", "m_wo"], W=[f"ps{pb_}"])
            yield
            tag = T + "ln_"
            for half in range(2):
                hs = slice(half * 512, (half + 1) * 512)
                pb_ = pbase + 1 + half
                sc.op("dve", lambda e, hs=hs, pb_=pb_: e.scalar_tensor_tensor(mg[i2][:, hs], h0[i2][:, hs], ALPHA, psb(k, pb_), ALU.mult, ALU.add),
                      R=[f"ps{pb_}", T + "h0"], W=[tag + "z", T + f"mg{half}"])
            yield
            for _ in layer_norm_tile(k, tag, mg[i2], g1, b1, h1[i2], scr[i2], stages=True):
                yield
            sc._record(sc.lastw[tag + "o"], [T + "mg0", T + "mg1"], [])
            sc.dma("sp", lambda e: e.dma_start(out=k.h1_d[tk, :], in_=h1[i2][:, :]), R=[tag + "o"], W=[("dram", "h1", t)])
            sc.op("act", lambda e: e.copy(h1b[i2][:, :], h1[i2][:, :]), R=[tag + "o"], W=[T + "h1b"])
            for c in range(8):
                sc.op("pe", lambda e, c=c: e.transpose(k.pp[pbase // 2 + 1][:, c * 128:(c + 1) * 128], h1[i2][:, c * 128:(c + 1) * 128], k.identf[:, :]),
                      R=[tag + "o", "identf"], W=[f"ps{pbase + 2}", f"ps{pbase + 3}"])
            yield
            sc.op("dve", lambda e: e.tensor_copy(h1T[:, :], k.pp[pbase // 2 + 1][:, :]), R=[f"ps{pbase + 2}", f"ps{pbase + 3}"], W=["m_h1T"])
            yield
            pl_ = pbase
            mmacc(k, psb(k, pl_)[:, 0:NE], [(h1T[:, c * 128:(c + 1) * 128], wr[:, c, :]) for c in range(8)],
                  R=["m_h1T", "m_wr"], W=[f"ps{pl_}"])
            yield
            sc.op("dve", lambda e: e.tensor_tensor(lg[:, :], psb(k, pl_)[:, 0:NE], brr[:, :], ALU.add), R=[f"ps{pl_}", "m_br"], W=["m_lg"])
            sc.op("dve", lambda e: e.max(v8[:, :], lg[:, :]), R=["m_lg"], W=["m_v8"])
            sc.op("dve", lambda e: e.tensor_scalar_mul(sm[:, 0:1], v8[:, 0:1], -1.0), R=["m_v8"], W=["m_sm0"])
            for kk in range(4):
                sc.op("dve", lambda e, kk=kk: e.tensor_scalar(oh[:, kk, :], lg[:, :], v8[:, kk:kk + 1], None, ALU.is_equal),
                      R=["m_lg", "m_v8"], W=[f"m_oh{kk}"])
            ohr = [f"m_oh{kk}" for kk in range(4)]
            sc.op("act", lambda e: e.activation(out=ex[:, :], in_=v8[:, 0:4], func=AF.Exp, bias=sm[:, 0:1], accum_out=sm[:, 1:2]),
                  R=["m_v8", "m_sm0"], W=["m_ex", "m_sm1"])
            sc.op("dve", lambda e: e.tensor_tensor(mask[:, :], oh[:, 0, :], oh[:, 1, :], ALU.add), R=ohr, W=["m_mask"])
            sc.op("dve", lambda e: e.tensor_tensor(mask[:, :], mask[:, :], oh[:, 2, :], ALU.add), R=ohr + ["m_mask"], W=["m_mask"])
            sc.op("dve", lambda e: e.tensor_tensor(maskb[:, :], mask[:, :], oh[:, 3, :], ALU.add), R=ohr + ["m_mask"], W=["m_maskb"])
            yield
            sc.op("pe", lambda e: e.matmul(psb(k, pl_)[:, 64:64 + NE], tri[:, :], maskb[:, :], start=True, stop=True),
                  R=["m_maskb", "m_tri"], W=[f"ps{pl_}b"])
            sc.op("pe", lambda e: e.matmul(psb(k, pl_)[:, 128:128 + NE], k.ones_bf[:, :], maskb[:, :], start=True, stop=True),
                  R=["m_maskb", "ones_bf"], W=[f"ps{pl_}c"])
            yield
            sc.op("dve", lambda e: e.tensor_tensor(rank[:, :], psb(k, pl_)[:, 64:64 + NE], base[:, :], ALU.add), R=[f"ps{pl_}b", "m_base"], W=["m_rank"])
            sc.op("dve", lambda e: e.tensor_tensor(base[:, :], psb(k, pl_)[:, 128:128 + NE], base[:, :], ALU.add), R=[f"ps{pl_}c", "m_base", "m_rank"], W=["m_base"])
            sc._record(sc.lastw["m_base"], [f"ps{pl_}"], [])
            sc.op("dve", lambda e: e.tensor_tensor(prod[:, :, :], oh[:, :, :], rank[:, :].unsqueeze(1).broadcast_to([128, 4, NE]), ALU.mult),
                  R=ohr + ["m_rank"], W=["m_prod"])
            sc.op("dve", lambda e: e.reduce_sum(rk[:, :], prod[:, :, :], AX.X), R=["m_prod"], W=["m_rk"])
            sc.op("dve", lambda e: e.tensor_tensor(prod[:, :, :], oh[:, :, :], ecap[:, :].unsqueeze(1).broadcast_to([128, 4, NE]), ALU.mult),
                  R=ohr + ["m_ecap", "m_rk"], W=["m_prod"])
            sc.op("dve", lambda e: e.reduce_sum(ek[:, :], prod[:, :, :], AX.X), R=["m_prod"], W=["m_ek"])
            sc.op("dve", lambda e: e.tensor_single_scalar(vl[:, :], rk[:, :], float(CAP), ALU.is_lt), R=["m_rk"], W=["m_vl"])
            sc.op("dve", lambda e: e.tensor_tensor(sl[:, :], rk[:, :], ek[:, :], ALU.add), R=["m_rk", "m_ek"], W=["m_sl"])
            sc.op("dve", lambda e: e.tensor_scalar_add(sl[:, :], sl[:, :], -BIG), R=["m_sl"], W=["m_sl"])
            sc.op("dve", lambda e: e.tensor_tensor(sl[:, :], sl[:, :], vl[:, :], ALU.mult), R=["m_sl", "m_vl"], W=["m_sl"])
            sc.op("dve", lambda e: e.tensor_scalar_add(rtd[:, 0:4], sl[:, :], BIG), R=["m_sl"], W=["m_rtd0"])
            sc.op("dve", lambda e: e.tensor_copy(k.slots[:, t, :], rtd[:, 0:4]), R=["m_rtd0"], W=[("slots", t)])
            sc.op("dve", lambda e: e.reciprocal(sm[:, 2:3], sm[:, 1:2]), R=["m_sm1"], W=["m_sm2"])
            sc.op("dve", lambda e: e.tensor_scalar(ex[:, :], ex[:, :], sm[:, 2:3], None, ALU.mult), R=["m_ex", "m_sm2"], W=["m_ex"])
            sc.op("dve", lambda e: e.tensor_tensor(k.wts[:, t, :], ex[:, :], vl[:, :], ALU.mult), R=["m_ex", "m_vl"], W=[("wts", t)])
            yield
            if k.dbg:
                sc.op("dve", lambda e: e.tensor_copy(rtd[:, 4:8], k.wts[:, t, :]), R=[("wts", t)], W=["m_rtd1"])
                sc.dma("sp", lambda e: e.dma_start(out=k.rt_d[tk, :], in_=rtd[:, :]), R=["m_rtd0", "m_rtd1"], W=[("dram", "rt", t)])
            for kk in range(4):
                sc.dma("pool", lambda e, kk=kk: e.indirect_dma_start(
                    out=k.xs_d[:, :], out_offset=bass.IndirectOffsetOnAxis(ap=k.slots[:, t, kk:kk + 1], axis=0),
                    in_=h1b[i2][:, :], in_offset=None, bounds_check=sc.bc_reg, oob_is_err=False),
                    R=[("slots", t), T + "h1b"], W=[("dram", "xs", t, kk)])
        run_pipelined((tile(t) for t in range(NT)), depth=2, skew=8)


def phase6(k):
    nc, sc = k.nc, k.sc
    with scope(k) as es:
        sb = lambda name, shape, dt=F32: es.enter_context(nc.sbuf_tensor(name, list(shape), dt))
        w1 = [sb(f"e_w1{i}", [128, 8, 2 * D], BF16) for i in range(2)]
        w2 = [sb(f"e_w2{i}", [128, 8, D], BF16) for i in range(2)]
        bo = [sb(f"e_bo{i}", [128, D]) for i in range(2)]
        bi_ = sb("e_bi", [128, NE * 16])
        sc.dma("sp", lambda e: e.dma_start(out=bi_[:, :], in_=k.b_ein[:, :]), W=["e_bi"])
        bi1 = sb("e_bi1", [128, NE * 16])
        sc.op("dve", lambda e: e.tensor_scalar_add(bi1[:, :], bi_[:, :], 1.0), R=["e_bi"], W=["e_bi1"])
        CH = 3
        NCH = CAPT // CH
        N = CH * 128
        xs = [sb(f"e_xs{i}", [128, CH, D], BF16) for i in range(2)]
        xT = [sb(f"e_xT{i}", [128, 8, N], BF16) for i in range(2)]
        aT = [sb(f"e_aT{i}", [128, 8, N], BF16) for i in range(2)]
        t1 = [sb(f"e_t1{i}", [128, N]) for i in range(2)]
        t2 = [sb(f"e_t2{i}", [128, N]) for i in range(2)]
        sg = [sb(f"e_sg{i}", [128, N]) for i in range(2)]
        ys = [sb(f"e_ys{i}", [128, D]) for i in range(2)]
        pt = [psbf(k, 4), psbf(k, 5)]
        xs_reads = [r for r in sc.lastw if isinstance(r, tuple) and r[0] == "dram" and r[1] == "xs"]
        gchunk = 0

        def load_xs(g):
            s0_ = (g // NCH) * CAP + (g % NCH) * N
            sc.dma("sp", lambda e: e.dma_start(out=xs[g % 2][:, :, :], in_=k.xs_d[s0_:s0_ + N, :].rearrange("(t p) d -> p t d", p=128)),
                   R=xs_reads if g == 0 else [], W=[f"e_xs{g % 2}"])
        for e_ in range(NE):
            wi = e_ % 2
            for hf in range(2):
                sc.dma("pool", lambda e, e_=e_, wi=wi, hf=hf: e.dma_start(
                    out=w1[wi][:, :, hf * D:(hf + 1) * D], in_=k.w_ein[e_, :, hf * D:(hf + 1) * D].rearrange("(c p) n -> p c n", p=128)),
                    W=[f"e_w1{wi}_{hf}"])
            sc.dma("pool", lambda e, e_=e_, wi=wi: e.dma_start(out=w2[wi][:, :, :], in_=k.w_eout[e_, :, :].rearrange("(c p) n -> p c n", p=128)),
                   W=[f"e_w2{wi}"])
            sc.dma("pool", lambda e, e_=e_, wi=wi: e.dma_start(out=bo[wi][:, :], in_=bcast_rows(k.b_eout[e_, :])), W=[f"e_bo{wi}"])
            for ch in range(NCH):
                ci = gchunk % 2
                gchunk += 1
                s0 = e_ * CAP + ch * N
                if gchunk == 1:
                    load_xs(0)
                if gchunk < NE * NCH:
                    load_xs(gchunk)
                for st in range(CH):
                    for c in range(8):
                        sc.op("pe", lambda e, st=st, c=c, ci=ci: e.transpose(pt[st % 2][:, c * 128:(c + 1) * 128], xs[ci][:, st, c * 128:(c + 1) * 128], k.ident[:, :]),
                              R=[f"e_xs{ci}", "ident"], W=[f"ps{4 + st % 2}"])
                    sc.op("dve" if st % 2 == 0 else "act",
                          (lambda e, st=st, ci=ci: e.tensor_copy(xT[ci][:, :, st * 128:(st + 1) * 128], pt[st % 2][:, :].rearrange("p (c n) -> p c n", c=8)))
                          if st % 2 == 0 else
                          (lambda e, st=st, ci=ci: e.copy(xT[ci][:, :, st * 128:(st + 1) * 128], pt[st % 2][:, :].rearrange("p (c n) -> p c n", c=8))),
                          R=[f"ps{4 + st % 2}"], W=[f"e_xT{ci}_{st}"])
                xTr = [f"e_xT{ci}_{st}" for st in range(CH)]
                for j in range(8):
                    ji = j % 2
                    pg, pl = ji, 2 + ji
                    mmacc(k, psb(k, pg)[:, 0:N], [(w1[wi][:, c, j * 128:(j + 1) * 128], xT[ci][:, c, :]) for c in range(8)],
                          R=xTr + [f"e_w1{wi}_0"], W=[f"ps{pg}"])
                    mmacc(k, psb(k, pl)[:, 0:N], [(w1[wi][:, c, D + j * 128:D + (j + 1) * 128], xT[ci][:, c, :]) for c in range(8)],
                          R=xTr + [f"e_w1{wi}_1"], W=[f"ps{pl}"])
                    bg_ = bi_[:, e_ * 16 + j:e_ * 16 + j + 1]
                    bl_ = bi1[:, e_ * 16 + 8 + j:e_ * 16 + 8 + j + 1]
                    sc.op("dve", lambda e, ji=ji, pg=pg, bg_=bg_: e.tensor_scalar(t1[ji][:, :], psb(k, pg)[:, 0:N], bg_, 7.0, ALU.add, ALU.min),
                          R=[f"ps{pg}", "e_bi"], W=[f"e_t1{ji}"])
                    sc.op("act", lambda e, ji=ji: e.activation(out=sg[ji][:, :], in_=t1[ji][:, :], func=AF.Sigmoid, scale=1.702),
                          R=[f"e_t1{ji}"], W=[f"e_sg{ji}"])
                    sc.op("dve", lambda e, ji=ji, pl=pl, bl_=bl_: e.tensor_scalar(t2[ji][:, :], psb(k, pl)[:, 0:N], bl_, 8.0, ALU.add, ALU.min),
                          R=[f"ps{pl}", "e_bi1"], W=[f"e_t2{ji}"])
                    sc.op("dve", lambda e, ji=ji: e.tensor_tensor(t1[ji][:, :], t1[ji][:, :], sg[ji][:, :], ALU.mult),
                          R=[f"e_t1{ji}", f"e_sg{ji}"], W=[f"e_t1{ji}"])
                    sc.op("dve", lambda e, ji=ji, j=j, ci=ci: e.scalar_tensor_tensor(aT[ci][:, j, :], t2[ji][:, :], -6.0, t1[ji][:, :], ALU.max, ALU.mult),
                          R=[f"e_t1{ji}", f"e_t2{ji}"], W=[f"e_aT{ci}_{j}"])
                aTr = [f"e_aT{ci}_{j}" for j in range(8)]
                for st in range(CH):
                    yi = st % 2
                    for half in range(2):
                        pb_ = 6 + half
                        mmacc(k, psb(k, pb_), [(aT[ci][:, j, st * 128:(st + 1) * 128], w2[wi][:, j, half * 512:(half + 1) * 512]) for j in range(8)],
                              R=aTr + [f"e_w2{wi}"], W=[f"ps{pb_}"])
                        sc.op("dve", lambda e, yi=yi, half=half, pb_=pb_, wi=wi: e.tensor_tensor(
                            ys[yi][:, half * 512:(half + 1) * 512], psb(k, pb_), bo[wi][:, half * 512:(half + 1) * 512], ALU.add),
                            R=[f"ps{pb_}", f"e_bo{wi}"], W=[f"e_ys{yi}_{half}"])
                    r0 = s0 + st * 128
                    sc.dma("sp", lambda e, yi=yi, r0=r0: e.dma_start(out=k.ys_d[r0:r0 + 128, :], in_=ys[yi][:, :]),
                           R=[f"e_ys{yi}_0", f"e_ys{yi}_1"], W=[("dram", "ys", r0)])


def phase7(k):
    nc, sc = k.nc, k.sc
    with scope(k) as es:
        sb = lambda name, shape, dt=F32: es.enter_context(nc.sbuf_tensor(name, list(shape), dt))
        g2 = sb("f_g2", [128, D])
        b2 = sb("f_b2", [128, D])
        sc.dma("sp", lambda e: e.dma_start(out=g2[:, :], in_=bcast_rows(k.ln2_g[0, :])), W=["lnconst"])
        sc.dma("sp", lambda e: e.dma_start(out=b2[:, :], in_=bcast_rows(k.ln2_b[0, :])), W=["lnconst"])
        NB2 = 3
        h1 = [sb(f"f_h1{i}", [128, D]) for i in range(NB2)]
        yk = [[sb(f"f_yk{i}_{kk}", [128, D]) for kk in range(4)] for i in range(NB2)]
        acc = [sb(f"f_acc{i}", [128, D]) for i in range(NB2)]
        out = [sb(f"f_out{i}", [128, D]) for i in range(NB2)]
        scr = [dict(st=sb(f"f_st{i}", [128, 12]), mv=sb(f"f_mv{i}", [128, 2]), rs=sb(f"f_rs{i}", [128, 2])) for i in range(NB2)]
        ys_w = [r for r in sc.lastw if isinstance(r, tuple) and r[0] == "dram" and r[1] == "ys"]
        for i in range(NB2):
            for kk in range(4):
                sc.op("pool", lambda e, i=i, kk=kk: e.memset(yk[i][kk][:, :], 0.0), W=[f"f_yk{i}_{kk}"])
        NB2 = 3

        def tile(t):
            i2 = t % NB2
            tk = slice(t * 128, (t + 1) * 128)
            tag = f"f_{i2}_"
            sc.dma("sp", lambda e: e.dma_start(out=h1[i2][:, :], in_=k.h1_d[tk, :]), R=[("dram", "h1", t)], W=[f"f_h1{i2}"])
            for kk in range(4):
                sc.dma("pool", lambda e, kk=kk: e.indirect_dma_start(
                    out=yk[i2][kk][:, :], out_offset=None, in_=k.ys_d[:, :],
                    in_offset=bass.IndirectOffsetOnAxis(ap=k.slots[:, t, kk:kk + 1], axis=0),
                    bounds_check=sc.bc_reg, oob_is_err=False),
                    R=(ys_w if t == 0 else []) + [("slots", t)], W=[f"f_yk{i2}_{kk}"])
            yield
            sc.op("dve", lambda e: e.scalar_tensor_tensor(acc[i2][:, :], yk[i2][0][:, :], k.wts[:, t, 0:1], h1[i2][:, :], ALU.mult, ALU.add),
                  R=[f"f_yk{i2}_0", ("wts", t), f"f_h1{i2}"], W=[tag + "z"])
            sc.op("dve", lambda e: e.scalar_tensor_tensor(acc[i2][:, :], h1[i2][:, :], ALPHA - 1.0, acc[i2][:, :], ALU.mult, ALU.add),
                  R=[tag + "z", f"f_h1{i2}"], W=[tag + "z"])
            for kk in range(1, 4):
                sc.op("dve", lambda e, kk=kk: e.scalar_tensor_tensor(acc[i2][:, :], yk[i2][kk][:, :], k.wts[:, t, kk:kk + 1], acc[i2][:, :], ALU.mult, ALU.add),
                      R=[f"f_yk{i2}_{kk}", ("wts", t), tag + "z"], W=[tag + "z"])
            yield
            for _ in layer_norm_tile(k, tag, acc[i2], g2, b2, out[i2], scr[i2], stages=True):
                yield
            sc.dma("sp", lambda e: e.dma_start(out=k.y[tk, :], in_=out[i2][:, :]), R=[tag + "o"], W=[("y", t)])
        run_pipelined((tile(t) for t in range(NT)), depth=3, skew=2)


def host_consts():
    c = {}
    c["c_ident"] = np.eye(128, dtype=np.float32).astype(ml_dtypes.bfloat16)
    c["c_identf"] = np.eye(128, dtype=np.float32)
    t = np.arange(S)
    row, col = t // 64, t % 64

    def tab(half):
        inv = (10000.0 ** (-np.arange(half, dtype=np.float32) / half)).astype(np.float32)
        ar = row.astype(np.float32)[:, None] * inv[None, :]
        ac = col.astype(np.float32)[:, None] * inv[None, :]
        cs = np.concatenate([np.cos(ar), np.cos(ar), np.cos(ac), np.cos(ac)], axis=1)
        sn = np.concatenate([-np.sin(ar), np.sin(ar), -np.sin(ac), np.sin(ac)], axis=1)
        return cs.astype(np.float32), sn.astype(np.float32)
    c64, s64 = tab(16)
    r64 = np.stack([c64, s64], 0).reshape(2, NT, 128, 64).transpose(2, 0, 1, 3)
    c["c_rope64"] = np.ascontiguousarray(r64)
    c32, s32 = tab(8)
    rT = np.stack([c32.T, s32.T], 0)
    rT = np.tile(rT[None], (4, 1, 1, 1)).transpose(0, 2, 1, 3).reshape(128, 2, S)
    c["c_ropeT"] = np.ascontiguousarray(rT)
    sel = np.zeros((128, 128), np.float32)
    sel[64, :] = 1.0
    c["c_sel"] = sel
    c["c_tri"] = np.triu(np.ones((128, 128), np.float32), 1).astype(ml_dtypes.bfloat16)
    c["c_ecap"] = np.tile((np.arange(NE, dtype=np.float32) * CAP)[None, :], (128, 1))
    return c


def swap_perm(n_heads, hd, rope_off, rope_dim):
    perm = np.arange(n_heads * hd)
    q = rope_dim // 4
    for h in range(n_heads):
        b = h * hd + rope_off
        for blk in range(2):
            o = b + blk * 2 * q
            perm[o:o + q] = np.arange(o + q, o + 2 * q)
            perm[o + q:o + 2 * q] = np.arange(o, o + q)
    return perm


_CACHE = {}


def kernel(**inputs):
    upto = inputs.pop("_upto", 99)
    dbg = tuple(inputs.pop("_dbg", ()))
    ncores = inputs.pop("_ncores", 8)
    key = (upto, dbg)
    if key not in _CACHE:
        _CACHE[key] = build(upto, dbg)
    nc, kk = _CACHE[key]
    f = lambda a: np.ascontiguousarray(np.asarray(a, dtype=np.float32))
    shared = {}
    shared["ln_in_g"] = f(inputs["ln_in_g"]).reshape(1, D)
    shared["ln_in_b"] = f(inputs["ln_in_b"]).reshape(1, D)
    shared["w_in_proj"] = f(inputs["w_in_proj"][0])
    shared["w_kr_sw"] = np.ascontiguousarray(shared["w_in_proj"][:, OFF_KR:OFF_KR + 32][:, swap_perm(1, 32, 0, 32)])
    shared["b_gate"] = f(inputs["b_gate"][0]).reshape(1, 3 * D)
    shared["gqa_q_norm"] = np.tile(f(inputs["gqa_q_norm"][0]), 8).reshape(1, 512)
    shared["gqa_k_norm"] = np.tile(f(inputs["gqa_k_norm"][0]), 2).reshape(1, 128)
    shared["mla_q_norm"] = f(inputs["mla_q_norm"][0]).reshape(1, 384)
    shared["mla_kv_norm"] = f(inputs["mla_kv_norm"][0]).reshape(1, 256)
    wqb = f(inputs["w_mla_qb"][0])
    shared["w_mla_qb"] = wqb
    shared["w_mla_qb_sw"] = np.ascontiguousarray(wqb[:, swap_perm(8, 96, 64, 32)])
    shared["w_mla_kvb"] = f(inputs["w_mla_kvb"][0])
    shared["w_mem_kv"] = f(inputs["w_mem_kv"][0])
    shared["w_br_gqa"] = f(inputs["w_br_gqa"][0])
    shared["w_br_mla"] = f(inputs["w_br_mla"][0])
    shared["w_br_mem"] = f(inputs["w_br_mem"][0])
    shared["w_out"] = f(inputs["w_out"][0])
    shared["ln1_g"] = f(inputs["ln1_g"][0]).reshape(1, D)
    shared["ln1_b"] = f(inputs["ln1_b"][0]).reshape(1, D)
    shared["w_router"] = f(inputs["w_router"][0])
    shared["b_router"] = f(inputs["b_router"][0]).reshape(1, NE)
    shared["w_exp_in"] = f(inputs["w_exp_in"][0])
    shared["b_exp_in"] = np.ascontiguousarray(
        f(inputs["b_exp_in"][0]).reshape(NE, 16, 128).transpose(2, 0, 1).reshape(128, NE * 16))
    shared["w_exp_out"] = f(inputs["w_exp_out"][0])
    shared["b_exp_out"] = f(inputs["b_exp_out"][0])
    shared["ln2_g"] = f(inputs["ln2_g"][0]).reshape(1, D)
    shared["ln2_b"] = f(inputs["ln2_b"][0]).reshape(1, D)
    shared.update(host_consts())
    x = f(inputs["x"])
    mem = f(inputs["mem"])
    in_maps = []
    for c in range(ncores):
        m = {n: shared[n] for n in kk.inputs if n in shared}
        m["x"] = x[c]
        if "mem" in kk.inputs:
            m["mem"] = mem[c]
        in_maps.append(m)
    res = run_bass_kernel_spmd(nc, in_maps, core_ids=list(range(ncores)))
    if dbg:
        return res.results
    return np.stack([r["y"] for r in res.results], axis=0)
```

```python
import numpy as np
import ml_dtypes
from contextlib import ExitStack, contextmanager

import concourse.bass as bass
import concourse.mybir as mybir
from concourse.bass_utils import run_bass_kernel_spmd

F32 = mybir.dt.float32
BF16 = mybir.dt.bfloat16
I32 = mybir.dt.int32
U32 = mybir.dt.uint32
ALU = mybir.AluOpType
AF = mybir.ActivationFunctionType
AX = mybir.AxisListType

S = 4096
D = 1024
NT = S // 128
NB = S // 512
MEM = 256
NE = 32
TOPK = 4
CAPT = 6
CAP = CAPT * 128
NSLOT = NE * CAP
ALPHA = 2.0 ** 0.25
RMS_EPS = 1e-6
LN_EPS = 1e-5
OFF_QA, OFF_KA, OFF_VA, OFF_QL, OFF_KVL, OFF_KR, OFF_QM, OFF_G = 0, 512, 640, 768, 1152, 1408, 1440, 1952
INW = 5024

ENGS = ("pe", "act", "dve", "pool", "sp")


class Sched:
    EPOCH = 30000

    def __init__(self, nc, es, ndma=None):
        self.nc, self.es = nc, es
        self.prog = {e: [] for e in ENGS}
        self.esem = {e: es.enter_context(nc.semaphore(f"e_{e}_0")) for e in ENGS}
        self.eold = {e: set() for e in ENGS}
        self.nep = {e: 0 for e in ENGS}
        self.ecnt = {e: 0 for e in ENGS}
        self.seen = {e: {} for e in ENGS}
        self.lastw, self.readers = {}, {}
        ndma = ndma or {"sp": 28, "pool": 24, "act": 6}
        self.dsem = {q: [es.enter_context(nc.semaphore(f"d_{q}_{i}")) for i in range(n)] for q, n in ndma.items()}
        self.duse = {q: [0] * n for q, n in ndma.items()}
        self.dnext = {q: 0 for q in ndma}
        self.ninstr = 0

    def _deps(self, e, R, W):
        deps = {}

        def add(tok):
            if tok is None:
                return
            s, v = tok
            if deps.get(id(s), (None, 0))[1] < v:
                deps[id(s)] = (s, v)
        for r in R:
            add(self.lastw.get(r))
        for w in W:
            add(self.lastw.get(w))
            for tok in self.readers.get(w, {}).values():
                add(tok)
        waits = []
        for k, (s, v) in deps.items():
            if id(s) in self.eold[e]:
                continue
            if s is self.esem[e] and e == "pe":
                continue
            if self.seen[e].get(k, 0) >= v:
                continue
            self.seen[e][k] = v
            waits.append((s, v))
        return waits

    def _record(self, tok, R, W):
        for w in W:
            self.lastw[w] = tok
            self.readers[w] = {}
        for r in R:
            d = self.readers.setdefault(r, {})
            if d.get(id(tok[0]), (None, 0))[1] < tok[1]:
                d[id(tok[0])] = tok

    def op(self, e, fn, R=(), W=()):
        waits = self._deps(e, R, W)
        if self.ecnt[e] >= self.EPOCH:
            self.eold[e].add(id(self.esem[e]))
            self.nep[e] += 1
            self.esem[e] = self.es.enter_context(self.nc.semaphore(f"e_{e}_{self.nep[e]}"))
            self.ecnt[e] = 0
        self.ecnt[e] += 1
        sem = self.esem[e]
        tok = (sem, self.ecnt[e])

        def emit(eng, waits=waits, fn=fn, sem=sem):
            for s, v in waits:
                eng.wait_ge(s, v)
            fn(eng).then_inc(sem, 1)
        self.prog[e].append(emit)
        self._record(tok, R, W)
        self.ninstr += 1 + len(waits)
        return tok

    def dma(self, q, fn, R=(), W=()):
        waits = self._deps(q, R, W)
        i = self.dnext[q]
        self.dnext[q] = (i + 1) % len(self.dsem[q])
        sem = self.dsem[q][i]
        prev = self.duse[q][i]
        if prev and self.seen[q].get(id(sem), 0) < 16 * prev:
            self.seen[q][id(sem)] = 16 * prev
            waits.append((sem, 16 * prev))
        self.duse[q][i] = prev + 1
        tok = (sem, 16 * (prev + 1))

        def emit(eng, waits=waits, fn=fn, sem=sem):
            for s, v in waits:
                eng.wait_ge(s, v)
            fn(eng).then_inc(sem, 16)
        self.prog[q].append(emit)
        self._record(tok, R, W)
        self.ninstr += 1 + len(waits)
        return tok

    def barrier(self):
        toks = [(self.esem[e], self.ecnt[e]) for e in ENGS if self.ecnt[e] > 0]
        for q in self.dsem:
            for i, s in enumerate(self.dsem[q]):
                if self.duse[q][i]:
                    toks.append((s, 16 * self.duse[q][i]))
        for e in ENGS:
            waits = [(s, v) for (s, v) in toks if self.seen[e].get(id(s), 0) < v]
            for s, v in waits:
                self.seen[e][id(s)] = v

            def emit(eng, waits=waits):
                for s, v in waits:
                    eng.wait_ge(s, v)
            self.prog[e].append(emit)
            self.ninstr += len(waits)

    def fence(self, e, toks):
        best = {}
        for s, v in toks:
            if best.get(id(s), (None, 0))[1] < v:
                best[id(s)] = (s, v)

        def emit(eng, best=best):
            for s, v in best.values():
                eng.wait_ge(s, v)
        self.prog[e].append(emit)

    def run(self):
        nc = self.nc
        with nc.Block() as block:
            @block.tensor
            def _(eng):
                for f in self.prog["pe"]:
                    f(eng)

            @block.scalar
            def _(eng):
                for f in self.prog["act"]:
                    f(eng)

            @block.vector
            def _(eng):
                for f in self.prog["dve"]:
                    f(eng)

            @block.gpsimd
            def _(eng):
                self.bc_reg = eng.to_reg(NSLOT - 1)
                for f in self.prog["pool"]:
                    f(eng)

            @block.sync
            def _(eng):
                for f in self.prog["sp"]:
                    f(eng)


@contextmanager
def scope(k):
    with ExitStack() as es:
        yield es
        k.sc.barrier()


def run_pipelined(gens, depth, skew=1):
    gens = iter(gens)
    active, rnd, done = [], 0, False
    while True:
        if not done and len(active) < depth and rnd % skew == 0:
            g = next(gens, None)
            if g is None:
                done = True
            else:
                active.append(g)
        if done and not active:
            break
        for g in list(active):
            try:
                next(g)
            except StopIteration:
                active.remove(g)
        rnd += 1


def bcast_rows(ap, n=128):
    return ap.partition_broadcast(n)


class K:
    pass


def build(upto=99, dbg=()):
    nc = bass.Bass("TRN2", target_bir_lowering=False)
    k = K()
    k.nc = nc
    k.inputs = []
    k.dbg = dbg

    def dt_in(name, shape, dt=F32):
        k.inputs.append(name)
        return nc.dram_tensor(name, list(shape), dt, kind="ExternalInput")
    k.x = dt_in("x", [S, D])
    k.ln_in_g = dt_in("ln_in_g", [1, D])
    k.ln_in_b = dt_in("ln_in_b", [1, D])
    k.c_ident = dt_in("c_ident", [128, 128], BF16)
    k.c_identf = dt_in("c_identf", [128, 128])
    if upto >= 2:
        k.mem = dt_in("mem", [MEM, D])
        k.w_in = dt_in("w_in_proj", [D, INW])
        k.w_kr_sw = dt_in("w_kr_sw", [D, 32])
        k.gqa_qn = dt_in("gqa_q_norm", [1, 512])
        k.gqa_kn = dt_in("gqa_k_norm", [1, 128])
        k.mla_qn = dt_in("mla_q_norm", [1, 384])
        k.mla_kvn = dt_in("mla_kv_norm", [1, 256])
        k.w_qb = dt_in("w_mla_qb", [384, 768])
        k.w_qb_sw = dt_in("w_mla_qb_sw", [384, 768])
        k.w_kvb = dt_in("w_mla_kvb", [256, 1024])
        k.w_mkv = dt_in("w_mem_kv", [D, D])
        k.c_rope64 = dt_in("c_rope64", [128, 2, NT, 64])
        k.c_ropeT = dt_in("c_ropeT", [128, 2, S])
        k.c_sel = dt_in("c_sel", [128, 128])
    if upto >= 5:
        k.b_gate = dt_in("b_gate", [1, 3 * D])
        k.w_br_a = dt_in("w_br_gqa", [512, D])
        k.w_br_b = dt_in("w_br_mla", [512, D])
        k.w_br_c = dt_in("w_br_mem", [512, D])
        k.w_out = dt_in("w_out", [D, D])
        k.ln1_g = dt_in("ln1_g", [1, D])
        k.ln1_b = dt_in("ln1_b", [1, D])
        k.w_router = dt_in("w_router", [D, NE])
        k.b_router = dt_in("b_router", [1, NE])
        k.c_tri = dt_in("c_tri", [128, 128], BF16)
        k.c_ecap = dt_in("c_ecap", [128, NE])
    if upto >= 6:
        k.w_ein = dt_in("w_exp_in", [NE, D, 2 * D])
        k.b_ein = dt_in("b_exp_in", [128, NE * 16])
        k.w_eout = dt_in("w_exp_out", [NE, D, D])
        k.b_eout = dt_in("b_exp_out", [NE, D])
        k.ln2_g = dt_in("ln2_g", [1, D])
        k.ln2_b = dt_in("ln2_b", [1, D])
    k.y = nc.dram_tensor("y", [S, D], F32, kind="ExternalOutput")
    sk = lambda name: ("ExternalOutput" if name in dbg else "Internal")
    k.h0_d = nc.dram_tensor("h0_d", [S, D], F32, kind=sk("h0_d"))
    k.h0T_d = nc.dram_tensor("h0T_d", [NT, 128, D], BF16, kind=sk("h0T_d"))
    k.oA_d = nc.dram_tensor("oA_d", [8, 64, S], BF16, kind=sk("oA_d"))
    k.oB_d = nc.dram_tensor("oB_d", [8, 64, S], BF16, kind=sk("oB_d"))
    k.oC_d = nc.dram_tensor("oC_d", [4, 128, S], BF16, kind=sk("oC_d"))
    k.h1_d = nc.dram_tensor("h1_d", [S, D], F32, kind=sk("h1_d"))
    k.xs_d = nc.dram_tensor("xs_d", [NSLOT, D], BF16, kind=sk("xs_d"))
    k.ys_d = nc.dram_tensor("ys_d", [NSLOT, D], F32, kind=sk("ys_d"))
    k.rt_d = nc.dram_tensor("rt_d", [S, 8], F32, kind=sk("rt_d"))

    with ExitStack() as es:
        sc = Sched(nc, es)
        k.sc = sc
        k.pp = [es.enter_context(nc.psum_tensor(f"pp{i}", [128, 1024], F32)) for i in range(4)]
        k.ppb = [p.bitcast(BF16) for p in k.pp]
        sb = lambda name, shape, dt=F32: es.enter_context(nc.sbuf_tensor(name, list(shape), dt))
        k.ident = sb("ident", [128, 128], BF16)
        k.identf = sb("identf", [128, 128], F32)
        k.neghalf = sb("neghalf", [128, 8], F32)
        k.ones_bf = sb("ones_bf", [128, 128], BF16)
        k.ones_f = sb("ones_f", [128, 128], F32)
        k.slots = sb("slots_all", [128, NT, 4], I32)
        k.wts = sb("wts_all", [128, NT, 4], F32)
        sc.dma("sp", lambda e: e.dma_start(out=k.ident[:, :], in_=k.c_ident[:, :]), W=["ident"])
        sc.dma("sp", lambda e: e.dma_start(out=k.identf[:, :], in_=k.c_identf[:, :]), W=["identf"])
        if upto >= 2:
            k.sel = sb("sel", [128, 128], F32)
            sc.dma("sp", lambda e: e.dma_start(out=k.sel[:, :], in_=k.c_sel[:, :]), W=["sel"])
        sc.op("pool", lambda e: e.memset(k.neghalf[:, :], -0.5), W=["neghalf"])
        sc.op("pool", lambda e: e.memset(k.ones_bf[:, :], 1.0), W=["ones_bf"])
        sc.op("pool", lambda e: e.memset(k.ones_f[:, :], 1.0), W=["ones_f"])

        phase1(k)
        if upto >= 2:
            phase2(k)
        if upto >= 3:
            phase3(k)
        if upto >= 4:
            phase4(k)
        if upto >= 5:
            phase5(k)
        if upto >= 6:
            phase6(k)
            phase7(k)
        toks = [t for r, t in sc.lastw.items() if isinstance(r, tuple) and r[0] in ("y", "dram")]
        sc.fence("sp", toks)
        sc.run()
    k.ninstr = sc.ninstr
    return nc, k


def psb(k, i):
    return k.pp[i // 2][:, (i % 2) * 512:(i % 2 + 1) * 512]


def psbf(k, i):
    return k.ppb[i // 2][:, (i % 2) * 1024:(i % 2 + 1) * 1024]


def layer_norm_tile(k, tag, z, g_rep, b_rep, out, scr, stages=False):
    g = _ln_gen(k, tag, z, g_rep, b_rep, out, scr)
    if stages:
        return g
    for _ in g:
        pass


def _ln_gen(k, tag, z, g_rep, b_rep, out, scr):
    sc = k.sc
    st, mv, rs = scr["st"], scr["mv"], scr["rs"]
    for h in range(2):
        sc.op("dve", lambda e, h=h: e.bn_stats(st[:, h * 6:(h + 1) * 6], z[:, h * 512:(h + 1) * 512]),
              R=[tag + "z"], W=[tag + f"st{h}"])
    sc.op("dve", lambda e: e.bn_aggr(mv[:, 0:2], st[:, 0:12]), R=[tag + "st0", tag + "st1"], W=[tag + "mv"])
    sc.op("dve", lambda e: e.tensor_scalar_add(rs[:, 0:1], mv[:, 1:2], LN_EPS), R=[tag + "mv"], W=[tag + "rs0"])
    yield
    sc.op("pool", lambda e: e.tensor_tensor(rs[:, 1:2], rs[:, 0:1], k.neghalf[:, 0:1], ALU.pow),
          R=[tag + "rs0", "neghalf"], W=[tag + "rs"])
    yield
    sc.op("dve", lambda e: e.tensor_scalar(out[:, :], z[:, :], mv[:, 0:1], rs[:, 1:2], ALU.subtract, ALU.mult),
          R=[tag + "z", tag + "mv", tag + "rs"], W=[tag + "o"])
    sc.op("dve", lambda e: e.tensor_tensor(out[:, :], out[:, :], g_rep[:, :], ALU.mult),
          R=[tag + "o", "lnconst"], W=[tag + "o"])
    sc.op("dve", lambda e: e.tensor_tensor(out[:, :], out[:, :], b_rep[:, :], ALU.add),
          R=[tag + "o", "lnconst"], W=[tag + "o"])
    yield


def phase1(k):
    nc, sc = k.nc, k.sc
    with scope(k) as es:
        sb = lambda name, shape, dt=F32: es.enter_context(nc.sbuf_tensor(name, list(shape), dt))
        g_rep = sb("p1_g", [128, D])
        b_rep = sb("p1_b", [128, D])
        sc.dma("sp", lambda e: e.dma_start(out=g_rep[:, :], in_=bcast_rows(k.ln_in_g[0, :])), W=["lnconst"])
        sc.dma("sp", lambda e: e.dma_start(out=b_rep[:, :], in_=bcast_rows(k.ln_in_b[0, :])), W=["lnconst"])
        NBUF = 3
        xt = [sb(f"p1_x{i}", [128, D]) for i in range(NBUF)]
        ht = [sb(f"p1_h{i}", [128, D]) for i in range(NBUF)]
        hb = [sb(f"p1_hb{i}", [128, D], BF16) for i in range(NBUF)]
        hT = [sb(f"p1_hT{i}", [128, D], BF16) for i in range(NBUF)]
        scr = [dict(st=sb(f"p1_st{i}", [128, 12]), mv=sb(f"p1_mv{i}", [128, 2]), rs=sb(f"p1_rs{i}", [128, 2]))
               for i in range(NBUF)]
        def tile(t):
            i = t % NBUF
            tag = f"p1_{i}_"
            sc.dma("sp", lambda e: e.dma_start(out=xt[i][:, :], in_=k.x[t * 128:(t + 1) * 128, :]), W=[tag + "z"])
            yield
            for _ in layer_norm_tile(k, tag, xt[i], g_rep, b_rep, ht[i], scr[i], stages=True):
                yield
            sc.dma("sp", lambda e: e.dma_start(out=k.h0_d[t * 128:(t + 1) * 128, :], in_=ht[i][:, :]),
                   R=[tag + "o"], W=[("dram", "h0", t)])
            sc.op("act", lambda e: e.copy(hb[i][:, :], ht[i][:, :]), R=[tag + "o"], W=[tag + "hb"])
            yield
            pst = psbf(k, t % 2)
            for c in range(8):
                sc.op("pe", lambda e, c=c: e.transpose(pst[:, c * 128:(c + 1) * 128], hb[i][:, c * 128:(c + 1) * 128], k.ident[:, :]),
                      R=[tag + "hb", "ident"], W=[f"ps{t % 2}"])
            yield
            sc.op("dve", lambda e: e.tensor_copy(hT[i][:, :], pst[:, :]), R=[f"ps{t % 2}"], W=[tag + "hT"])
            yield
            sc.dma("sp", lambda e: e.dma_start(out=k.h0T_d[t, :, :], in_=hT[i][:, :]), R=[tag + "hT"], W=[("dram", "h0T", t)])
        run_pipelined((tile(t) for t in range(NT)), depth=3, skew=3)


def mmacc(k, out, pairs, R, W):
    n = len(pairs)
    for i, (l, r) in enumerate(pairs):
        k.sc.op("pe", lambda e, l=l, r=r, i=i: e.matmul(out, l, r, start=(i == 0), stop=(i == n - 1)), R=R, W=W)


def attention(k, tag, units, nkt, qk_fn, v_fn, dv, scale, out_fn, aug, Rres, nh):
    nc, sc = k.nc, k.sc
    W = 512 * nh
    with scope(k) as es:
        sb = lambda name, shape, dt=F32: es.enter_context(nc.sbuf_tensor(name, list(shape), dt))
        pt = [sb(f"{tag}_pt{i}", [128, 1024], BF16) for i in range(2)]
        osb = [sb(f"{tag}_osb{i}", [128, W]) for i in range(2)]
        rec = [sb(f"{tag}_rec{i}", [128, W]) for i in range(2)]
        onb = [sb(f"{tag}_onb{i}", [128, W], BF16) for i in range(2)]
        den = [sb(f"{tag}_den{i}", [1, 512]) for i in range(2)] if not aug else None
        G = nkt // 2 if nh == 1 else nkt
        gs = [(ui, g) for ui in range(len(units)) for g in range(G)]
        M = dv + 1 if aug else dv
        slot_of = {}
        pending = []

        def kts(g):
            return (2 * g, 2 * g + 1) if nh == 1 else (g, g)

        def qk(idx):
            ui, g = gs[idx]
            u = units[ui]
            sl = idx % 2
            slot_of[idx] = sl
            for i in range(2):
                l, r = qk_fn(u, kts(g)[i], i)
                sc.op("pe", lambda e, l=l, r=r, b=2 * sl + i: e.matmul(psb(k, b), l, r, start=True, stop=True),
                      R=Rres, W=[f"ps{2 * sl + i}"])
            sc.op("act", lambda e, sl=sl: e.activation(out=pt[sl][:, :], in_=k.pp[sl][:, :], func=AF.Exp, scale=scale),
                  R=[f"ps{2 * sl}", f"ps{2 * sl + 1}"], W=[f"{tag}_pt{sl}"])

        def accb(ui, i):
            return 4 + ui % 2 if nh == 1 else 4 + i

        def epi2(ui, I):
            u = units[ui]
            s2 = ui % 2
            if nh == 2:
                bcb, bc_ap = [6, 7], k.pp[3][0:dv, 0:W]
            elif aug:
                bcb, bc_ap = [6 + ui % 2], psb(k, 6 + ui % 2)[0:dv, :]
            else:
                sl = (I + 1) % 2
                bcb, bc_ap = [2 * sl], k.pp[sl][0:dv, 0:W]
            for i in range(nh):
                b = bcb[i]
                if aug:
                    sc.op("pe", lambda e, b=b, i=i: e.matmul(psb(k, b)[0:dv, :], k.sel[0:M, 0:dv], osb[s2][0:M, i * 512:(i + 1) * 512], start=True, stop=True),
                          R=[f"{tag}_osb{s2}", "sel"], W=[f"ps{b}"])
                else:
                    sc.op("pe", lambda e, b=b: e.matmul(psb(k, b)[0:dv, :], k.ones_f[0:1, 0:dv], den[s2][0:1, :], start=True, stop=True),
                          R=[f"{tag}_den{s2}", "ones_f"], W=[f"ps{b}"])
            sc.op("dve", lambda e: e.reciprocal(rec[s2][0:dv, :], bc_ap),
                  R=[f"ps{b}" for b in bcb], W=[f"{tag}_rec{s2}"])
            sc.op("dve", lambda e: e.tensor_tensor(onb[s2][0:dv, :], osb[s2][0:dv, :], rec[s2][0:dv, :], ALU.mult),
                  R=[f"{tag}_osb{s2}", f"{tag}_rec{s2}"], W=[f"{tag}_onb{s2}"])
            src = onb[s2][0:dv, :] if nh == 1 else onb[s2][0:dv, :].rearrange("p (h t) -> p h t", h=2)
            sc.dma("sp", lambda e, src=src: e.dma_start(out=out_fn(u), in_=src),
                   R=[f"{tag}_onb{s2}"], W=[("dram", tag, u[0], u[1])])

        def pv(idx):
            ui, g = gs[idx]
            u = units[ui]
            sl = slot_of.pop(idx)
            s2 = ui % 2
            for i in range(2):
                kt = kts(g)[i]
                ob = accb(ui, i)
                first = (kt == 0) if nh == 2 else (kt == 0)
                last = (kt == nkt - 1)
                vv = v_fn(u, kt, i)
                sc.op("pe", lambda e, ob=ob, vv=vv, i=i, first=first, last=last: e.matmul(
                    psb(k, ob)[0:vv.shape[1], :], vv, pt[sl][:, i * 512:(i + 1) * 512], start=first, stop=last),
                    R=[f"{tag}_pt{sl}"] + Rres, W=[f"ps{ob}"])
                if not aug:
                    sc.op("pe", lambda e, kt=kt, i=i, first=first, last=last: e.matmul(
                        psb(k, 6 + ui % 2)[0:1, :], k.ones_bf[:, 0:1], pt[sl][:, i * 512:(i + 1) * 512], start=first, stop=last),
                        R=[f"{tag}_pt{sl}"], W=[f"ps{6 + ui % 2}"])
            if g == G - 1:
                if nh == 1:
                    sc.op("dve", lambda e: e.tensor_copy(osb[s2][0:M, :], psb(k, accb(ui, 0))[0:M, :]),
                          R=[f"ps{accb(ui, 0)}"], W=[f"{tag}_osb{s2}"])
                else:
                    sc.op("dve", lambda e: e.tensor_copy(osb[s2][0:M, :], k.pp[2][0:M, :]),
                          R=[f"ps{accb(ui, 0)}", f"ps{accb(ui, 1)}"], W=[f"{tag}_osb{s2}"])
                if not aug:
                    sc.op("dve", lambda e: e.tensor_copy(den[s2][0:1, :], psb(k, 6 + ui % 2)[0:1, :]),
                          R=[f"ps{6 + ui % 2}"], W=[f"{tag}_den{s2}"])
                pending.append((idx + 2, ui))

        n = len(gs)
        for idx in range(n + 1):
            if idx < n:
                qk(idx)
            while pending and pending[0][0] <= idx:
                epi2(pending.pop(0)[1], idx)
            if idx >= 1:
                pv(idx - 1)
        while pending:
            epi2(pending.pop(0)[1], n)


def rms_rope_tm(k, tag, src, srcres, nh, gain_rep, r64, t, scr, out_bf):
    sc = k.sc
    w = nh * 64
    sq, ss, r, qn, t1, t2 = scr["sq"], scr["ss"], scr["r"], scr["qn"], scr["t1"], scr["t2"]
    v3 = lambda ap: ap.rearrange("p (h d) -> p h d", h=nh)
    v5 = lambda ap: ap.rearrange("p (h b two j) -> p h b two j", h=nh, b=2, two=2, j=16)
    Ct = r64[:, 0, t, :].unsqueeze(1).broadcast_to([128, nh, 64])
    St = r64[:, 1, t, :].rearrange("p (b two j) -> p b two j", b=2, two=2, j=16)
    sc.op("act", lambda e: e.activation(out=sq[:, 0:w], in_=src, func=AF.Square), R=[srcres], W=[tag + "sq"])
    yield
    sc.op("dve", lambda e: e.reduce_sum(ss[:, 0:nh], v3(sq[:, 0:w]), AX.X), R=[tag + "sq"], W=[tag + "ss"])
    sc.op("dve", lambda e: e.tensor_scalar(ss[:, 0:nh], ss[:, 0:nh], 1.0 / 64, RMS_EPS, ALU.mult, ALU.add),
          R=[tag + "ss"], W=[tag + "ss"])
    yield
    sc.op("pool", lambda e: e.tensor_tensor(r[:, 0:nh], ss[:, 0:nh], k.neghalf[:, 0:nh], ALU.pow),
          R=[tag + "ss", "neghalf"], W=[tag + "r"])
    yield
    sc.op("dve", lambda e: e.tensor_tensor(v3(qn[:, 0:w]), v3(src), r[:, 0:nh].unsqueeze(2).broadcast_to([128, nh, 64]), ALU.mult),
          R=[srcres, tag + "r"], W=[tag + "qn"])
    sc.op("dve", lambda e: e.tensor_tensor(qn[:, 0:w], qn[:, 0:w], gain_rep[:, 0:w], ALU.mult),
          R=[tag + "qn", "gains"], W=[tag + "qn"])
    sc.op("dve", lambda e: e.tensor_tensor(v3(t1[:, 0:w]), v3(qn[:, 0:w]), Ct, ALU.mult),
          R=[tag + "qn", "r64"], W=[tag + "t1"])
    for half in range(2):
        sc.op("dve", lambda e, half=half: e.tensor_tensor(
            v5(t2[:, 0:w])[:, :, :, half, :], v5(qn[:, 0:w])[:, :, :, 1 - half, :],
            St[:, :, half, :].unsqueeze(1).broadcast_to([128, nh, 2, 16]), ALU.mult),
            R=[tag + "qn", "r64"], W=[tag + f"t2{half}"])
    sc.op("dve", lambda e: e.tensor_tensor(out_bf, t1[:, 0:w], t2[:, 0:w], ALU.add),
          R=[tag + "t1", tag + "t20", tag + "t21"], W=[tag + "out"])
    yield


def phase2(k):
    nc, sc = k.nc, k.sc
    with scope(k) as es:
        sb = lambda name, shape, dt=F32: es.enter_context(nc.sbuf_tensor(name, list(shape), dt))
        qT = sb("a_qT", [128, 4, S], BF16)
        kd = sb("a_kd", [128, 2, S], BF16)
        va = sb("a_v", [128, NT, 2, 128], BF16)
        with scope(k) as es2:
            sb2 = lambda name, shape, dt=F32: es2.enter_context(nc.sbuf_tensor(name, list(shape), dt))
            wA = sb2("a_w", [128, 8, 768], BF16)
            sc.dma("pool", lambda e: e.dma_start(out=wA[:, :, :], in_=k.w_in[:, 0:768].rearrange("(c p) n -> p c n", p=128)),
                   W=["a_w"])
            gq = sb2("a_gq", [128, 512])
            gk = sb2("a_gk", [128, 128])
            r64 = sb2("a_r64", [128, 2, NT, 64])
            sc.dma("sp", lambda e: e.dma_start(out=gq[:, :], in_=bcast_rows(k.gqa_qn[0, :])), W=["gains"])
            sc.dma("sp", lambda e: e.dma_start(out=gk[:, :], in_=bcast_rows(k.gqa_kn[0, :])), W=["gains"])
            sc.dma("sp", lambda e: e.dma_start(out=r64[:, :, :, :], in_=k.c_rope64[:, :, :, :]), W=["r64"])
            sc.op("pool", lambda e: e.memset(va[:, :, :, 64:128], 0.0), W=["aV1"])
            sc.op("pool", lambda e: e.memset(va[:, :, :, 64:65], 1.0), W=["aV1"])
            hb = [sb2(f"a_hb{i}", [128, 4, D], BF16) for i in range(2)]
            mk = lambda i, w: dict(sq=sb2(f"a_sq{w}{i}", [128, w]), ss=sb2(f"a_ss{w}{i}", [128, 8]), r=sb2(f"a_r{w}{i}", [128, 8]),
                                   qn=sb2(f"a_qn{w}{i}", [128, w]), t1=sb2(f"a_t1{w}{i}", [128, w]), t2=sb2(f"a_t2{w}{i}", [128, w]))
            scq = [mk(i, 512) for i in range(2)]
            sck = [mk(i, 128) for i in range(2)]
            qr = [sb2(f"a_qr{i}", [128, 512], BF16) for i in range(2)]
            kr = [sb2(f"a_kr{i}", [128, 128], BF16) for i in range(2)]
            kdb = [sb2(f"a_kdb{i}", [128, 256], BF16) for i in range(2)]
            ptq = psbf(k, 4)
            ptk = psbf(k, 5)
            def load_hb(b):
                sc.dma("sp", lambda e: e.dma_start(out=hb[b % 2][:, :, :], in_=k.h0T_d[4 * b:4 * b + 4, :, :].rearrange("t p f -> p t f")),
                       R=[("dram", "h0T", 4 * b + j) for j in range(4)], W=[f"a_hb{b % 2}"])

            def tile(t):
                b, j = t // 4, t % 4
                hbb = hb[b % 2]
                if t == 0:
                    load_hb(0)
                if j == 0 and b + 1 < NB:
                    load_hb(b + 1)
                i2 = t % 2
                pa, pb = 2 * i2, 2 * i2 + 1
                mmacc(k, psb(k, pa), [(hbb[:, j, c * 128:(c + 1) * 128], wA[:, c, 0:512]) for c in range(8)],
                      R=[f"a_hb{b % 2}", "a_w"], W=[f"ps{pa}"])
                mmacc(k, psb(k, pb)[:, 0:256], [(hbb[:, j, c * 128:(c + 1) * 128], wA[:, c, 512:768]) for c in range(8)],
                      R=[f"a_hb{b % 2}", "a_w"], W=[f"ps{pb}"])
                yield
                tq, tk = f"a_q{i2}_", f"a_k{i2}_"
                gq_ = rms_rope_tm(k, tq, psb(k, pa), f"ps{pa}", 8, gq, r64, t, scq[i2], qr[i2][:, :])
                gk_ = rms_rope_tm(k, tk, psb(k, pb)[:, 0:128], f"ps{pb}", 2, gk, r64, t, sck[i2], kr[i2][:, :])
                sc.op("act", lambda e: e.copy(va[:, t, :, 0:64], psb(k, pb)[:, 128:256].rearrange("p (g d) -> p g d", g=2)),
                      R=[f"ps{pb}"], W=["aV"])
                for _ in zip(gq_, gk_):
                    yield
                for p in range(4):
                    sc.op("pe", lambda e, p=p: e.transpose(ptq[:, p * 128:(p + 1) * 128], qr[i2][:, p * 128:(p + 1) * 128], k.ident[:, :]),
                          R=[tq + "out", "ident"], W=["ps4"])
                kdv = kdb[i2][:, :].rearrange("p (g u d) -> p g u d", g=2, u=2, d=64)
                for u in range(2):
                    sc.op("act", lambda e, u=u: e.copy(kdv[:, :, u, :], kr[i2][:, :].rearrange("p (g d) -> p g d", g=2)),
                          R=[tk + "out"], W=[f"a_kdb{i2}_{u}"])
                yield
                sc.op("dve", lambda e: e.tensor_copy(qT[:, :, t * 128:(t + 1) * 128], ptq[:, 0:512].rearrange("p (c n) -> p c n", c=4)),
                      R=["ps4"], W=["aQ"])
                for g in range(2):
                    sc.op("pe", lambda e, g=g: e.transpose(ptk[:, g * 128:(g + 1) * 128], kdb[i2][:, g * 128:(g + 1) * 128], k.ident[:, :]),
                          R=[f"a_kdb{i2}_0", f"a_kdb{i2}_1", "ident"], W=["ps5"])
                yield
                sc.op("dve", lambda e: e.tensor_copy(kd[:, :, t * 128:(t + 1) * 128], ptk[:, 0:256].rearrange("p (c n) -> p c n", c=2)),
                      R=["ps5"], W=["aK"])
            run_pipelined((tile(t) for t in range(NT)), depth=2, skew=4)
        attention(k, "a", [(p, qb) for qb in range(NB) for p in range(4)], NT,
                  qk_fn=lambda u, kt, i: (kd[i * 64:i * 64 + 64, u[0] // 2, kt * 128:(kt + 1) * 128],
                                          qT[i * 64:i * 64 + 64, u[0], u[1] * 512:(u[1] + 1) * 512]),
                  v_fn=lambda u, kt, i: va[:, kt, u[0] // 2, :],
                  dv=64, scale=0.125,
                  out_fn=lambda u: k.oA_d[2 * u[0]:2 * u[0] + 2, :, u[1] * 512:(u[1] + 1) * 512].rearrange("h d t -> d h t"),
                  aug=True, Rres=["aQ", "aK", "aV", "aV1"], nh=2)


def phase3(k):
    nc, sc = k.nc, k.sc
    with scope(k) as es:
        sb = lambda name, shape, dt=F32: es.enter_context(nc.sbuf_tensor(name, list(shape), dt))
        qlT = sb("b_qlT", [128, 3, S], BF16)
        kvT = sb("b_kvT", [128, 2, S], BF16)
        kpe = sb("b_kpe", [128, S], BF16)
        rT = [sb(f"b_rT{i}", [128, 2, 512]) for i in range(2)]
        with scope(k) as es2:
            sb2 = lambda name, shape, dt=F32: es2.enter_context(nc.sbuf_tensor(name, list(shape), dt))
            ta = [sb2(f"b_ta{i}", [128, 512]) for i in range(2)]
            tb = [sb2(f"b_tb{i}", [128, 512]) for i in range(2)]
            wB = sb2("b_w", [128, 8, 672], BF16)
            sc.dma("pool", lambda e: e.dma_start(out=wB[:, :, :], in_=k.w_in[:, 768:1440].rearrange("(c p) n -> p c n", p=128)), W=["b_w"])
            wks = sb2("b_wks", [128, 8, 96], BF16)
            sc.op("pool", lambda e: e.memset(wks[:, :, 0:64], 0.0), W=["b_wks0"])
            sc.dma("pool", lambda e: e.dma_start(out=wks[:, :, 64:96], in_=k.w_kr_sw[:, :].rearrange("(c p) n -> p c n", p=128)), W=["b_wks1"])
            gql = sb2("b_gql", [128, 384])
            gkv = sb2("b_gkv", [128, 256])
            sc.dma("sp", lambda e: e.dma_start(out=gql[:, :], in_=bcast_rows(k.mla_qn[0, :])), W=["gains"])
            sc.dma("sp", lambda e: e.dma_start(out=gkv[:, :], in_=bcast_rows(k.mla_kvn[0, :])), W=["gains"])
            hb = [sb2(f"b_hb{i}", [128, 4, D], BF16) for i in range(3)]
            rT1 = [sb2(f"b_rT1{i}", [128, 2, 512]) for i in range(3)]
            sq = [sb2(f"b_sq{i}", [128, 512]) for i in range(2)]
            st = [sb2(f"b_st{i}", [128, 8]) for i in range(2)]
            qn = [sb2(f"b_qn{i}", [128, 384]) for i in range(2)]
            kn = [sb2(f"b_kn{i}", [128, 256]) for i in range(2)]
            qlb = [sb2(f"b_qlb{i}", [128, 384], BF16) for i in range(2)]
            kvb = [sb2(f"b_kvb{i}", [128, 256], BF16) for i in range(2)]
            ptq = psbf(k, 4)
            ptk = psbf(k, 5)
            def load_blk(b):
                sc.dma("sp", lambda e: e.dma_start(out=hb[b % 3][:, :, :], in_=k.h0T_d[4 * b:4 * b + 4, :, :].rearrange("t p f -> p t f")),
                       R=[("dram", "h0T", 4 * b + j) for j in range(4)], W=[f"b_hb{b % 3}"])
                sc.dma("sp", lambda e: e.dma_start(out=rT1[b % 3][:, :, :], in_=k.c_ropeT[:, :, b * 512:(b + 1) * 512]), W=[f"b_rT1{b % 3}"])

            def tile(t):
                b, j = t // 4, t % 4
                hbb, rTb = hb[b % 3], rT1[b % 3]
                blk = slice(b * 512, (b + 1) * 512)
                if t == 0:
                    load_blk(0)
                if j == 0 and b + 1 < NB:
                    load_blk(b + 1)
                i2 = t % 2
                pa, pb = 2 * i2, 2 * i2 + 1
                tg = f"b1_{i2}_"
                mmacc(k, psb(k, pa), [(hbb[:, j, c * 128:(c + 1) * 128], wB[:, c, 0:512]) for c in range(8)],
                      R=[f"b_hb{b % 3}", "b_w"], W=[f"ps{pa}"])
                mmacc(k, psb(k, pb)[:, 0:128], [(hbb[:, j, c * 128:(c + 1) * 128], wB[:, c, 512:640]) for c in range(8)],
                      R=[f"b_hb{b % 3}", "b_w"], W=[f"ps{pb}"])
                yield
                s_, st_ = sq[i2], st[i2]
                sc.op("act", lambda e: e.activation(out=s_[:, 0:384], in_=psb(k, pa)[:, 0:384], func=AF.Square, accum_out=st_[:, 0:1]),
                      R=[f"ps{pa}"], W=[tg + "sq", tg + "s0"])
                sc.op("act", lambda e: e.activation(out=s_[:, 384:512], in_=psb(k, pa)[:, 384:512], func=AF.Square, accum_out=st_[:, 1:2]),
                      R=[f"ps{pa}"], W=[tg + "sq2", tg + "s1"])
                sc.op("act", lambda e: e.activation(out=s_[:, 0:128], in_=psb(k, pb)[:, 0:128], func=AF.Square, accum_out=st_[:, 2:3]),
                      R=[f"ps{pb}", tg + "sq"], W=[tg + "sq", tg + "s2"])
                yield
                sc.op("dve", lambda e: e.tensor_scalar(st_[:, 3:4], st_[:, 0:1], 1.0 / 384, RMS_EPS, ALU.mult, ALU.add),
                      R=[tg + "s0"], W=[tg + "s3"])
                sc.op("dve", lambda e: e.tensor_tensor(st_[:, 4:5], st_[:, 1:2], st_[:, 2:3], ALU.add),
                      R=[tg + "s1", tg + "s2"], W=[tg + "s4"])
                sc.op("dve", lambda e: e.tensor_scalar(st_[:, 4:5], st_[:, 4:5], 1.0 / 256, RMS_EPS, ALU.mult, ALU.add),
                      R=[tg + "s4"], W=[tg + "s4"])
                yield
                sc.op("pool", lambda e: e.tensor_tensor(st_[:, 5:7], st_[:, 3:5], k.neghalf[:, 0:2], ALU.pow),
                      R=[tg + "s3", tg + "s4", "neghalf"], W=[tg + "r"])
                yield
                sc.op("dve", lambda e: e.tensor_scalar(qn[i2][:, :], psb(k, pa)[:, 0:384], st_[:, 5:6], None, ALU.mult),
                      R=[f"ps{pa}", tg + "r"], W=[tg + "qn"])
                sc.op("dve", lambda e: e.tensor_tensor(qlb[i2][:, :], qn[i2][:, :], gql[:, :], ALU.mult),
                      R=[tg + "qn", "gains"], W=[tg + "qlb"])
                sc.op("dve", lambda e: e.tensor_scalar(kn[i2][:, 0:128], psb(k, pa)[:, 384:512], st_[:, 6:7], None, ALU.mult),
                      R=[f"ps{pa}", tg + "r"], W=[tg + "kn0"])
                sc.op("dve", lambda e: e.tensor_scalar(kn[i2][:, 128:256], psb(k, pb)[:, 0:128], st_[:, 6:7], None, ALU.mult),
                      R=[f"ps{pb}", tg + "r"], W=[tg + "kn1"])
                sc.op("dve", lambda e: e.tensor_tensor(kvb[i2][:, :], kn[i2][:, :], gkv[:, :], ALU.mult),
                      R=[tg + "kn0", tg + "kn1", "gains"], W=[tg + "kvb"])
                yield
                for c in range(3):
                    sc.op("pe", lambda e, c=c: e.transpose(ptq[:, c * 128:(c + 1) * 128], qlb[i2][:, c * 128:(c + 1) * 128], k.ident[:, :]),
                          R=[tg + "qlb", "ident"], W=["ps4"])
                for c in range(2):
                    sc.op("pe", lambda e, c=c: e.transpose(ptk[:, c * 128:(c + 1) * 128], kvb[i2][:, c * 128:(c + 1) * 128], k.ident[:, :]),
                          R=[tg + "kvb", "ident"], W=["ps5"])
                yield
                sc.op("dve", lambda e: e.tensor_copy(qlT[:, :, t * 128:(t + 1) * 128], ptq[:, 0:384].rearrange("p (c n) -> p c n", c=3)),
                      R=["ps4"], W=["b_qlT"])
                sc.op("act", lambda e: e.copy(kvT[:, :, t * 128:(t + 1) * 128], ptk[:, 0:256].rearrange("p (c n) -> p c n", c=2)),
                      R=["ps5"], W=["b_kvT"])
                if j == 3:
                    yield
                    mmacc(k, psb(k, 6)[0:96, :], [(wB[:, c, 576:672], hbb[:, :, c * 128:(c + 1) * 128]) for c in range(8)],
                          R=[f"b_hb{b % 3}", "b_w"], W=["ps6"])
                    mmacc(k, psb(k, 7)[0:96, :], [(wks[:, c, 0:96], hbb[:, :, c * 128:(c + 1) * 128]) for c in range(8)],
                          R=[f"b_hb{b % 3}", "b_wks0", "b_wks1"], W=["ps7"])
                    yield
                    a_, b_ = ta[b % 2], tb[b % 2]
                    sc.op("dve", lambda e: e.tensor_tensor(a_[64:96, :], psb(k, 6)[64:96, :], rTb[64:96, 0, :], ALU.mult),
                          R=["ps6", f"b_rT1{b % 3}"], W=[f"b_ta{b % 2}"])
                    sc.op("dve", lambda e: e.tensor_tensor(b_[64:96, :], psb(k, 7)[64:96, :], rTb[64:96, 1, :], ALU.mult),
                          R=["ps7", f"b_rT1{b % 3}"], W=[f"b_tb{b % 2}"])
                    sc.op("dve", lambda e: e.tensor_tensor(kpe[64:96, blk], a_[64:96, :], b_[64:96, :], ALU.add),
                          R=[f"b_ta{b % 2}", f"b_tb{b % 2}"], W=["b_kpe"])
            run_pipelined((tile(t) for t in range(NT)), depth=2, skew=4)
        wqb = sb("b_wqb", [128, 3, 768], BF16)
        wqs = sb("b_wqs", [128, 3, 768], BF16)
        wkv = sb("b_wkv", [128, 2, 1024], BF16)
        sc.dma("pool", lambda e: e.dma_start(out=wqb[:, :, :], in_=k.w_qb[:, :].rearrange("(c p) n -> p c n", p=128)), W=["b_wqb"])
        sc.dma("pool", lambda e: e.dma_start(out=wqs[:, :, :], in_=k.w_qb_sw[:, :].rearrange("(c p) n -> p c n", p=128)), W=["b_wqs"])
        sc.dma("pool", lambda e: e.dma_start(out=wkv[:, :, :], in_=k.w_kvb[:, :].rearrange("(c p) n -> p c n", p=128)), W=["b_wkv"])
        qT = sb("b_qT", [128, 4, S], BF16)
        kT = sb("b_kT", [128, 4, S], BF16)
        vb = sb("b_v", [128, NT, 4, 128], BF16)
        sc.op("pool", lambda e: e.memset(vb[:, :, :, 64:128], 0.0), W=["bV1"])
        sc.op("pool", lambda e: e.memset(vb[:, :, :, 64:65], 1.0), W=["bV1"])
        sc.op("pool", lambda e: e.memset(qT[96:128, :, :], 0.0), W=["bV1"])
        sc.op("pool", lambda e: e.memset(kT[96:128, :, :], 0.0), W=["bV1"])
        ta4 = [sb(f"b_ta4{i}", [128, 512]) for i in range(4)]
        tb4 = [sb(f"b_tb4{i}", [128, 512]) for i in range(4)]
        for half in range(2):
            def blockgen(b, half=half):
                blk = slice(b * 512, (b + 1) * 512)
                rTb = rT[b % 2]
                sc.dma("sp", lambda e: e.dma_start(out=rTb[:, :, :], in_=k.c_ropeT[:, :, b * 512:(b + 1) * 512]), W=[f"b_rT{b % 2}"])
                yield
                for hl in range(4):
                    h = 4 * half + hl
                    pq, pswp, pn = hl % 2, 2 + hl % 2, 4 + hl % 2
                    x4 = (b % 2) * 2 + hl % 2
                    a_, b_ = ta4[x4], tb4[x4]
                    mmacc(k, psb(k, pq)[0:96, :], [(wqb[:, c, h * 96:(h + 1) * 96], qlT[:, c, blk]) for c in range(3)],
                          R=["b_wqb", "b_qlT"], W=[f"ps{pq}"])
                    mmacc(k, psb(k, pswp)[0:96, :], [(wqs[:, c, h * 96:(h + 1) * 96], qlT[:, c, blk]) for c in range(3)],
                          R=["b_wqs", "b_qlT"], W=[f"ps{pswp}"])
                    mmacc(k, psb(k, pn)[0:64, :], [(wkv[:, c, h * 128:h * 128 + 64], kvT[:, c, blk]) for c in range(2)],
                          R=["b_wkv", "b_kvT"], W=[f"ps{pn}"])
                    yield
                    sc.op("act", lambda e, hl=hl, pq=pq: e.copy(qT[0:64, hl, blk], psb(k, pq)[0:64, :]), R=[f"ps{pq}"], W=["bQ"])
                    sc.op("dve", lambda e, a_=a_, pq=pq: e.tensor_tensor(a_[64:96, :], psb(k, pq)[64:96, :], rTb[64:96, 0, :], ALU.mult),
                          R=[f"ps{pq}", f"b_rT{b % 2}"], W=[f"b_ta4{x4}"])
                    sc.op("dve", lambda e, b_=b_, pswp=pswp: e.tensor_tensor(b_[64:96, :], psb(k, pswp)[64:96, :], rTb[64:96, 1, :], ALU.mult),
                          R=[f"ps{pswp}", f"b_rT{b % 2}"], W=[f"b_tb4{x4}"])
                    sc.op("act", lambda e, hl=hl, pn=pn: e.copy(kT[0:64, hl, blk], psb(k, pn)[0:64, :]), R=[f"ps{pn}"], W=["bK"])
                    sc.op("act", lambda e, hl=hl: e.copy(kT[64:96, hl, blk], kpe[64:96, blk]), R=["b_kpe"], W=["bK"])
                    yield
                    sc.op("dve", lambda e, a_=a_, b_=b_, hl=hl: e.tensor_tensor(qT[64:96, hl, blk], a_[64:96, :], b_[64:96, :], ALU.add),
                          R=[f"b_ta4{x4}", f"b_tb4{x4}"], W=["bQ"])
                for j in range(4):
                    t = 4 * b + j
                    pv_ = 6 + t % 2
                    wv = [wkv[:, c, :].rearrange("p (h n) -> p h n", n=128)[:, 4 * half:4 * half + 4, 64:128] for c in range(2)]
                    mmacc(k, psb(k, pv_)[:, 0:256], [(kvT[:, c, t * 128:(t + 1) * 128], wv[c]) for c in range(2)],
                          R=["b_wkv", "b_kvT"], W=[f"ps{pv_}"])
                    yield
                    sc.op("dve", lambda e, t=t, pv_=pv_: e.tensor_copy(vb[:, t, :, 0:64], psb(k, pv_)[:, 0:256].rearrange("p (h d) -> p h d", h=4)),
                          R=[f"ps{pv_}"], W=["bV"])
            run_pipelined((blockgen(b) for b in range(NB)), depth=2, skew=5)
            attention(k, f"b{half}", [(hl, qb) for qb in range(NB) for hl in range(4)], NT,
                      qk_fn=lambda u, kt, i: (kT[:, u[0], kt * 128:(kt + 1) * 128], qT[:, u[0], u[1] * 512:(u[1] + 1) * 512]),
                      v_fn=lambda u, kt, i: vb[:, kt, u[0], :],
                      dv=64, scale=96.0 ** -0.5,
                      out_fn=lambda u, half=half: k.oB_d[4 * half + u[0], :, u[1] * 512:(u[1] + 1) * 512],
                      aug=True, Rres=["bQ", "bK", "bV", "bV1"], nh=1)


def phase4(k):
    nc, sc = k.nc, k.sc
    with scope(k) as es:
        sb = lambda name, shape, dt=F32: es.enter_context(nc.sbuf_tensor(name, list(shape), dt))
        qm = sb("c_q", [128, 4, S], BF16)
        mkT = sb("c_mk", [128, 4, MEM], BF16)
        mv = sb("c_mv", [128, 2, 4, 128], BF16)
        with scope(k) as es2:
            sb2 = lambda name, shape, dt=F32: es2.enter_context(nc.sbuf_tensor(name, list(shape), dt))
            wq = sb2("c_wq", [128, 8, 512], BF16)
            wm = sb2("c_wm", [128, 8, D], BF16)
            sc.dma("pool", lambda e: e.dma_start(out=wq[:, :, :], in_=k.w_in[:, 1440:1952].rearrange("(c p) n -> p c n", p=128)), W=["c_wq"])
            sc.dma("pool", lambda e: e.dma_start(out=wm[:, :, :], in_=k.w_mkv[:, :].rearrange("(c p) n -> p c n", p=128)), W=["c_wm"])
            memf = sb2("c_memf", [128, 2, D])
            memb = sb2("c_memb", [128, 2, D], BF16)
            memT = sb2("c_memT", [128, 8, MEM], BF16)
            sc.dma("sp", lambda e: e.dma_start(out=memf[:, :, :], in_=k.mem[:, :].rearrange("(t p) d -> p t d", p=128)), W=["c_memf"])
            sc.op("act", lambda e: e.copy(memb[:, :, :], memf[:, :, :]), R=["c_memf"], W=["c_memb"])
            pt4 = psbf(k, 4)
            pt5 = psbf(k, 5)
            for t in range(2):
                pt = pt4 if t == 0 else pt5
                for c in range(8):
                    sc.op("pe", lambda e, t=t, c=c, pt=pt: e.transpose(pt[:, c * 128:(c + 1) * 128], memb[:, t, c * 128:(c + 1) * 128], k.ident[:, :]),
                          R=["c_memb", "ident"], W=[f"ps{4 + t}"])
                sc.op("dve", lambda e, t=t, pt=pt: e.tensor_copy(memT[:, :, t * 128:(t + 1) * 128], pt[:, :].rearrange("p (c n) -> p c n", c=8)),
                      R=[f"ps{4 + t}"], W=["c_memT"])
            for h in range(4):
                pb_ = h % 2
                mmacc(k, psb(k, pb_)[:, 0:MEM], [(wm[:, c, h * 128:(h + 1) * 128], memT[:, c, :]) for c in range(8)],
                      R=["c_wm", "c_memT"], W=[f"ps{pb_}"])
                sc.op("act", lambda e, h=h, pb_=pb_: e.copy(mkT[:, h, :], psb(k, pb_)[:, 0:MEM]), R=[f"ps{pb_}"], W=["cK"])
            for t in range(2):
                mmacc(k, psb(k, 2 + t), [(memT[:, c, t * 128:(t + 1) * 128], wm[:, c, 512:1024]) for c in range(8)],
                      R=["c_wm", "c_memT"], W=[f"ps{2 + t}"])
                sc.op("dve", lambda e, t=t: e.tensor_copy(mv[:, t, :, :], psb(k, 2 + t).rearrange("p (h d) -> p h d", h=4)),
                      R=[f"ps{2 + t}"], W=["cV"])
            hb = [sb2(f"c_hb{i}", [128, 4, D], BF16) for i in range(2)]
            for b in range(NB):
                hbb = hb[b % 2]
                blk = slice(b * 512, (b + 1) * 512)
                sc.dma("sp", lambda e, b=b, hbb=hbb: e.dma_start(out=hbb[:, :, :], in_=k.h0T_d[4 * b:4 * b + 4, :, :].rearrange("t p f -> p t f")),
                       R=[("dram", "h0T", 4 * b + j) for j in range(4)], W=[f"c_hb{b % 2}"])
                for h in range(4):
                    pb_ = 4 + h % 2 if h < 2 else 6 + h % 2
                    mmacc(k, psb(k, pb_), [(wq[:, c, h * 128:(h + 1) * 128], hbb[:, :, c * 128:(c + 1) * 128]) for c in range(8)],
                          R=["c_wq", f"c_hb{b % 2}"], W=[f"ps{pb_}"])
                    eng = "act" if h % 2 == 0 else "dve"
                    if eng == "act":
                        sc.op("act", lambda e, h=h, blk=blk, pb_=pb_: e.copy(qm[:, h, blk], psb(k, pb_)), R=[f"ps{pb_}"], W=["cQ"])
                    else:
                        sc.op("dve", lambda e, h=h, blk=blk, pb_=pb_: e.tensor_copy(qm[:, h, blk], psb(k, pb_)), R=[f"ps{pb_}"], W=["cQ"])
        attention(k, "c", [(h, qb) for qb in range(NB) for h in range(4)], 2,
                  qk_fn=lambda u, kt, i: (mkT[:, u[0], kt * 128:(kt + 1) * 128], qm[:, u[0], u[1] * 512:(u[1] + 1) * 512]),
                  v_fn=lambda u, kt, i: mv[:, kt, u[0], :],
                  dv=128, scale=128.0 ** -0.5, out_fn=lambda u: k.oC_d[u[0], :, u[1] * 512:(u[1] + 1) * 512],
                  aug=False, Rres=["cQ", "cK", "cV"], nh=1)


def phase5(k):
    nc, sc = k.nc, k.sc
    BIG = float(1 << 22)
    with scope(k) as es:
        sb = lambda name, shape, dt=F32: es.enter_context(nc.sbuf_tensor(name, list(shape), dt))
        wg = sb("m_wg", [128, 8, 3 * D], BF16)
        for i in range(3):
            sc.dma("pool", lambda e, i=i: e.dma_start(out=wg[:, :, i * D:(i + 1) * D],
                                                     in_=k.w_in[:, OFF_G + i * D:OFF_G + (i + 1) * D].rearrange("(c p) n -> p c n", p=128)),
                   W=[f"m_wg{i}"])
        bg = sb("m_bg", [128, 3 * D])
        sc.dma("sp", lambda e: e.dma_start(out=bg[:, :], in_=bcast_rows(k.b_gate[0, :])), W=["m_bg"])
        wa = sb("m_wa", [64, 8, D], BF16)
        wb_ = sb("m_wb", [64, 8, D], BF16)
        wc = sb("m_wc", [128, 4, D], BF16)
        wo = sb("m_wo", [128, 8, D], BF16)
        sc.dma("pool", lambda e: e.dma_start(out=wa[:, :, :], in_=k.w_br_a[:, :].rearrange("(h p) n -> p h n", p=64)), W=["m_wa"])
        sc.dma("pool", lambda e: e.dma_start(out=wb_[:, :, :], in_=k.w_br_b[:, :].rearrange("(h p) n -> p h n", p=64)), W=["m_wb"])
        sc.dma("pool", lambda e: e.dma_start(out=wc[:, :, :], in_=k.w_br_c[:, :].rearrange("(h p) n -> p h n", p=128)), W=["m_wc"])
        sc.dma("pool", lambda e: e.dma_start(out=wo[:, :, :], in_=k.w_out[:, :].rearrange("(c p) n -> p c n", p=128)), W=["m_wo"])
        wr = sb("m_wr", [128, 8, NE])
        sc.dma("sp", lambda e: e.dma_start(out=wr[:, :, :], in_=k.w_router[:, :].rearrange("(c p) n -> p c n", p=128)), W=["m_wr"])
        brr = sb("m_br", [128, NE])
        sc.dma("sp", lambda e: e.dma_start(out=brr[:, :], in_=bcast_rows(k.b_router[0, :])), W=["m_br"])
        g1 = sb("m_g1", [128, D])
        b1 = sb("m_b1", [128, D])
        sc.dma("sp", lambda e: e.dma_start(out=g1[:, :], in_=bcast_rows(k.ln1_g[0, :])), W=["lnconst"])
        sc.dma("sp", lambda e: e.dma_start(out=b1[:, :], in_=bcast_rows(k.ln1_b[0, :])), W=["lnconst"])
        tri = sb("m_tri", [128, 128], BF16)
        ecap = sb("m_ecap", [128, NE])
        sc.dma("sp", lambda e: e.dma_start(out=tri[:, :], in_=k.c_tri[:, :]), W=["m_tri"])
        sc.dma("sp", lambda e: e.dma_start(out=ecap[:, :], in_=k.c_ecap[:, :]), W=["m_ecap"])
        base = sb("m_base", [128, NE])
        sc.op("pool", lambda e: e.memset(base[:, :], 0.0), W=["m_base"])
        NB2 = 2
        hT = [sb(f"m_hT{i}", [128, D], BF16) for i in range(NB2)]
        oa = [sb(f"m_oa{i}", [64, 8, 128], BF16) for i in range(NB2)]
        ob = [sb(f"m_ob{i}", [64, 8, 128], BF16) for i in range(NB2)]
        oc = [sb(f"m_oc{i}", [128, 4, 128], BF16) for i in range(NB2)]
        h0 = [sb(f"m_h0{i}", [128, D]) for i in range(NB2)]
        gs = [[sb(f"m_gs{i}_{g}", [128, 512]) for g in range(2)] for i in range(NB2)]
        mg = [sb(f"m_mg{i}", [128, D]) for i in range(NB2)]
        mgb = [sb(f"m_mgb{i}", [128, D], BF16) for i in range(NB2)]
        mT = [sb(f"m_mT{i}", [128, D], BF16) for i in range(NB2)]
        h1 = [sb(f"m_h1{i}", [128, D]) for i in range(NB2)]
        h1b = [sb(f"m_h1b{i}", [128, D], BF16) for i in range(NB2)]
        h1T = sb("m_h1T", [128, D])
        scr = [dict(st=sb(f"m_st{i}", [128, 12]), mv=sb(f"m_mv{i}", [128, 2]), rs=sb(f"m_rs{i}", [128, 2])) for i in range(NB2)]
        lg = sb("m_lg", [128, NE])
        v8 = sb("m_v8", [128, 8])
        sm = sb("m_sm", [128, 16])
        ex = sb("m_ex", [128, 4])
        oh = sb("m_oh", [128, 4, NE])
        prod = sb("m_prod", [128, 4, NE])
        mask = sb("m_mask", [128, NE])
        maskb = sb("m_maskb", [128, NE], BF16)
        rank = sb("m_rank", [128, NE])
        rk = sb("m_rk", [128, 4])
        ek = sb("m_ek", [128, 4])
        vl = sb("m_vl", [128, 4])
        sl = sb("m_sl", [128, 4])
        rtd = sb("m_rtd", [128, 8])
        brs = [(oa, wa, 8, 64, k.oA_d), (ob, wb_, 8, 64, k.oB_d), (oc, wc, 4, 128, k.oC_d)]

        def tile(t):
            i2 = t % NB2
            tk = slice(t * 128, (t + 1) * 128)
            T = f"m{i2}_"
            sc.dma("sp", lambda e: e.dma_start(out=hT[i2][:, :], in_=k.h0T_d[t, :, :]), R=[("dram", "h0T", t)], W=[T + "hT"])
            sc.dma("sp", lambda e: e.dma_start(out=h0[i2][:, :], in_=k.h0_d[tk, :]), R=[("dram", "h0", t)], W=[T + "h0"])
            for bi, (ot, _, nh, kr_, od) in enumerate(brs):
                tagn = "abc"[bi]
                rd = [r for r in sc.lastw if isinstance(r, tuple) and r[0] == "dram" and str(r[1]).startswith(tagn) and r[3] == t // 4]
                sc.dma("sp", lambda e, ot=ot, od=od: e.dma_start(out=ot[i2][:, :, :], in_=od[:, :, tk].rearrange("h d t -> d h t")),
                       R=rd, W=[T + "o" + tagn])
            yield
            pbase = 4 * i2
            for bi, (ot, wbr, nh, kr_, od) in enumerate(brs):
                tagn = "abc"[bi]
                for half in range(2):
                    gi = half
                    col = slice(bi * D + half * 512, bi * D + half * 512 + 512)
                    hs = slice(half * 512, (half + 1) * 512)
                    pg, pbr = pbase + gi, pbase + 2 + gi
                    g_ = gs[i2][gi]
                    mmacc(k, psb(k, pg), [(hT[i2][:, c * 128:(c + 1) * 128], wg[:, c, col]) for c in range(8)],
                          R=[T + "hT", f"m_wg{bi}"], W=[f"ps{pg}"])
                    mmacc(k, psb(k, pbr), [(ot[i2][0:kr_, h, :], wbr[0:kr_, h, hs]) for h in range(nh)],
                          R=[T + "o" + tagn, f"m_w{tagn}"], W=[f"ps{pbr}"])
                    sc.op("dve", lambda e, g_=g_, pg=pg, col=col: e.tensor_tensor(g_[:, :], psb(k, pg), bg[:, col], ALU.add),
                          R=[f"ps{pg}", "m_bg"], W=[T + f"gs{gi}"])
                    sc.op("act", lambda e, g_=g_: e.activation(out=g_[:, :], in_=g_[:, :], func=AF.Sigmoid),
                          R=[T + f"gs{gi}"], W=[T + f"gs{gi}"])
                    yield
                    if bi == 0:
                        sc.op("dve", lambda e, g_=g_, pbr=pbr, hs=hs: e.tensor_tensor(mg[i2][:, hs], g_[:, :], psb(k, pbr), ALU.mult),
                              R=[T + f"gs{gi}", f"ps{pbr}"], W=[T + f"mg{half}"])
                    else:
                        sc.op("dve", lambda e, g_=g_, pbr=pbr: e.tensor_tensor(g_[:, :], g_[:, :], psb(k, pbr), ALU.mult),
                              R=[T + f"gs{gi}", f"ps{pbr}"], W=[T + f"gs{gi}"])
                        sc.op("dve", lambda e, g_=g_, hs=hs: e.tensor_tensor(mg[i2][:, hs], mg[i2][:, hs], g_[:, :], ALU.add),
                              R=[T + f"gs{gi}", T + f"mg{half}"], W=[T + f"mg{half}"])
            yield
            sc.op("act", lambda e: e.copy(mgb[i2][:, :], mg[i2][:, :]), R=[T + "mg0", T + "mg1"], W=[T + "mgb"])
            yield
            ptb = psbf(k, pbase)
            for c in range(8):
                sc.op("pe", lambda e, c=c: e.transpose(ptb[:, c * 128:(c + 1) * 128], mgb[i2][:, c * 128:(c + 1) * 128], k.ident[:, :]),
                      R=[T + "mgb", "ident"], W=[f"ps{pbase}"])
            yield
            sc.op("dve", lambda e: e.tensor_copy(mT[i2][:, :], ptb[:, :]), R=[f"ps{pbase}"], W=[T + "mT"])
            yield
            for half in range(2):
                hs = slice(half * 512, (half + 1) * 512)
                pb_ = pbase + 1 + half
                mmacc(k, psb(k, pb_), [(mT[i2][:, c * 128:(c + 1) * 128], wo[:, c, hs]) for c in range(8)],
                      R=[T + "mT", "m_wo"], W=[f"ps{pb_}"])
            yield
            tag = T + "ln_"
            for half in range(2):
                hs = slice(half * 512, (half + 1) * 512)
                pb_ = pbase + 1 + half
                sc.op("dve", lambda e, hs=hs, pb_=pb_: e.scalar_tensor_tensor(mg[i2][:, hs], h0[i2][:, hs], ALPHA, psb(k, pb_), ALU.mult, ALU.add),
                      R=[f"ps{pb_}", T + "h0"], W=[tag + "z", T + f"mg{half}"])
            yield
            for _ in layer_norm_tile(k, tag, mg[i2], g1, b1, h1[i2], scr[i2], stages=True):
                yield
            sc._record(sc.lastw[tag + "o"], [T + "mg0", T + "mg1"], [])
            sc.dma("sp", lambda e: e.dma_start(out=k.h1_d[tk, :], in_=h1[i2][:, :]), R=[tag + "o"], W=[("dram", "h1", t)])
            sc.op("act", lambda e: e.copy(h1b[i2][:, :], h1[i2][:, :]), R=[tag + "o"], W=[T + "h1b"])
            for c in range(8):
                sc.op("pe", lambda e, c=c: e.transpose(k.pp[pbase // 2 + 1][:, c * 128:(c + 1) * 128], h1[i2][:, c * 128:(c + 1) * 128], k.identf[:, :]),
                      R=[tag + "o", "identf"], W=[f"ps{pbase + 2}", f"ps{pbase + 3}"])
            yield
            sc.op("dve", lambda e: e.tensor_copy(h1T[:, :], k.pp[pbase // 2 + 1][:, :]), R=[f"ps{pbase + 2}", f"ps{pbase + 3}"], W=["m_h1T"])
            yield
            pl_ = pbase
            mmacc(k, psb(k, pl_)[:, 0:NE], [(h1T[:, c * 128:(c + 1) * 128], wr[:, c, :]) for c in range(8)],
                  R=["m_h1T", "m_wr"], W=[f"ps{pl_}"])
            yield
            sc.op("dve", lambda e: e.tensor_tensor(lg[:, :], psb(k, pl_)[:, 0:NE], brr[:, :], ALU.add), R=[f"ps{pl_}", "m_br"], W=["m_lg"])
            sc.op("dve", lambda e: e.max(v8[:, :], lg[:, :]), R=["m_lg"], W=["m_v8"])
            sc.op("dve", lambda e: e.tensor_scalar_mul(sm[:, 0:1], v8[:, 0:1], -1.0), R=["m_v8"], W=["m_sm0"])
            for kk in range(4):
                sc.op("dve", lambda e, kk=kk: e.tensor_scalar(oh[:, kk, :], lg[:, :], v8[:, kk:kk + 1], None, ALU.is_equal),
                      R=["m_lg", "m_v8"], W=[f"m_oh{kk}"])
            ohr = [f"m_oh{kk}" for kk in range(4)]
            sc.op("act", lambda e: e.activation(out=ex[:, :], in_=v8[:, 0:4], func=AF.Exp, bias=sm[:, 0:1], accum_out=sm[:, 1:2]),
                  R=["m_v8", "m_sm0"], W=["m_ex", "m_sm1"])
            sc.op("dve", lambda e: e.tensor_tensor(mask[:, :], oh[:, 0, :], oh[:, 1, :], ALU.add), R=ohr, W=["m_mask"])
            sc.op("dve", lambda e: e.tensor_tensor(mask[:, :], mask[:, :], oh[:, 2, :], ALU.add), R=ohr + ["m_mask"], W=["m_mask"])
            sc.op("dve", lambda e: e.tensor_tensor(maskb[:, :], mask[:, :], oh[:, 3, :], ALU.add), R=ohr + ["m_mask"], W=["m_maskb"])
            yield
            sc.op("pe", lambda e: e.matmul(psb(k, pl_)[:, 64:64 + NE], tri[:, :], maskb[:, :], start=True, stop=True),
                  R=["m_maskb", "m_tri"], W=[f"ps{pl_}b"])
            sc.op("pe", lambda e: e.matmul(psb(k, pl_)[:, 128:128 + NE], k.ones_bf[:, :], maskb[:, :], start=True, stop=True),
                  R=["m_maskb", "ones_bf"], W=[f"ps{pl_}c"])
            yield
            sc.op("dve", lambda e: e.tensor_tensor(rank[:, :], psb(k, pl_)[:, 64:64 + NE], base[:, :], ALU.add), R=[f"ps{pl_}b", "m_base"], W=["m_rank"])
            sc.op("dve", lambda e: e.tensor_tensor(base[:, :], psb(k, pl_)[:, 128:128 + NE], base[:, :], ALU.add), R=[f"ps{pl_}c", "m_base", "m_rank"], W=["m_base"])
            sc._record(sc.lastw["m_base"], [f"ps{pl_}"], [])
            sc.op("dve", lambda e: e.tensor_tensor(prod[:, :, :], oh[:, :, :], rank[:, :].unsqueeze(1).broadcast_to([128, 4, NE]), ALU.mult),
                  R=ohr + ["m_rank"], W=["m_prod"])
            sc.op("dve", lambda e: e.reduce_sum(rk[:, :], prod[:, :, :], AX.X), R=["m_prod"], W=["m_rk"])
            sc.op("dve", lambda e: e.tensor_tensor(prod[:, :, :], oh[:, :, :], ecap[:, :].unsqueeze(1).broadcast_to([128, 4, NE]), ALU.mult),
                  R=ohr + ["m_ecap", "m_rk"], W=["m_prod"])
            sc.op("dve", lambda e: e.reduce_sum(ek[:, :], prod[:, :, :], AX.X), R=["m_prod"], W=["m_ek"])
            sc.op("dve", lambda e: e.tensor_single_scalar(vl[:, :], rk[:, :], float(CAP), ALU.is_lt), R=["m_rk"], W=["m_vl"])
            sc.op("dve", lambda e: e.tensor_tensor(sl[:, :], rk[:, :], ek[:, :], ALU.add), R=["m_rk", "m_ek"], W=["m_sl"])
            sc.op("dve", lambda e: e.tensor_scalar_add(sl[:, :], sl[:, :], -BIG), R=["m_sl"], W=["m_sl"])
            sc.op("dve", lambda e: e.tensor_tensor(sl[:, :], sl[:, :], vl[:, :], ALU.mult), R=["m_sl", "m_vl"], W=["m_sl"])
            sc.op("dve", lambda e: e.tensor_scalar_add(rtd[:, 0:4], sl[:, :], BIG), R=["m_sl"], W=["m_rtd0"])
            sc.op("dve", lambda e: e.tensor_copy(k.slots[:, t, :], rtd[:, 0:4]), R=["m_rtd0"], W=[("slots", t)])
            sc.op("dve", lambda e: e.reciprocal(sm[:, 2:3], sm[:, 1:2]), R=["m_sm1"], W=["m_sm2"])
            sc.op("dve", lambda e: e.tensor_scalar(ex[:, :], ex[:, :], sm[:, 2:3], None, ALU.mult), R=["m_ex", "m_sm2"], W=["m_ex"])
            sc.op("dve", lambda e: e.tensor_tensor(k.wts[:, t, :], ex[:, :], vl[:, :], ALU.mult), R=["m_ex", "m_vl"], W=[("wts", t)])
            yield
            if k.dbg:
                sc.op("dve", lambda e: e.tensor_copy(rtd[:, 4:8], k.wts[:, t, :]), R=[("wts", t)], W=["m_rtd1"])
                sc.dma("sp", lambda e: e.dma_start(out=k.rt_d[tk, :], in_=rtd[:, :]), R=["m_rtd0", "m_rtd1"], W=[("dram", "rt", t)])
            for kk in range(4):
                sc.dma("pool", lambda e, kk=kk: e.indirect_dma_start(
                    out=k.xs_d[:, :], out_offset=bass.IndirectOffsetOnAxis(ap=k.slots[:, t, kk:kk + 1], axis=0),
                    in_=h1b[i2][:, :], in_offset=None, bounds_check=sc.bc_reg, oob_is_err=False),
                    R=[("slots", t), T + "h1b"], W=[("dram", "xs", t, kk)])
        run_pipelined((tile(t) for t in range(NT)), depth=2, skew=12)


def phase6(k):
    nc, sc = k.nc, k.sc
    with scope(k) as es:
        sb = lambda name, shape, dt=F32: es.enter_context(nc.sbuf_tensor(name, list(shape), dt))
        w1 = [sb(f"e_w1{i}", [128, 8, 2 * D], BF16) for i in range(2)]
        w2 = [sb(f"e_w2{i}", [128, 8, D], BF16) for i in range(2)]
        bo = [sb(f"e_bo{i}", [128, D]) for i in range(2)]
        bi_ = sb("e_bi", [128, NE * 16])
        sc.dma("sp", lambda e: e.dma_start(out=bi_[:, :], in_=k.b_ein[:, :]), W=["e_bi"])
        bi1 = sb("e_bi1", [128, NE * 16])
        sc.op("dve", lambda e: e.tensor_scalar_add(bi1[:, :], bi_[:, :], 1.0), R=["e_bi"], W=["e_bi1"])
        CH = 3
        NCH = CAPT // CH
        N = CH * 128
        xs = [sb(f"e_xs{i}", [128, CH, D], BF16) for i in range(2)]
        xT = [sb(f"e_xT{i}", [128, 8, N], BF16) for i in range(2)]
        aT = [sb(f"e_aT{i}", [128, 8, N], BF16) for i in range(2)]
        t1 = [sb(f"e_t1{i}", [128, N]) for i in range(2)]
        t2 = [sb(f"e_t2{i}", [128, N]) for i in range(2)]
        sg = [sb(f"e_sg{i}", [128, N]) for i in range(2)]
        ys = [sb(f"e_ys{i}", [128, D]) for i in range(2)]
        pt = [psbf(k, 4), psbf(k, 5)]
        xs_reads = [r for r in sc.lastw if isinstance(r, tuple) and r[0] == "dram" and r[1] == "xs"]
        gchunk = 0

        def load_xs(g):
            s0_ = (g // NCH) * CAP + (g % NCH) * N
            sc.dma("sp", lambda e: e.dma_start(out=xs[g % 2][:, :, :], in_=k.xs_d[s0_:s0_ + N, :].rearrange("(t p) d -> p t d", p=128)),
                   R=xs_reads if g == 0 else [], W=[f"e_xs{g % 2}"])
        for e_ in range(NE):
            wi = e_ % 2
            for hf in range(2):
                sc.dma("pool", lambda e, e_=e_, wi=wi, hf=hf: e.dma_start(
                    out=w1[wi][:, :, hf * D:(hf + 1) * D], in_=k.w_ein[e_, :, hf * D:(hf + 1) * D].rearrange("(c p) n -> p c n", p=128)),
                    W=[f"e_w1{wi}_{hf}"])
            sc.dma("pool", lambda e, e_=e_, wi=wi: e.dma_start(out=w2[wi][:, :, :], in_=k.w_eout[e_, :, :].rearrange("(c p) n -> p c n", p=128)),
                   W=[f"e_w2{wi}"])
            sc.dma("pool", lambda e, e_=e_, wi=wi: e.dma_start(out=bo[wi][:, :], in_=bcast_rows(k.b_eout[e_, :])), W=[f"e_bo{wi}"])
            for ch in range(NCH):
                ci = gchunk % 2
                gchunk += 1
                s0 = e_ * CAP + ch * N
                if gchunk == 1:
                    load_xs(0)
                if gchunk < NE * NCH:
                    load_xs(gchunk)
                for st in range(CH):
                    for c in range(8):
                        sc.op("pe", lambda e, st=st, c=c, ci=ci: e.transpose(pt[st % 2][:, c * 128:(c + 1) * 128], xs[ci][:, st, c * 128:(c + 1) * 128], k.ident[:, :]),
                              R=[f"e_xs{ci}", "ident"], W=[f"ps{4 + st % 2}"])
                    sc.op("dve" if st % 2 == 0 else "act",
                          (lambda e, st=st, ci=ci: e.tensor_copy(xT[ci][:, :, st * 128:(st + 1) * 128], pt[st % 2][:, :].rearrange("p (c n) -> p c n", c=8)))
                          if st % 2 == 0 else
                          (lambda e, st=st, ci=ci: e.copy(xT[ci][:, :, st * 128:(st + 1) * 128], pt[st % 2][:, :].rearrange("p (c n) -> p c n", c=8))),
                          R=[f"ps{4 + st % 2}"], W=[f"e_xT{ci}_{st}"])
                xTr = [f"e_xT{ci}_{st}" for st in range(CH)]
                for j in range(8):
                    ji = j % 2
                    pg, pl = ji, 2 + ji
                    mmacc(k, psb(k, pg)[:, 0:N], [(w1[wi][:, c, j * 128:(j + 1) * 128], xT[ci][:, c, :]) for c in range(8)],
                          R=xTr + [f"e_w1{wi}_0"], W=[f"ps{pg}"])
                    mmacc(k, psb(k, pl)[:, 0:N], [(w1[wi][:, c, D + j * 128:D + (j + 1) * 128], xT[ci][:, c, :]) for c in range(8)],
                          R=xTr + [f"e_w1{wi}_1"], W=[f"ps{pl}"])
                    bg_ = bi_[:, e_ * 16 + j:e_ * 16 + j + 1]
                    bl_ = bi1[:, e_ * 16 + 8 + j:e_ * 16 + 8 + j + 1]
                    sc.op("dve", lambda e, ji=ji, pg=pg, bg_=bg_: e.tensor_scalar(t1[ji][:, :], psb(k, pg)[:, 0:N], bg_, 7.0, ALU.add, ALU.min),
                          R=[f"ps{pg}", "e_bi"], W=[f"e_t1{ji}"])
                    sc.op("act", lambda e, ji=ji: e.activation(out=sg[ji][:, :], in_=t1[ji][:, :], func=AF.Sigmoid, scale=1.702),
                          R=[f"e_t1{ji}"], W=[f"e_sg{ji}"])
                    sc.op("dve", lambda e, ji=ji, pl=pl, bl_=bl_: e.tensor_scalar(t2[ji][:, :], psb(k, pl)[:, 0:N], bl_, 8.0, ALU.add, ALU.min),
                          R=[f"ps{pl}", "e_bi1"], W=[f"e_t2{ji}"])
                    sc.op("dve", lambda e, ji=ji: e.tensor_tensor(t1[ji][:, :], t1[ji][:, :], sg[ji][:, :], ALU.mult),
                          R=[f"e_t1{ji}", f"e_sg{ji}"], W=[f"e_t1{ji}"])
                    sc.op("dve", lambda e, ji=ji, j=j, ci=ci: e.scalar_tensor_tensor(aT[ci][:, j, :], t2[ji][:, :], -6.0, t1[ji][:, :], ALU.max, ALU.mult),
                          R=[f"e_t1{ji}", f"e_t2{ji}"], W=[f"e_aT{ci}_{j}"])
                aTr = [f"e_aT{ci}_{j}" for j in range(8)]
                for st in range(CH):
                    yi = st % 2
                    for half in range(2):
                        pb_ = 6 + half
                        mmacc(k, psb(k, pb_), [(aT[ci][:, j, st * 128:(st + 1) * 128], w2[wi][:, j, half * 512:(half + 1) * 512]) for j in range(8)],
                              R=aTr + [f"e_w2{wi}"], W=[f"ps{pb_}"])
                        sc.op("dve", lambda e, yi=yi, half=half, pb_=pb_, wi=wi: e.tensor_tensor(
                            ys[yi][:, half * 512:(half + 1) * 512], psb(k, pb_), bo[wi][:, half * 512:(half + 1) * 512], ALU.add),
                            R=[f"ps{pb_}", f"e_bo{wi}"], W=[f"e_ys{yi}_{half}"])
                    r0 = s0 + st * 128
                    sc.dma("sp", lambda e, yi=yi, r0=r0: e.dma_start(out=k.ys_d[r0:r0 + 128, :], in_=ys[yi][:, :]),
                           R=[f"e_ys{yi}_0", f"e_ys{yi}_1"], W=[("dram", "ys", r0)])


def phase7(k):
    nc, sc = k.nc, k.sc
    with scope(k) as es:
        sb = lambda name, shape, dt=F32: es.enter_context(nc.sbuf_tensor(name, list(shape), dt))
        g2 = sb("f_g2", [128, D])
        b2 = sb("f_b2", [128, D])
        sc.dma("sp", lambda e: e.dma_start(out=g2[:, :], in_=bcast_rows(k.ln2_g[0, :])), W=["lnconst"])
        sc.dma("sp", lambda e: e.dma_start(out=b2[:, :], in_=bcast_rows(k.ln2_b[0, :])), W=["lnconst"])
        NB2 = 3
        h1 = [sb(f"f_h1{i}", [128, D]) for i in range(NB2)]
        yk = [[sb(f"f_yk{i}_{kk}", [128, D]) for kk in range(4)] for i in range(NB2)]
        acc = [sb(f"f_acc{i}", [128, D]) for i in range(NB2)]
        out = [sb(f"f_out{i}", [128, D]) for i in range(NB2)]
        scr = [dict(st=sb(f"f_st{i}", [128, 12]), mv=sb(f"f_mv{i}", [128, 2]), rs=sb(f"f_rs{i}", [128, 2])) for i in range(NB2)]
        ys_w = [r for r in sc.lastw if isinstance(r, tuple) and r[0] == "dram" and r[1] == "ys"]
        for i in range(NB2):
            for kk in range(4):
                sc.op("pool", lambda e, i=i, kk=kk: e.memset(yk[i][kk][:, :], 0.0), W=[f"f_yk{i}_{kk}"])
        NB2 = 3

        def tile(t):
            i2 = t % NB2
            tk = slice(t * 128, (t + 1) * 128)
            tag = f"f_{i2}_"
            sc.dma("sp", lambda e: e.dma_start(out=h1[i2][:, :], in_=k.h1_d[tk, :]), R=[("dram", "h1", t)], W=[f"f_h1{i2}"])
            for kk in range(4):
                sc.dma("pool", lambda e, kk=kk: e.indirect_dma_start(
                    out=yk[i2][kk][:, :], out_offset=None, in_=k.ys_d[:, :],
                    in_offset=bass.IndirectOffsetOnAxis(ap=k.slots[:, t, kk:kk + 1], axis=0),
                    bounds_check=sc.bc_reg, oob_is_err=False),
                    R=(ys_w if t == 0 else []) + [("slots", t)], W=[f"f_yk{i2}_{kk}"])
            yield
            sc.op("dve", lambda e: e.scalar_tensor_tensor(acc[i2][:, :], yk[i2][0][:, :], k.wts[:, t, 0:1], h1[i2][:, :], ALU.mult, ALU.add),
                  R=[f"f_yk{i2}_0", ("wts", t), f"f_h1{i2}"], W=[tag + "z"])
            sc.op("dve", lambda e: e.scalar_tensor_tensor(acc[i2][:, :], h1[i2][:, :], ALPHA - 1.0, acc[i2][:, :], ALU.mult, ALU.add),
                  R=[tag + "z", f"f_h1{i2}"], W=[tag + "z"])
            for kk in range(1, 4):
                sc.op("dve", lambda e, kk=kk: e.scalar_tensor_tensor(acc[i2][:, :], yk[i2][kk][:, :], k.wts[:, t, kk:kk + 1], acc[i2][:, :], ALU.mult, ALU.add),
                      R=[f"f_yk{i2}_{kk}", ("wts", t), tag + "z"], W=[tag + "z"])
            yield
            for _ in layer_norm_tile(k, tag, acc[i2], g2, b2, out[i2], scr[i2], stages=True):
                yield
            sc.dma("sp", lambda e: e.dma_start(out=k.y[tk, :], in_=out[i2][:, :]), R=[tag + "o"], W=[("y", t)])
        run_pipelined((tile(t) for t in range(NT)), depth=3, skew=2)


def host_consts():
    c = {}
    c["c_ident"] = np.eye(128, dtype=np.float32).astype(ml_dtypes.bfloat16)
    c["c_identf"] = np.eye(128, dtype=np.float32)
    t = np.arange(S)
    row, col = t // 64, t % 64

    def tab(half):
        inv = (10000.0 ** (-np.arange(half, dtype=np.float32) / half)).astype(np.float32)
        ar = row.astype(np.float32)[:, None] * inv[None, :]
        ac = col.astype(np.float32)[:, None] * inv[None, :]
        cs = np.concatenate([np.cos(ar), np.cos(ar), np.cos(ac), np.cos(ac)], axis=1)
        sn = np.concatenate([-np.sin(ar), np.sin(ar), -np.sin(ac), np.sin(ac)], axis=1)
        return cs.astype(np.float32), sn.astype(np.float32)
    c64, s64 = tab(16)
    r64 = np.stack([c64, s64], 0).reshape(2, NT, 128, 64).transpose(2, 0, 1, 3)
    c["c_rope64"] = np.ascontiguousarray(r64)
    c32, s32 = tab(8)
    rT = np.stack([c32.T, s32.T], 0)
    rT = np.tile(rT[None], (4, 1, 1, 1)).transpose(0, 2, 1, 3).reshape(128, 2, S)
    c["c_ropeT"] = np.ascontiguousarray(rT)
    sel = np.zeros((128, 128), np.float32)
    sel[64, :] = 1.0
    c["c_sel"] = sel
    c["c_tri"] = np.triu(np.ones((128, 128), np.float32), 1).astype(ml_dtypes.bfloat16)
    c["c_ecap"] = np.tile((np.arange(NE, dtype=np.float32) * CAP)[None, :], (128, 1))
    return c


def swap_perm(n_heads, hd, rope_off, rope_dim):
    perm = np.arange(n_heads * hd)
    q = rope_dim // 4
    for h in range(n_heads):
        b = h * hd + rope_off
        for blk in range(2):
            o = b + blk * 2 * q
            perm[o:o + q] = np.arange(o + q, o + 2 * q)
            perm[o + q:o + 2 * q] = np.arange(o, o + q)
    return perm


_CACHE = {}


def kernel(**inputs):
    upto = inputs.pop("_upto", 99)
    dbg = tuple(inputs.pop("_dbg", ()))
    ncores = inputs.pop("_ncores", 8)
    key = (upto, dbg)
    if key not in _CACHE:
        _CACHE[key] = build(upto, dbg)
    nc, kk = _CACHE[key]
    f = lambda a: np.ascontiguousarray(np.asarray(a, dtype=np.float32))
    shared = {}
    shared["ln_in_g"] = f(inputs["ln_in_g"]).reshape(1, D)
    shared["ln_in_b"] = f(inputs["ln_in_b"]).reshape(1, D)
    shared["w_in_proj"] = f(inputs["w_in_proj"][0])
    shared["w_kr_sw"] = np.ascontiguousarray(shared["w_in_proj"][:, OFF_KR:OFF_KR + 32][:, swap_perm(1, 32, 0, 32)])
    shared["b_gate"] = f(inputs["b_gate"][0]).reshape(1, 3 * D)
    shared["gqa_q_norm"] = np.tile(f(inputs["gqa_q_norm"][0]), 8).reshape(1, 512)
    shared["gqa_k_norm"] = np.tile(f(inputs["gqa_k_norm"][0]), 2).reshape(1, 128)
    shared["mla_q_norm"] = f(inputs["mla_q_norm"][0]).reshape(1, 384)
    shared["mla_kv_norm"] = f(inputs["mla_kv_norm"][0]).reshape(1, 256)
    wqb = f(inputs["w_mla_qb"][0])
    shared["w_mla_qb"] = wqb
    shared["w_mla_qb_sw"] = np.ascontiguousarray(wqb[:, swap_perm(8, 96, 64, 32)])
    shared["w_mla_kvb"] = f(inputs["w_mla_kvb"][0])
    shared["w_mem_kv"] = f(inputs["w_mem_kv"][0])
    shared["w_br_gqa"] = f(inputs["w_br_gqa"][0])
    shared["w_br_mla"] = f(inputs["w_br_mla"][0])
    shared["w_br_mem"] = f(inputs["w_br_mem"][0])
    shared["w_out"] = f(inputs["w_out"][0])
    shared["ln1_g"] = f(inputs["ln1_g"][0]).reshape(1, D)
    shared["ln1_b"] = f(inputs["ln1_b"][0]).reshape(1, D)
    shared["w_router"] = f(inputs["w_router"][0])
    shared["b_router"] = f(inputs["b_router"][0]).reshape(1, NE)
    shared["w_exp_in"] = f(inputs["w_exp_in"][0])
    shared["b_exp_in"] = np.ascontiguousarray(
        f(inputs["b_exp_in"][0]).reshape(NE, 16, 128).transpose(2, 0, 1).reshape(128, NE * 16))
    shared["w_exp_out"] = f(inputs["w_exp_out"][0])
    shared["b_exp_out"] = f(inputs["b_exp_out"][0])
    shared["ln2_g"] = f(inputs["ln2_g"][0]).reshape(1, D)
    shared["ln2_b"] = f(inputs["ln2_b"][0]).reshape(1, D)
    shared.update(host_consts())
    x = f(inputs["x"])
    mem = f(inputs["mem"])
    in_maps = []
    for c in range(ncores):
        m = {n: shared[n] for n in kk.inputs if n in shared}
        m["x"] = x[c]
        if "mem" in kk.inputs:
            m["mem"] = mem[c]
        in_maps.append(m)
    res = run_bass_kernel_spmd(nc, in_maps, core_ids=list(range(ncores)))
    if dbg:
        return res.results
    return np.stack([r["y"] for r in res.results], axis=0)
```
